# Optimizing a Trainium2 kernel written in Bass

```python
import jax, jax.numpy as jnp
from jax import lax
import numpy as np

D_MODEL = 1024
BATCH = 4
SEQ = 4096
DEPTH = 1

NSA_HEADS = 8
NSA_KV_GROUPS = 2
NSA_HEADS_PER_GROUP = NSA_HEADS // NSA_KV_GROUPS
NSA_HEAD_DIM = 64
CMP_BLOCK = 32
CMP_STRIDE = 16
CMP_RATIO = CMP_BLOCK // CMP_STRIDE
CMP_HIDDEN = 256
SLC_BLOCK = 64
SLC_TOPK = 16
WINDOW = 512
Q_BLOCK = 128
GLA_HEADS = 4
GLA_KEY_DIM = 64
GLA_VAL_DIM = 128
GLA_RANK = 16
GLA_TAU = 16.0
GLA_CHUNK = 64
MOE_GROUPS = 4
MOE_EXPERTS_PER_GROUP = 8
MOE_N_EXPERTS = MOE_GROUPS * MOE_EXPERTS_PER_GROUP
MOE_TOPK = 2
MOE_D_FF = 512

ROPE_THETA = 10000.0
EPS = 1e-6
NEG_INF = -1e30
FORCED_SCORE = 1e4

NSA_Q_W = NSA_HEADS * NSA_HEAD_DIM
NSA_KV_W = NSA_KV_GROUPS * NSA_HEAD_DIM
GLA_QK_W = GLA_HEADS * GLA_KEY_DIM
GLA_V_W = GLA_HEADS * GLA_VAL_DIM
IN_SPLITS = (NSA_Q_W,) + (NSA_KV_W,) * 6 + (3 * NSA_HEADS, GLA_QK_W, GLA_QK_W, GLA_V_W, GLA_RANK, GLA_V_W, D_MODEL, D_MODEL)
IN_WIDTH = sum(IN_SPLITS)

kernel_name = "hybrid_nsa_gla_hmoe_block"


def rms_norm(x, g):
    xf = x.astype(jnp.float32)
    y = xf * lax.rsqrt(jnp.mean(xf * xf, axis=-1, keepdims=True) + EPS)
    return (y * g.astype(jnp.float32)).astype(x.dtype)


def rope(x, pos):
    half = x.shape[-1] // 2
    inv = 1.0 / (ROPE_THETA ** (jnp.arange(half, dtype=jnp.float32) / half))
    ang = pos.astype(jnp.float32)[..., None] * inv
    cos, sin = jnp.cos(ang), jnp.sin(ang)
    x1 = x[..., :half].astype(jnp.float32)
    x2 = x[..., half:].astype(jnp.float32)
    return jnp.concatenate([x1 * cos - x2 * sin, x2 * cos + x1 * sin], axis=-1).astype(x.dtype)


def _heads(t, n, dh):
    b, s, _ = t.shape
    return t.reshape(b, s, n, dh).transpose(0, 2, 1, 3)


def compress_blocks(t, pos_emb, w1, b1, w2, b2):
    bsz, g, seq, dh = t.shape
    chunks = t.reshape(bsz, g, seq // CMP_STRIDE, CMP_STRIDE, dh)
    n_cmp = seq // CMP_STRIDE - CMP_RATIO + 1
    blocks = jnp.concatenate([chunks[:, :, m:m + n_cmp] for m in range(CMP_RATIO)], axis=3)
    blocks = blocks + pos_emb
    flat = blocks.reshape(bsz, g, n_cmp, CMP_BLOCK * dh)
    return jax.nn.gelu(flat @ w1 + b1) @ w2 + b2


def cmp_to_slc_overlap(n_cmp, n_slc):
    c0 = CMP_STRIDE * jnp.arange(n_cmp)[:, None]
    s0 = SLC_BLOCK * jnp.arange(n_slc)[None, :]
    ov = jnp.clip(jnp.minimum(c0 + CMP_BLOCK, s0 + SLC_BLOCK) - jnp.maximum(c0, s0), 0, None)
    return ov.astype(jnp.float32) / CMP_BLOCK


def nsa_attention(q, k_c, v_c, k_s, v_s, k_w, v_w, gate_logits, positions,
                  cmp_pos_k, cmp_w1_k, cmp_b1_k, cmp_w2_k, cmp_b2_k,
                  cmp_pos_v, cmp_w1_v, cmp_b1_v, cmp_w2_v, cmp_b2_v):
    f32 = jnp.float32
    bsz, seq, _ = q.shape
    G, hpg, dh = NSA_KV_GROUPS, NSA_HEADS_PER_GROUP, NSA_HEAD_DIM
    pos_h = positions[:, None, :]
    q = (rope(_heads(q, NSA_HEADS, dh), pos_h) * dh ** -0.5).reshape(bsz, G, hpg, seq, dh)
    k_cmp = compress_blocks(_heads(k_c, G, dh), cmp_pos_k, cmp_w1_k, cmp_b1_k, cmp_w2_k, cmp_b2_k)
    v_cmp = compress_blocks(_heads(v_c, G, dh), cmp_pos_v, cmp_w1_v, cmp_b1_v, cmp_w2_v, cmp_b2_v)
    n_cmp = k_cmp.shape[2]
    cmp_end = CMP_STRIDE * jnp.arange(n_cmp) + CMP_BLOCK - 1
    k_cmp = rope(k_cmp, jnp.take(positions, cmp_end, axis=1)[:, None, :])
    n_slc = seq // SLC_BLOCK
    top_n = min(SLC_TOPK, n_slc)
    overlap = cmp_to_slc_overlap(n_cmp, n_slc)
    k_blocks = rope(_heads(k_s, G, dh), pos_h).reshape(bsz, G, n_slc, SLC_BLOCK, dh)
    v_blocks = _heads(v_s, G, dh).reshape(bsz, G, n_slc, SLC_BLOCK, dh)
    pad = ((0, 0), (0, 0), (WINDOW, 0), (0, 0))
    k_win = jnp.pad(rope(_heads(k_w, G, dh), pos_h), pad)
    v_win = jnp.pad(_heads(v_w, G, dh), pad)
    gates = jax.nn.sigmoid(gate_logits.astype(f32)).reshape(bsz, seq, NSA_HEADS, 3)
    gates = gates.transpose(0, 2, 1, 3).reshape(bsz, G, hpg, seq, 3)
    b_idx = jnp.arange(bsz)[:, None, None, None]
    g_idx = jnp.arange(G)[None, :, None, None]
    blk_ids = jnp.arange(n_slc)

    def query_block(c):
        q0 = c * Q_BLOCK
        t_idx = q0 + jnp.arange(Q_BLOCK)
        qc = lax.dynamic_slice_in_dim(q, q0, Q_BLOCK, axis=3)
        gc = lax.dynamic_slice_in_dim(gates, q0, Q_BLOCK, axis=3)
        s = jnp.einsum('bghqd,bgnd->bghqn', qc, k_cmp).astype(f32)
        cmp_ok = cmp_end[None, :] <= t_idx[:, None]
        p_cmp = jax.nn.softmax(jnp.where(cmp_ok, s, NEG_INF), axis=-1) * cmp_ok
        o_cmp = jnp.einsum('bghqn,bgnd->bghqd', p_cmp.astype(v_cmp.dtype), v_cmp)
        imp = jnp.einsum('bghqn,nj->bgqj', p_cmp, overlap)
        cur = t_idx // SLC_BLOCK
        forced = (blk_ids[None, :] == 0) | (blk_ids[None, :] == cur[:, None]) | (blk_ids[None, :] == cur[:, None] - 1)
        imp = jnp.where(forced, FORCED_SCORE, imp)
        imp = jnp.where(blk_ids[None, :] <= cur[:, None], imp, NEG_INF)
        _, sel = lax.top_k(imp, top_n)
        k_sel = k_blocks[b_idx, g_idx, sel].reshape(bsz, G, Q_BLOCK, top_n * SLC_BLOCK, dh)
        v_sel = v_blocks[b_idx, g_idx, sel].reshape(bsz, G, Q_BLOCK, top_n * SLC_BLOCK, dh)
        key_pos = (sel[..., None] * SLC_BLOCK + jnp.arange(SLC_BLOCK)).reshape(bsz, G, Q_BLOCK, top_n * SLC_BLOCK)
        sel_ok = (key_pos <= t_idx[:, None])[:, :, None]
        s = jnp.einsum('bghqd,bgqkd->bghqk', qc, k_sel).astype(f32)
        p = jax.nn.softmax(jnp.where(sel_ok, s, NEG_INF), axis=-1)
        o_slc = jnp.einsum('bghqk,bgqkd->bghqd', p.astype(v_sel.dtype), v_sel)
        kw = lax.dynamic_slice_in_dim(k_win, q0, Q_BLOCK + WINDOW, axis=2)
        vw = lax.dynamic_slice_in_dim(v_win, q0, Q_BLOCK + WINDOW, axis=2)
        kpos = q0 - WINDOW + jnp.arange(Q_BLOCK + WINDOW)
        win_ok = (kpos[None, :] >= 0) & (kpos[None, :] <= t_idx[:, None]) & (kpos[None, :] > t_idx[:, None] - WINDOW)
        s = jnp.einsum('bghqd,bgkd->bghqk', qc, kw).astype(f32)
        p = jax.nn.softmax(jnp.where(win_ok, s, NEG_INF), axis=-1)
        o_win = jnp.einsum('bghqk,bgkd->bghqd', p.astype(vw.dtype), vw)
        o = gc[..., 0:1] * o_cmp + gc[..., 1:2] * o_slc + gc[..., 2:3] * o_win
        return o.astype(q.dtype)

    out = lax.map(query_block, jnp.arange(seq // Q_BLOCK))
    return out.transpose(1, 0, 4, 2, 3, 5).reshape(bsz, seq, NSA_HEADS * dh)


def gla_attention(q, k, v, a_low, r, w_a2, b_a, norm_g):
    f32 = jnp.float32
    bsz, seq, _ = q.shape
    H, dk, dv, C = GLA_HEADS, GLA_KEY_DIM, GLA_VAL_DIM, GLA_CHUNK
    q = _heads(q, H, dk).astype(f32) * dk ** -0.5
    k = _heads(k, H, dk).astype(f32)
    v = _heads(v, H, dv).astype(f32)
    log_a = _heads(jax.nn.log_sigmoid((a_low @ w_a2 + b_a).astype(f32)) / GLA_TAU, H, dk)
    nc = seq // C

    def to_chunks(t):
        return t.reshape(bsz, H, nc, C, t.shape[-1]).transpose(2, 0, 1, 3, 4)

    causal = jnp.tril(jnp.ones((C, C), dtype=bool))[:, :, None]

    def step(state, xs):
        qc, kc, vc, lac = xs
        b = jnp.cumsum(lac, axis=2)
        o_inter = jnp.einsum('bhtd,bhde->bhte', qc * jnp.exp(b), state)
        diff = b[:, :, :, None, :] - b[:, :, None, :, :]
        decay = jnp.where(causal, jnp.exp(jnp.where(causal, diff, 0.0)), 0.0)
        attn = jnp.einsum('bhtd,bhsd,bhtsd->bhts', qc, kc, decay)
        o_intra = jnp.einsum('bhts,bhse->bhte', attn, vc)
        b_last = b[:, :, -1, :]
        k_dec = kc * jnp.exp(b_last[:, :, None, :] - b)
        state = jnp.exp(b_last)[..., None] * state + jnp.einsum('bhsd,bhse->bhde', k_dec, vc)
        return state, o_inter + o_intra

    state0 = jnp.zeros((bsz, H, dk, dv), f32)
    _, o = lax.scan(step, state0, (to_chunks(q), to_chunks(k), to_chunks(v), to_chunks(log_a)))
    o = o.transpose(1, 2, 0, 3, 4).reshape(bsz, H, seq, dv)
    o = o * lax.rsqrt(jnp.mean(o * o, axis=-1, keepdims=True) + EPS)
    o = o.transpose(0, 2, 1, 3).reshape(bsz, seq, H * dv) * norm_g.astype(f32)
    return (o * jax.nn.silu(r.astype(f32))).astype(r.dtype)


def hybrid_mixer(u, positions, w_in,
                 cmp_pos_k, cmp_w1_k, cmp_b1_k, cmp_w2_k, cmp_b2_k,
                 cmp_pos_v, cmp_w1_v, cmp_b1_v, cmp_w2_v, cmp_b2_v,
                 gla_w_a2, gla_b_a, gla_norm_g, w_proj_nsa, w_proj_gla, w_out):
    z = u @ w_in
    (nsa_q, k_c, v_c, k_s, v_s, k_w, v_w, nsa_g,
     g_q, g_k, g_v, g_a, g_r, m_a, m_b) = jnp.split(z, np.cumsum(IN_SPLITS)[:-1].tolist(), axis=-1)
    y_a = nsa_attention(nsa_q, k_c, v_c, k_s, v_s, k_w, v_w, nsa_g, positions,
                        cmp_pos_k, cmp_w1_k, cmp_b1_k, cmp_w2_k, cmp_b2_k,
                        cmp_pos_v, cmp_w1_v, cmp_b1_v, cmp_w2_v, cmp_b2_v) @ w_proj_nsa
    y_b = gla_attention(g_q, g_k, g_v, g_a, g_r, gla_w_a2, gla_b_a, gla_norm_g) @ w_proj_gla
    mixed = jax.nn.sigmoid(m_a) * y_a + jax.nn.sigmoid(m_b) * y_b
    return mixed @ w_out


def hier_moe(v, w_grp, b_grp, w_exp, b_exp, w_gate, w_up, w_down):
    f32 = jnp.float32
    bsz, seq, d = v.shape
    vt = v.reshape(-1, d)
    grp_prob = jax.nn.softmax((vt @ w_grp + b_grp).astype(f32), axis=-1)
    p_grp, g_sel = lax.top_k(grp_prob, 1)
    exp_logits = (vt @ w_exp + b_exp).astype(f32).reshape(-1, MOE_GROUPS, MOE_EXPERTS_PER_GROUP)
    in_grp = jnp.take_along_axis(exp_logits, g_sel[:, :, None], axis=1)[:, 0]
    p_in, e_sel = lax.top_k(jax.nn.softmax(in_grp, axis=-1), MOE_TOPK)
    w = p_grp * p_in / jnp.sum(p_in, axis=-1, keepdims=True)
    expert_id = g_sel * MOE_EXPERTS_PER_GROUP + e_sel
    combine = jnp.einsum('tk,tke->te', w, jax.nn.one_hot(expert_id, MOE_N_EXPERTS, dtype=f32))
    y = jnp.zeros(vt.shape, f32)
    for e in range(MOE_N_EXPERTS):
        hdn = jax.nn.silu(vt @ w_gate[e]) * (vt @ w_up[e])
        y = y + (combine[:, e:e + 1].astype(hdn.dtype) * hdn) @ w_down[e]
    return y.reshape(bsz, seq, d).astype(v.dtype)


def setup_inputs(seed: int = 0) -> dict:
    key = jax.random.key(seed)
    ks = jax.random.split(key, 32)
    f32 = jnp.float32
    L, D, dh = DEPTH, D_MODEL, NSA_HEAD_DIM

    def nrm(k, shape, scale):
        return jax.random.normal(k, shape, f32) * scale

    start = jax.random.randint(ks[1], (BATCH,), 0, 1024)
    positions = (start[:, None] + jnp.arange(SEQ)[None, :]).astype(jnp.int32)
    return {
        "x": nrm(ks[0], (BATCH, SEQ, D), 1.0),
        "positions": positions,
        "g_mix": 1.0 + nrm(ks[2], (L, D), 0.02),
        "w_in": nrm(ks[3], (L, D, IN_WIDTH), D ** -0.5),
        "cmp_pos_k": nrm(ks[4], (L, CMP_BLOCK, dh), 0.02),
        "cmp_w1_k": nrm(ks[5], (L, CMP_BLOCK * dh, CMP_HIDDEN), (CMP_BLOCK * dh) ** -0.5),
        "cmp_b1_k": nrm(ks[6], (L, CMP_HIDDEN), 0.01),
        "cmp_w2_k": nrm(ks[7], (L, CMP_HIDDEN, dh), CMP_HIDDEN ** -0.5),
        "cmp_b2_k": nrm(ks[8], (L, dh), 0.01),
        "cmp_pos_v": nrm(ks[9], (L, CMP_BLOCK, dh), 0.02),
        "cmp_w1_v": nrm(ks[10], (L, CMP_BLOCK * dh, CMP_HIDDEN), (CMP_BLOCK * dh) ** -0.5),
        "cmp_b1_v": nrm(ks[11], (L, CMP_HIDDEN), 0.01),
        "cmp_w2_v": nrm(ks[12], (L, CMP_HIDDEN, dh), CMP_HIDDEN ** -0.5),
        "cmp_b2_v": nrm(ks[13], (L, dh), 0.01),
        "gla_w_a2": nrm(ks[14], (L, GLA_RANK, GLA_QK_W), GLA_RANK ** -0.5),
        "gla_b_a": nrm(ks[15], (L, GLA_QK_W), 0.1),
        "gla_norm_g": 1.0 + nrm(ks[16], (L, GLA_V_W), 0.02),
        "w_proj_nsa": nrm(ks[17], (L, NSA_Q_W, D), NSA_Q_W ** -0.5),
        "w_proj_gla": nrm(ks[18], (L, GLA_V_W, D), GLA_V_W ** -0.5),
        "w_out": nrm(ks[19], (L, D, D), D ** -0.5),
        "g_ffn": 1.0 + nrm(ks[20], (L, D), 0.02),
        "w_grp": nrm(ks[21], (L, D, MOE_GROUPS), D ** -0.5),
        "b_grp": nrm(ks[22], (L, MOE_GROUPS), 0.01),
        "w_exp": nrm(ks[23], (L, D, MOE_N_EXPERTS), D ** -0.5),
        "b_exp": nrm(ks[24], (L, MOE_N_EXPERTS), 0.01),
        "w_gate": nrm(ks[25], (L, MOE_N_EXPERTS, D, MOE_D_FF), D ** -0.5),
        "w_up": nrm(ks[26], (L, MOE_N_EXPERTS, D, MOE_D_FF), D ** -0.5),
        "w_down": nrm(ks[27], (L, MOE_N_EXPERTS, MOE_D_FF, D), MOE_D_FF ** -0.5),
        "g_final": 1.0 + nrm(ks[28], (D,), 0.02),
    }


def reference(x, positions, g_mix, w_in,
              cmp_pos_k, cmp_w1_k, cmp_b1_k, cmp_w2_k, cmp_b2_k,
              cmp_pos_v, cmp_w1_v, cmp_b1_v, cmp_w2_v, cmp_b2_v,
              gla_w_a2, gla_b_a, gla_norm_g, w_proj_nsa, w_proj_gla, w_out,
              g_ffn, w_grp, b_grp, w_exp, b_exp, w_gate, w_up, w_down, g_final):
    h = x
    for layer in range(DEPTH):
        u = rms_norm(h, g_mix[layer])
        h = h + hybrid_mixer(u, positions, w_in[layer],
                             cmp_pos_k[layer], cmp_w1_k[layer], cmp_b1_k[layer], cmp_w2_k[layer], cmp_b2_k[layer],
                             cmp_pos_v[layer], cmp_w1_v[layer], cmp_b1_v[layer], cmp_w2_v[layer], cmp_b2_v[layer],
                             gla_w_a2[layer], gla_b_a[layer], gla_norm_g[layer],
                             w_proj_nsa[layer], w_proj_gla[layer], w_out[layer])
        h = h + hier_moe(rms_norm(h, g_ffn[layer]), w_grp[layer], b_grp[layer], w_exp[layer], b_exp[layer],
                         w_gate[layer], w_up[layer], w_down[layer])
    return rms_norm(h, g_final)
```

```python
import math
import numpy as np
from contextlib import ExitStack
import concourse.bass as bass
import concourse.mybir as mybir
from concourse.bass_utils import run_bass_kernel_spmd

F32 = mybir.dt.float32
BF16 = mybir.dt.bfloat16
I32 = mybir.dt.int32
AF = mybir.ActivationFunctionType
ALU = mybir.AluOpType
AX = mybir.AxisListType

SAME_ENGINE_SYNC = True
EPOCH = 12000
NEG = -30000.0
DBG_BR = None
DBG_D = 3
DBG_NOIF = False
DBG_E = '012'
D = 1024


class Stage:
    ENGS = ("tensor", "vector", "scalar", "gpsimd", "sync")

    def __init__(self, nc, name):
        self.nc = nc
        self.name = name
        self.es = ExitStack()
        self.ops = {e: [] for e in self.ENGS}
        self.last_write = {}
        self.readers = {}
        self.seen = {e: {} for e in self.ENGS}
        self.sems = {}
        self.cnt = {}
        self.epoch = {e: 0 for e in self.ENGS}
        self.dma_sems_by_eng = {e: set() for e in self.ENGS}
        self.nalloc = 0

    def sb(self, name, shape, dtype):
        return self.es.enter_context(self.nc.sbuf_tensor(f"{self.name}_{name}", list(shape), dtype))

    def ps(self, name, shape, dtype=F32):
        return self.es.enter_context(self.nc.psum_tensor(f"{self.name}_{name}", list(shape), dtype))

    def _sem(self, semkey):
        if semkey not in self.sems:
            self.nalloc += 1
            self.sems[semkey] = self.nc.alloc_semaphore(name=f"{self.name}_s{self.nalloc}")
            self.cnt[semkey] = 0
        return self.sems[semkey]

    def _deps(self, eng, reads, writes):
        need = {}

        def want(c):
            if c is None:
                return
            sk, v = c
            if need.get(sk, 0) < v:
                need[sk] = v

        for r in reads:
            want(self.last_write.get(r))
        for w in writes:
            want(self.last_write.get(w))
            for c in self.readers.get(w, ()):
                want(c)
        waits = []
        for sk, v in need.items():
            if (not SAME_ENGINE_SYNC or eng == "tensor") and sk[0] == "E" and sk[1] == eng:
                continue
            if self.seen[eng].get(sk, 0) >= v:
                continue
            self.seen[eng][sk] = v
            waits.append((sk, v))
        return waits

    def _commit(self, comp, reads, writes):
        for w in writes:
            self.last_write[w] = comp
            self.readers[w] = []
        for r in reads:
            if r in writes:
                continue
            self.readers.setdefault(r, []).append(comp)

    def op(self, eng, fn, reads=(), writes=()):
        reads = list(reads)
        writes = list(writes)
        waits = self._deps(eng, reads, writes)
        sk = ("E", eng, self.epoch[eng])
        self._sem(sk)
        self.cnt[sk] += 1
        comp = (sk, self.cnt[sk])
        if self.cnt[sk] >= EPOCH:
            self.epoch[eng] += 1
        self.ops[eng].append((fn, waits, sk, 1))
        self._commit(comp, reads, writes)
        return comp

    def dma(self, eng, fn, semkey, reads=(), writes=()):
        reads = list(reads)
        writes = list(writes)
        waits = self._deps(eng, reads, writes)
        sk = ("D", semkey)
        self._sem(sk)
        self.cnt[sk] += 16
        comp = (sk, self.cnt[sk])
        self.ops[eng].append((fn, waits, sk, 16))
        self.dma_sems_by_eng[eng].add(sk)
        self._commit(comp, reads, writes)
        return comp

    def do(self, eng, method, *args, reads=(), writes=(), **kw):
        return self.op(eng, lambda e: getattr(e, method)(*args, **kw), reads=reads, writes=writes)

    def dma_percore(self, ncores, mk, semkey, reads=(), writes=()):
        if DBG_NOIF:
            return self.dma("gpsimd", lambda e: mk(e, 0), semkey, reads=reads, writes=writes)
        sk = ("D", semkey)
        self._sem(sk)

        class _Done:
            def then_inc(self_, *a):
                return self_

        def fn(e):
            if getattr(self, "_pid", None) is None:
                self._pid = e.partition_id()
            pid = self._pid
            for k in range(ncores):
                with e.If(pid == k):
                    mk(e, k).then_inc(self.sems[sk], 16)
            return _Done()

        return self.dma("gpsimd", fn, semkey, reads=reads, writes=writes)

    def load(self, eng, out_ap, in_ap, key):
        return self.dma(eng, lambda e: e.dma_start(out=out_ap, in_=in_ap), key, writes=[key])

    def store(self, eng, out_ap, in_ap, key):
        return self.dma(eng, lambda e: e.dma_start(out=out_ap, in_=in_ap), key, reads=[key])

    def emit(self):
        nc = self.nc
        with nc.Block() as block:
            for ename in self.ENGS:
                ops = self.ops[ename]
                if not ops:
                    continue
                final = []
                for sk in self.dma_sems_by_eng[ename]:
                    final.append((sk, self.cnt[sk]))
                for ep in range(self.epoch[ename] + 1):
                    sk = ("E", ename, ep)
                    if sk in self.cnt and self.cnt[sk] > 0:
                        final.append((sk, self.cnt[sk]))

                def body(e, ops=ops, final=final):
                    for fn, waits, sk, inc in ops:
                        for wk, wv in waits:
                            e.wait_ge(self.sems[wk], wv)
                        fn(e).then_inc(self.sems[sk], inc)
                    for wk, wv in final:
                        e.wait_ge(self.sems[wk], wv)

                getattr(block, ename)(body)
        self.nc.clear_and_free_semaphores(list(self.sems.values()))
        self.nc.all_engine_barrier()
        self.es.close()


def host_consts(S):
    c = {}
    c["ident"] = np.eye(128, dtype=np.float32)
    half = 32
    inv = 1.0 / (10000.0 ** (np.arange(half, dtype=np.float32) / half))
    inv64 = np.concatenate([inv, inv]).astype(np.float32)
    c["invs"] = (np.concatenate([inv64, inv64]) / np.float32(2 * math.pi)).astype(np.float32).reshape(128, 1)
    tl = np.arange(128)[:, None]
    npr = np.arange(-1, 7)[None, :]
    c["cmpbias"] = np.where(16 * npr + 31 <= tl, 0.0, NEG).astype(np.float32)
    kk = np.arange(128)[None, :]
    c["tri"] = np.where(kk <= tl, 0.0, NEG).astype(np.float32)
    kw = np.arange(640)[None, :]
    c["winbias"] = np.where((kw > tl) & (kw <= tl + 512), 0.0, NEG).astype(np.float32)
    n = np.arange(256)[:, None]
    j = np.arange(64)[None, :]
    ov = np.clip(np.minimum(16 * n + 32, 64 * j + 64) - np.maximum(16 * n, 64 * j), 0, None) / 32.0
    ov[255] = 0.0
    c["overlap"] = ov.astype(np.float32)
    K = np.zeros((128, 3), np.float32)
    B = np.zeros((128, 3), np.float32)
    lo = np.arange(128) < 64
    K[lo] = [0, 0, 0]
    B[lo] = [1e4, 1e4, -1e30]
    K[~lo] = [1, 0, 0]
    B[~lo] = [0, 1e4, 1e4]
    c["selK"] = K
    c["selB"] = B
    c["rowvalid"] = (np.arange(128) >= 31).astype(np.float32).reshape(128, 1)
    s_ = np.arange(64)[:, None]
    t_ = np.arange(64)[None, :]
    c["glamask"] = (s_ <= t_).astype(np.float32)
    return c


CONST_SHAPES = {"ident": [128, 128], "invs": [128, 1], "cmpbias": [128, 8], "tri": [128, 128],
                "winbias": [128, 640], "overlap": [256, 64], "selK": [128, 3], "selB": [128, 3],
                "rowvalid": [128, 1], "glamask": [64, 64]}

WA_COLS = 3624
SRC = dict(q=0, kc=512, vc=640, ks=768, vs=896, kw=1024, vw=1152, gates=1280, gq=1304, gk=1560,
           gv=1816, al=2328, gr=2344, ma=2856, mb=3880)
DST = dict(q=0, qR=512, ks=1024, ksR=1152, kw=1280, kwR=1408, kc=1536, vc=1664, gq=1792, gk=2048,
           al=2304, vs=2320, vw=2448, gates=2576, gv=2600, gr=3112)


def bcast_rows(ap_row, nparts):
    return ap_row.broadcast_to([nparts, ap_row.shape[-1]])


def stage_R(nc, S, T, C, scr):
    st = Stage(nc, "R")
    pos = T["positions"]
    invs = st.sb("invs", [64, 1], F32)
    cosT = st.sb("cosT", [64, S], F32)
    sinT = st.sb("sinT", [64, S], F32)
    posi = st.sb("posi", [64, S], I32)
    tq = st.sb("tq", [64, S], F32)
    t2 = st.sb("t2", [64, S], F32)
    t3 = st.sb("t3", [64, S], F32)
    ni = st.sb("ni", [64, S], I32)
    st.load("sync", invs[:], C["invs"][0:64, :], "invs")
    st.load("sync", posi[:], bcast_rows(pos, 64), "posi")
    st.op("vector", lambda e: e.tensor_copy(out=tq[:], in_=posi[:]), reads=["posi"], writes=["tq"])
    st.op("vector", lambda e: e.tensor_scalar(out=tq[:], in0=tq[:], scalar1=invs[:, 0:1], scalar2=None, op0=ALU.mult),
          reads=["tq", "invs"], writes=["tq"])

    def table(dst, shift):
        st.op("vector", lambda e: e.tensor_scalar(out=t2[:], in0=tq[:], scalar1=float(shift), scalar2=None, op0=ALU.add),
              reads=["tq"], writes=["t2"])
        st.op("vector", lambda e: e.tensor_copy(out=ni[:], in_=t2[:]), reads=["t2"], writes=["ni"])
        st.op("vector", lambda e: e.tensor_copy(out=t3[:], in_=ni[:]), reads=["ni"], writes=["t3"])
        st.op("vector", lambda e: e.tensor_tensor(out=t2[:], in0=t2[:], in1=t3[:], op=ALU.subtract),
              reads=["t2", "t3"], writes=["t2"])
        st.op("vector", lambda e: e.tensor_scalar(out=t3[:], in0=t2[:], scalar1=0.5, scalar2=None, op0=ALU.is_gt),
              reads=["t2"], writes=["t3"])
        st.op("vector", lambda e: e.tensor_tensor(out=t2[:], in0=t2[:], in1=t3[:], op=ALU.subtract),
              reads=["t2", "t3"], writes=["t2"])
        st.op("vector", lambda e: e.tensor_scalar(out=t3[:], in0=t2[:], scalar1=-0.5, scalar2=None, op0=ALU.is_lt),
              reads=["t2"], writes=["t3"])
        st.op("vector", lambda e: e.tensor_tensor(out=t2[:], in0=t2[:], in1=t3[:], op=ALU.add),
              reads=["t2", "t3"], writes=["t2"])
        st.op("scalar", lambda e: e.activation(out=dst[:], in_=t2[:], func=AF.Sin, scale=6.283185),
              reads=["t2"], writes=[dst.name])

    table(sinT, 0.0)
    table(cosT, 0.25)
    st.store("sync", scr["SIN"], sinT[:], sinT.name)
    st.store("sync", scr["COS"], cosT[:], cosT.name)
    st.emit()


def stage_A(nc, S, T, C, scr):
    st = Stage(nc, "A")
    SH = S // 2
    NT = SH // 128
    NC4 = SH // 512
    x, w_in = T["x"], T["w_in"]
    uT = st.sb("uT", [128, 8, SH], BF16)
    WA = st.sb("WA", [128, 8, WA_COLS], BF16)
    ident = st.sb("ident", [128, 128], BF16)
    gm = st.sb("gm", [128, 8], F32)
    cosT = st.sb("cosT", [128, SH], F32)
    sinT = st.sb("sinT", [128, SH], F32)
    st.dma("gpsimd", lambda e: e.dma_start(out=ident[:], in_=C["ident"]), "ident", writes=["ident"])
    st.dma("sync", lambda e: e.dma_start(out=gm[:], in_=T["g_mix"].rearrange("o (kt p) -> p (o kt)", p=128)),
           "gm", writes=["gm"])

    wst = [st.sb(f"wst{i}", [128, 2856], F32) for i in range(2)]
    for kt in range(8):
        i = kt % 2
        wk = f"wst{i}"
        st.load("sync" if kt % 2 == 0 else "gpsimd", wst[i][:], w_in[0, kt * 128:(kt + 1) * 128, 0:2856], wk)
        g = gm[:, kt:kt + 1]
        wkey = ("WA", kt)

        def cp(eng, dst0, src0, n, mul=1.0, i=i, kt=kt, g=g, wk=wk, wkey=wkey):
            st.op(eng, lambda e: e.tensor_scalar(out=WA[:, kt, dst0:dst0 + n], in0=wst[i][:, src0:src0 + n],
                                                 scalar1=g, scalar2=float(mul), op0=ALU.mult, op1=ALU.mult),
                  reads=[wk, "gm"], writes=[wkey])

        def rot(eng, dst0, src0, nh, mul=1.0, i=i, kt=kt, g=g, wk=wk, wkey=wkey):
            s4 = wst[i][:, src0:src0 + nh * 64].rearrange("p (h two j) -> p h two j", two=2, j=32)
            d4 = WA[:, kt, dst0:dst0 + nh * 64].rearrange("p (h two j) -> p h two j", two=2, j=32)
            st.op(eng, lambda e: e.tensor_scalar(out=d4[:, :, 0, :], in0=s4[:, :, 1, :], scalar1=g, scalar2=float(-mul),
                                                 op0=ALU.mult, op1=ALU.mult), reads=[wk, "gm"], writes=[wkey])
            st.op(eng, lambda e: e.tensor_scalar(out=d4[:, :, 1, :], in0=s4[:, :, 0, :], scalar1=g, scalar2=float(mul),
                                                 op0=ALU.mult, op1=ALU.mult), reads=[wk, "gm"], writes=[wkey])

        cp("vector", DST["q"], SRC["q"], 512, 0.125)
        rot("gpsimd", DST["qR"], SRC["q"], 8, 0.125)
        cp("vector", DST["ks"], SRC["ks"], 128)
        rot("gpsimd", DST["ksR"], SRC["ks"], 2)
        cp("vector", DST["kw"], SRC["kw"], 128)
        rot("gpsimd", DST["kwR"], SRC["kw"], 2)
        cp("vector", DST["kc"], SRC["kc"], 256)
        cp("gpsimd", DST["gq"], SRC["gq"], 256, 0.125)
        cp("gpsimd", DST["gk"], SRC["gk"], 256)
        cp("vector", DST["al"], SRC["al"], 16)
        cp("vector", DST["vs"], SRC["vs"], 128)
        cp("vector", DST["vw"], SRC["vw"], 128)
        cp("vector", DST["gates"], SRC["gates"], 24)
        cp("gpsimd", DST["gv"], SRC["gv"], 512)
        cp("vector", DST["gr"], SRC["gr"], 512)
    WAK = [("WA", kt) for kt in range(8)]

    xt = [st.sb(f"xt{i}", [128, D], F32) for i in range(2)]
    sq = [st.sb(f"sq{i}", [128, D], BF16) for i in range(2)]
    xn = [st.sb(f"xn{i}", [128, D], BF16) for i in range(2)]
    ss = [st.sb(f"ss{i}", [128, 1], F32) for i in range(2)]
    rs = [st.sb(f"rs{i}", [128, 1], F32) for i in range(2)]
    ptr = [st.ps(f"ptr{i}", [128, 8, 128], BF16) for i in range(2)]
    pa = [st.ps(f"pa{i}", [128, 512], F32) for i in range(2)]
    pb = [st.ps(f"pb{i}", [128, 512], F32) for i in range(2)]
    r1 = [st.sb(f"r1{i}", [128, 512], F32) for i in range(2)]
    r2 = [st.sb(f"r2{i}", [128, 512], F32) for i in range(2)]
    fo = [st.sb(f"fo{i}", [128, 512], BF16) for i in range(3)]
    pt = [st.ps(f"pt{i}", [128, 512], F32) for i in range(2)]
    tA = [st.sb(f"tA{i}", [128, 256], BF16) for i in range(2)]
    tG = [st.sb(f"tG{i}", [128, 24], F32) for i in range(2)]
    tV = [st.sb(f"tV{i}", [128, 512], BF16) for i in range(2)]
    tR = [st.sb(f"tR{i}", [128, 512], BF16) for i in range(2)]
    for hs in range(2):
        H0 = hs * SH
        for hh in range(2):
            st.load("sync", cosT[hh * 64:(hh + 1) * 64, :], scr["COS"][:, H0:H0 + SH], cosT.name)
            st.load("sync", sinT[hh * 64:(hh + 1) * 64, :], scr["SIN"][:, H0:H0 + SH], sinT.name)
        for t in range(NT):
            i = t % 2
            st.load("sync", xt[i][:], x[H0 + t * 128:H0 + (t + 1) * 128, :], f"xt{i}")
            st.op("scalar", lambda e, i=i: e.activation(out=sq[i][:], in_=xt[i][:], func=AF.Square, accum_out=ss[i][:]),
                  reads=[f"xt{i}"], writes=[f"sq{i}", f"ss{i}"])
            st.op("vector", lambda e, i=i: e.tensor_scalar(out=rs[i][:], in0=ss[i][:], scalar1=1.0 / D, scalar2=1e-6,
                                                           op0=ALU.mult, op1=ALU.add), reads=[f"ss{i}"], writes=[f"rs{i}"])
            st.op("scalar", lambda e, i=i: e.activation(out=rs[i][:], in_=rs[i][:], func=AF.Sqrt),
                  reads=[f"rs{i}"], writes=[f"rs{i}"])
            st.op("vector", lambda e, i=i: e.reciprocal(out=rs[i][:], in_=rs[i][:]), reads=[f"rs{i}"], writes=[f"rs{i}"])
            st.op("vector", lambda e, i=i: e.tensor_scalar(out=xn[i][:], in0=xt[i][:], scalar1=rs[i][:, 0:1], scalar2=None,
                                                           op0=ALU.mult), reads=[f"xt{i}", f"rs{i}"], writes=[f"xn{i}"])
            for kt in range(8):
                st.op("tensor", lambda e, i=i, kt=kt: e.transpose(out=ptr[i][:, kt, :], in_=xn[i][:, kt * 128:(kt + 1) * 128],
                                                                  identity=ident[:]),
                      reads=[f"xn{i}", "ident"], writes=[f"ptr{i}"])
            st.op("scalar", lambda e, i=i, t=t: e.copy(out=uT[:, :, t * 128:(t + 1) * 128], in_=ptr[i][:]),
                  reads=[f"ptr{i}"], writes=[("uT", t)])

        rope_groups = [(DST["q"] + 128 * p, DST["qR"] + 128 * p, scr["QT"], 128 * p) for p in range(4)]
        rope_groups += [(DST["ks"], DST["ksR"], scr["KST"], 0), (DST["kw"], DST["kwR"], scr["KWT"], 0)]
        plain_groups = [(DST["kc"], 128, scr["KCT"], 0), (DST["vc"], 128, scr["VCT"], 0),
                        (DST["gq"], 128, scr["GQT"], 0), (DST["gq"] + 128, 128, scr["GQT"], 128),
                        (DST["gk"], 128, scr["GKT"], 0), (DST["gk"] + 128, 128, scr["GKT"], 128),
                        (DST["al"], 16, scr["ALT"], 0)]
        it = 0
        fi = 0
        for tc in range(NC4):
            tsl = slice(tc * 512, (tc + 1) * 512)
            ukeys = [("uT", tc * 4 + j) for j in range(4)]
            for (ca, cb, dst, row0) in rope_groups:
                i = it % 2
                it += 1
                f = fi % 3
                fi += 1
                for kt in range(8):
                    st.op("tensor", lambda e, i=i, kt=kt, ca=ca, tsl=tsl: e.matmul(pa[i][:], WA[:, kt, ca:ca + 128], uT[:, kt, tsl],
                                                                          start=(kt == 0), stop=(kt == 7)),
                          reads=ukeys + [("WA", kt)], writes=[f"pa{i}"])
                for kt in range(8):
                    st.op("tensor", lambda e, i=i, kt=kt, cb=cb, tsl=tsl: e.matmul(pb[i][:], WA[:, kt, cb:cb + 128], uT[:, kt, tsl],
                                                                          start=(kt == 0), stop=(kt == 7)),
                          reads=ukeys + [("WA", kt)], writes=[f"pb{i}"])
                st.op("vector", lambda e, i=i, tsl=tsl: e.tensor_tensor(out=r1[i][:], in0=pa[i][:], in1=cosT[:, tsl], op=ALU.mult),
                      reads=[f"pa{i}", cosT.name], writes=[f"r1{i}"])
                st.op("vector", lambda e, i=i, tsl=tsl: e.tensor_tensor(out=r2[i][:], in0=pb[i][:], in1=sinT[:, tsl], op=ALU.mult),
                      reads=[f"pb{i}", sinT.name], writes=[f"r2{i}"])
                st.op("gpsimd", lambda e, i=i, f=f: e.tensor_tensor(out=fo[f][:], in0=r1[i][:], in1=r2[i][:], op=ALU.add),
                      reads=[f"r1{i}", f"r2{i}"], writes=[f"fo{f}"])
                st.store("sync", dst[row0:row0 + 128, H0 + tc * 512:H0 + (tc + 1) * 512], fo[f][:], f"fo{f}")
            for (ca, n, dst, row0) in plain_groups:
                i = it % 2
                it += 1
                f = fi % 3
                fi += 1
                for kt in range(8):
                    st.op("tensor", lambda e, i=i, kt=kt, ca=ca, n=n, tsl=tsl: e.matmul(pa[i][0:n, :], WA[:, kt, ca:ca + n], uT[:, kt, tsl],
                                                                               start=(kt == 0), stop=(kt == 7)),
                          reads=ukeys + [("WA", kt)], writes=[f"pa{i}"])
                st.op("scalar", lambda e, i=i, f=f, n=n: e.copy(out=fo[f][0:n, :], in_=pa[i][0:n, :]),
                      reads=[f"pa{i}"], writes=[f"fo{f}"])
                st.store("sync", dst[row0:row0 + n, H0 + tc * 512:H0 + (tc + 1) * 512], fo[f][0:n, :], f"fo{f}")

        it = 0
        for t in range(NT):
            rows = slice(t * 128, (t + 1) * 128)
            drows = slice(H0 + t * 128, H0 + (t + 1) * 128)
            b = t % 2
            for (c0, n, kind) in [(DST["vs"], 280, 0), (DST["gv"], 512, 1), (DST["gr"], 512, 2)]:
                i = it % 2
                it += 1
                for kt in range(8):
                    st.op("tensor", lambda e, i=i, kt=kt, c0=c0, n=n, rows=rows: e.matmul(pt[i][:, 0:n], uT[:, kt, rows], WA[:, kt, c0:c0 + n],
                                                                               start=(kt == 0), stop=(kt == 7)),
                          reads=[("uT", t), ("WA", kt)], writes=[f"pt{i}"])
                if kind == 0:
                    st.op("vector", lambda e, i=i, b=b: e.tensor_copy(out=tA[b][:], in_=pt[i][:, 0:256]),
                          reads=[f"pt{i}"], writes=[f"tA{b}"])
                    st.op("scalar", lambda e, i=i, b=b: e.activation(out=tG[b][:], in_=pt[i][:, 256:280], func=AF.Sigmoid),
                          reads=[f"pt{i}"], writes=[f"tG{b}"])
                    st.store("gpsimd", scr["VS"][drows, :], tA[b][:, 0:128], f"tA{b}")
                    st.store("gpsimd", scr["VW"][drows, :], tA[b][:, 128:256], f"tA{b}")
                    st.store("gpsimd", scr["GATE"][drows, :], tG[b][:], f"tG{b}")
                elif kind == 1:
                    st.op("vector", lambda e, i=i, b=b: e.tensor_copy(out=tV[b][:], in_=pt[i][:]),
                          reads=[f"pt{i}"], writes=[f"tV{b}"])
                    st.store("gpsimd", scr["GV"][drows, :], tV[b][:], f"tV{b}")
                else:
                    st.op("scalar", lambda e, i=i, b=b: e.activation(out=tR[b][:], in_=pt[i][:], func=AF.Silu),
                          reads=[f"pt{i}"], writes=[f"tR{b}"])
                    st.store("gpsimd", scr["GR"][drows, :], tR[b][:], f"tR{b}")

    st.emit()


SCR_SPEC = lambda S: {
    "SIN": ([64, S], F32), "COS": ([64, S], F32),
    "QT": ([512, S], BF16), "KST": ([128, S], BF16), "KWT": ([128, S], BF16),
    "KCT": ([128, S], BF16), "VCT": ([128, S], BF16), "GQT": ([256, S], BF16), "GKT": ([256, S], BF16),
    "ALT": ([16, S], BF16), "VS": ([S, 128], BF16), "VW": ([S, 128], BF16), "GATE": ([S, 24], F32),
    "GV": ([S, 512], BF16), "GR": ([S, 512], BF16),
    "KCMPT": ([128, S // 16], BF16), "VCMP": ([2, S // 16, 64], BF16),
    "ONSA": ([S, 512], BF16), "OGLA": ([S, 512], BF16),
}

IN_SHAPES = lambda S: {
    "x": ([S, D], F32), "xh": ([S // 2, D], F32), "positions": ([1, S], I32), "g_mix": ([1, D], F32),
    "w_in": ([1, D, 4904], F32),
    "cmp_pos_k": ([1, 32, 64], F32), "cmp_w1_k": ([1, 2048, 256], F32), "cmp_b1_k": ([1, 256], F32),
    "cmp_w2_k": ([1, 256, 64], F32), "cmp_b2_k": ([1, 64], F32),
    "cmp_pos_v": ([1, 32, 64], F32), "cmp_w1_v": ([1, 2048, 256], F32), "cmp_b1_v": ([1, 256], F32),
    "cmp_w2_v": ([1, 256, 64], F32), "cmp_b2_v": ([1, 64], F32),
    "gla_w_a2": ([1, 16, 256], F32), "gla_b_a": ([1, 256], F32), "gla_norm_g": ([1, 512], F32),
    "w_proj_nsa": ([1, 512, D], F32), "w_proj_gla": ([1, 512, D], F32), "w_out": ([1, D, D], F32),
    "g_ffn": ([1, D], F32), "w_grp": ([1, D, 4], F32), "b_grp": ([1, 4], F32), "w_exp": ([1, D, 32], F32),
    "b_exp": ([1, 32], F32), "w_gate": ([1, 32, D, 512], F32), "w_up": ([1, 32, D, 512], F32),
    "w_down": ([1, 32, 512, D], F32), "g_final": ([1, D], F32), "halfidx": ([1, 1], I32),
}


def build(S, stages="ABCDE", debug=(), scr_in=(), ncores=2):
    return build_full(S, ncores, stages=("R" + stages) if "A" in stages else stages, debug=debug, scr_in=scr_in)


def stage_B(nc, S, T, C, scr):
    st = Stage(nc, "B")
    NCMP = S // 16 - 1
    srcT = {"k": st.sb("kcT", [128, S], BF16), "v": st.sb("vcT", [128, S], BF16)}
    st.load("sync", srcT["k"][:], scr["KCT"], "kcT")
    st.load("sync", srcT["v"][:], scr["VCT"], "vcT")
    cosf = st.sb("cosf", [64, S], F32)
    sinf = st.sb("sinf", [64, S], F32)
    st.load("sync", cosf[:], scr["COS"], "cosf")
    st.load("sync", sinf[:], scr["SIN"], "sinf")
    cos_e = cosf[:, 31:31 + 16 * (NCMP - 1) + 1:16]
    sin_e = sinf[:, 31:31 + 16 * (NCMP - 1) + 1:16]
    ph = [st.ps(f"ph{i}", [128, 512], F32) for i in range(2)]
    pbias = st.ps("pbias", [128, 2], F32)
    pk = [st.ps(f"pk{i}", [64, 512], F32) for i in range(2)]
    pv = st.ps("pv", [128, 64], F32)
    for kv in ("k", "v"):
        W1 = st.sb(f"W1{kv}", [128, 32, 256], BF16)
        w1src = T[f"cmp_w1_{kv}"][0].rearrange("(i d) h -> d i h", d=64)
        st.dma("gpsimd", lambda e, W1=W1, w1src=w1src: e.dma_start(out=W1[0:64], in_=w1src), f"W1{kv}", writes=[f"W1{kv}"])
        st.dma("gpsimd", lambda e, W1=W1, w1src=w1src: e.dma_start(out=W1[64:128], in_=w1src), f"W1{kv}", writes=[f"W1{kv}"])
        posf = st.sb(f"posf{kv}", [64, 32], F32)
        posb = st.sb(f"posb{kv}", [64, 32], BF16)
        st.load("sync", posf[:], T[f"cmp_pos_{kv}"][0].rearrange("i d -> d i"), f"posf{kv}")
        st.do("vector", "tensor_copy", out=posb[:], in_=posf[:], reads=[f"posf{kv}"], writes=[f"posb{kv}"])
        b1 = st.sb(f"b1{kv}", [128, 2], F32)
        st.load("sync", b1[:], T[f"cmp_b1_{kv}"].rearrange("o (hh p) -> p (o hh)", p=128), f"b1{kv}")
        w2f = st.sb(f"w2f{kv}", [128, 2, 64], F32)
        st.load("sync", w2f[:], T[f"cmp_w2_{kv}"][0].rearrange("(hh p) d -> p hh d", p=128), f"w2f{kv}")
        W2 = st.sb(f"W2{kv}", [128, 2, 64], BF16)
        st.do("vector", "tensor_copy", out=W2[:], in_=w2f[:], reads=[f"w2f{kv}"], writes=[f"W2{kv}"])
        bias1 = st.sb(f"bias1{kv}", [128, 2], F32)
        for hh in range(2):
            for i in range(32):
                st.do("tensor", "matmul", pbias[:, hh:hh + 1], W1[0:64, i, hh * 128:(hh + 1) * 128], posb[:, i:i + 1],
                      start=(i == 0), stop=(i == 31), reads=[f"W1{kv}", f"posb{kv}"], writes=["pbias"])
        st.do("vector", "tensor_tensor", out=bias1[:], in0=pbias[:], in1=b1[:], op=ALU.add,
              reads=["pbias", f"b1{kv}"], writes=[f"bias1{kv}"])
        if kv == "k":
            W2R = st.sb("W2R", [128, 2, 64], BF16)
            for hh in range(2):
                st.do("vector", "tensor_scalar", out=W2R[:, hh, 0:32], in0=w2f[:, hh, 32:64], scalar1=-1.0, scalar2=None,
                      op0=ALU.mult, reads=["w2fk"], writes=["W2R"])
                st.do("vector", "tensor_copy", out=W2R[:, hh, 32:64], in_=w2f[:, hh, 0:32], reads=["w2fk"], writes=["W2R"])
            b2 = st.sb("b2k", [64, 1], F32)
            b2R = st.sb("b2R", [64, 1], F32)
            b2src = T["cmp_b2_k"].rearrange("o d -> d o")
            st.load("sync", b2[:], b2src, "b2k")
            st.load("sync", b2R[0:32], b2src[32:64], "b2R")
            st.load("sync", b2R[32:64], b2src[0:32], "b2R")
            st.do("vector", "tensor_scalar", out=b2R[0:32], in0=b2R[0:32], scalar1=-1.0, scalar2=None, op0=ALU.mult,
                  reads=["b2R"], writes=["b2R"])
        else:
            b2v = st.sb("b2v", [128, 64], F32)
            st.load("sync", b2v[:], bcast_rows(T["cmp_b2_v"], 128), "b2v")
        for g in range(2):
            gp = slice(g * 64, (g + 1) * 64)
            h1T = [st.sb(f"h1T{kv}{g}{hh}", [128, 256 if NCMP <= 256 else NCMP], BF16) for hh in range(2)]
            for hh in range(2):
                hk = f"h1T{kv}{g}{hh}"
                for i in range(32):
                    st.do("tensor", "matmul", ph[hh][:, 0:NCMP], W1[gp, i, hh * 128:(hh + 1) * 128],
                          srcT[kv][gp, i:i + 16 * (NCMP - 1) + 1:16], start=(i == 0), stop=(i == 31),
                          reads=[f"W1{kv}", f"{kv}cT"], writes=[f"ph{hh}"])
                xh = st.sb(f"xh{kv}{g}{hh}", [128, NCMP], F32)
                t1 = st.sb(f"t1{kv}{g}{hh}", [128, NCMP], F32)
                xk, tk = f"xh{kv}{g}{hh}", f"t1{kv}{g}{hh}"
                st.do("scalar", "activation", out=xh[:], in_=ph[hh][:, 0:NCMP], func=AF.Identity, bias=bias1[:, hh:hh + 1],
                      reads=[f"ph{hh}", f"bias1{kv}"], writes=[xk])
                st.do("vector", "tensor_tensor", out=t1[:], in0=xh[:], in1=xh[:], op=ALU.mult, reads=[xk], writes=[tk])
                st.do("vector", "tensor_scalar", out=t1[:], in0=t1[:], scalar1=0.044715, scalar2=1.0, op0=ALU.mult, op1=ALU.add,
                      reads=[tk], writes=[tk])
                st.do("vector", "tensor_tensor", out=t1[:], in0=t1[:], in1=xh[:], op=ALU.mult, reads=[tk, xk], writes=[tk])
                st.do("scalar", "activation", out=t1[:], in_=t1[:], func=AF.Tanh, scale=0.7978845608028654,
                      reads=[tk], writes=[tk])
                st.do("vector", "tensor_scalar", out=t1[:], in0=t1[:], scalar1=1.0, scalar2=0.5, op0=ALU.add, op1=ALU.mult,
                      reads=[tk], writes=[tk])
                st.do("vector", "tensor_tensor", out=h1T[hh][:, 0:NCMP], in0=t1[:], in1=xh[:], op=ALU.mult,
                      reads=[tk, xk], writes=[hk])
            hks = [f"h1T{kv}{g}{hh}" for hh in range(2)]
            if kv == "k":
                for hh in range(2):
                    st.do("tensor", "matmul", pk[0][:, 0:NCMP], W2[:, hh, :], h1T[hh][:, 0:NCMP], start=(hh == 0), stop=(hh == 1),
                          reads=hks + ["W2k"], writes=["pk0"])
                for hh in range(2):
                    st.do("tensor", "matmul", pk[1][:, 0:NCMP], W2R[:, hh, :], h1T[hh][:, 0:NCMP], start=(hh == 0), stop=(hh == 1),
                          reads=hks + ["W2R"], writes=["pk1"])
                ka = st.sb(f"ka{g}", [64, NCMP], F32)
                kb = st.sb(f"kb{g}", [64, NCMP], F32)
                ko = st.sb(f"ko{g}", [64, NCMP], BF16)
                st.do("scalar", "activation", out=ka[:], in_=pk[0][:, 0:NCMP], func=AF.Identity, bias=b2[:, 0:1],
                      reads=["pk0", "b2k"], writes=[f"ka{g}"])
                st.do("scalar", "activation", out=kb[:], in_=pk[1][:, 0:NCMP], func=AF.Identity, bias=b2R[:, 0:1],
                      reads=["pk1", "b2R"], writes=[f"kb{g}"])
                st.do("vector", "tensor_tensor", out=ka[:], in0=ka[:], in1=cos_e, op=ALU.mult, reads=[f"ka{g}", "cosf"], writes=[f"ka{g}"])
                st.do("vector", "tensor_tensor", out=kb[:], in0=kb[:], in1=sin_e, op=ALU.mult, reads=[f"kb{g}", "sinf"], writes=[f"kb{g}"])
                st.do("vector", "tensor_tensor", out=ko[:], in0=ka[:], in1=kb[:], op=ALU.add, reads=[f"ka{g}", f"kb{g}"], writes=[f"ko{g}"])
                st.store("sync", scr["KCMPT"][gp, 0:NCMP], ko[:], f"ko{g}")
            else:
                for ci, n0 in enumerate(range(0, NCMP, 128)):
                    n = min(128, NCMP - n0)
                    for hh in range(2):
                        st.do("tensor", "matmul", pv[0:n, :], h1T[hh][:, n0:n0 + n], W2[:, hh, :], start=(hh == 0), stop=(hh == 1),
                              reads=hks + ["W2v"], writes=["pv"])
                    vo = st.sb(f"vo{g}{ci}", [128, 64], BF16)
                    st.do("vector", "tensor_tensor", out=vo[0:n, :], in0=pv[0:n, :], in1=b2v[0:n, :], op=ALU.add,
                          reads=["pv", "b2v"], writes=[f"vo{g}{ci}"])
                    st.store("sync", scr["VCMP"][g, n0:n0 + n, :], vo[0:n, :], f"vo{g}{ci}")
    st.emit()


def stage_C(nc, S, T, C, scr):
    st = Stage(nc, "C")
    NT = S // 128
    NSLC = S // 64
    NCP = S // 16
    NCH = (NCP + 127) // 128
    SW = 640
    ident = st.sb("ident", [128, 128], BF16)
    st.dma("gpsimd", lambda e: e.dma_start(out=ident[:], in_=C["ident"]), "ident", writes=["ident"])
    ovl = st.sb("ovl", [128, NCH, NSLC], BF16)
    for j in range(NCH):
        n = min(128, NCP - j * 128)
        st.dma("gpsimd", lambda e, j=j, n=n: e.dma_start(out=ovl[0:n, j, :], in_=C["overlap"][j * 128:j * 128 + n, 0:NSLC]),
               "ovl", writes=["ovl"])
    cst = {}
    for k in ("cmpbias", "tri", "winbias", "selK", "selB", "rowvalid"):
        cst[k] = st.sb("c_" + k, CONST_SHAPES[k], F32)
        st.load("sync", cst[k][:], C[k], "c_" + k)
    gates = st.sb("gates", [128, NT, 24], F32)
    st.load("sync", gates[:], scr["GATE"].rearrange("(c p) k -> p c k", p=128), "gates")

    ps = [st.ps(f"ps{i}", [128, 512], F32) for i in range(2)]
    poS = st.ps("poS", [128, 512], F32)
    pT = [st.ps(f"pT{i}", [128, 8, 128], BF16) for i in range(2)]
    po = st.ps("po", [128, 512], F32)
    po2 = st.ps("po2", [128, 512], F32)
    pimp = st.ps("pimp", [128, 512], F32)

    S_sb = [st.sb(f"S_sb{i}", [128, S], F32) for i in range(4)]
    P_sb = [st.sb(f"P_sb{i}", [128, S], BF16) for i in range(4)]
    PT4 = st.sb("PT4", [128, NT, 4, 128], BF16)
    oT_sb = st.sb("oT_sb", [64, 512], BF16)
    Sc = [st.sb(f"Sc{i}", [128, 256], F32) for i in range(4)]
    Pc = [st.sb(f"Pc{i}", [128, 256], BF16) for i in range(4)]
    mxc = [st.sb(f"mxc{i}", [128, 1], F32) for i in range(4)]
    ocmp = [st.sb(f"ocmp{i}", [128, 256], F32) for i in range(2)]
    Sw = [st.sb(f"Sw{i}", [128, SW], F32) for i in range(4)]
    Pw = [st.sb(f"Pw{i}", [128, SW], BF16) for i in range(4)]
    PTw = [st.sb(f"PTw{i}", [128, 5, 128], BF16) for i in range(2)]
    mx = [st.sb(f"mx{i}", [128, 1], F32) for i in range(4)]
    mxw = [st.sb(f"mxw{i}", [128, 1], F32) for i in range(4)]
    sums = [st.sb(f"sums{i}", [128, 12], F32) for i in range(2)]
    rr = [st.sb(f"rr{i}", [128, 12], F32) for i in range(2)]
    impS = st.sb("impS", [128, NSLC], F32)
    imp2 = st.sb("imp2", [128, NSLC], F32)
    m8a = st.sb("m8a", [128, 8], F32)
    m8b = st.sb("m8b", [128, 8], F32)
    mb = st.sb("mb", [128, NSLC], F32)
    acc = st.sb("acc", [128, 256], F32)
    oout = [st.sb(f"oout{i}", [128, 256], BF16) for i in range(2)]
    QTb = [st.sb(f"QTb{i}", [64, 4, 128], BF16) for i in range(2)]
    KS = st.sb("KS", [64, S], BF16)
    KW = st.sb("KW", [64, S], BF16)
    VS = st.sb("VS", [128, NT, 64], BF16)
    VW = st.sb("VW", [128, NT, 64], BF16)
    KC = st.sb("KC", [64, NCP], BF16)
    VC = st.sb("VC", [128, NCH, 64], BF16)
    cnt = {"tb": 0, "sb": 0, "cp": 0}

    def next_ps():
        i = cnt["sb"] % 2
        cnt["sb"] += 1
        return i

    def tail_group(items, act_only=False, no_pv=False):
        rounds = []
        for it in items:
            L = it[4]
            nkt = (L + 127) // 128
            for k0 in range(0, nkt, 8):
                rounds.append((it, k0, min(8, nkt - k0), nkt))

        def emit_T(r):
            (Pt, pkey, PTt, ptkey, L, Vt, vkey, po_ap, pokey, extra), k0, nb, nkt = r
            b = cnt["tb"] % 2
            cnt["tb"] += 1
            for kk in range(nb):
                kt = k0 + kk
                nj = min(128, L - kt * 128)
                st.do("tensor", "transpose", out=pT[b][0:nj, kk, :], in_=Pt[:, kt * 128:kt * 128 + nj], identity=ident[:],
                      reads=[pkey, "ident"], writes=[f"pT{b}"])
            njl = min(128, L - (k0 + nb - 1) * 128)
            cnt["cp"] += 1
            full = nb if njl == 128 else nb - 1
            if act_only or cnt["cp"] % 2 == 0:
                eng, meth = "scalar", "copy"
            else:
                eng, meth = "vector", "tensor_copy"
            if full > 0:
                st.do(eng, meth, out=PTt[:, k0:k0 + full, :], in_=pT[b][:, 0:full, :], reads=[f"pT{b}"], writes=[ptkey])
            if full < nb:
                st.do(eng, meth, out=PTt[0:njl, k0 + nb - 1, :], in_=pT[b][0:njl, nb - 1, :], reads=[f"pT{b}"], writes=[ptkey])

        def emit_PV(r):
            (Pt, pkey, PTt, ptkey, L, Vt, vkey, po_ap, pokey, extra), k0, nb, nkt = r
            for kk in range(nb):
                kt = k0 + kk
                nj = min(128, L - kt * 128)
                st.do("tensor", "matmul", po_ap, PTt[0:nj, kt, :], Vt(kt, nj), start=(kt == 0), stop=(kt == nkt - 1),
                      reads=[ptkey, vkey], writes=[pokey])
                if extra is not None:
                    extra(kt, nj, nkt)

        for i, r in enumerate(rounds):
            emit_T(r)
            if i >= 1 and not no_pv:
                emit_PV(rounds[i - 1])
        if not no_pv:
            emit_PV(rounds[-1])

    for g in range(2):
        gp = slice(g * 64, (g + 1) * 64)
        st.load("sync", KS[:], scr["KST"][gp, :], "KS")
        st.load("sync", KW[:], scr["KWT"][gp, :], "KW")
        st.load("gpsimd", VS[:], scr["VS"][:, gp].rearrange("(kt p) d -> p kt d", p=128), "VS")
        st.load("gpsimd", VW[:], scr["VW"][:, gp].rearrange("(kt p) d -> p kt d", p=128), "VW")
        st.load("sync", KC[:, 0:NCP - 1], scr["KCMPT"][gp, 0:NCP - 1], "KC")
        st.do("vector", "memset", VC[:], 0.0, reads=[], writes=["VC"])
        for j in range(NCH):
            n = min(128, NCP - 1 - j * 128)
            st.load("sync", VC[0:n, j, :], scr["VCMP"][g, j * 128:j * 128 + n, :], "VC")

        def make_block(c):
            use_sel = (2 * c + 2) > 16
            cblk = slice(c * 128, (c + 1) * 128)
            cb = c % 2
            qb = c % 2
            qk = f"QTb{qb}"
            ncmp = 8 * c + 7
            ncp32 = ((ncmp + 31) // 32) * 32
            L = 128 * (c + 1)
            k0w = max(0, 128 * c - 512)
            Lw = L - k0w
            boff = k0w - (128 * c - 512)
            kt0 = k0w // 128

            def slc_p1():
                for h in range(4):
                    sk = f"S_sb{h}"
                    for k0 in range(0, L, 512):
                        w = min(512, L - k0)
                        si = next_ps()
                        st.do("tensor", "matmul", ps[si][:, 0:w], QTb[qb][:, h, :], KS[:, k0:k0 + w], start=True, stop=True,
                              reads=[qk, "KS"], writes=[f"ps{si}"])
                        if use_sel:
                            nb = w // 64
                            mbb = mb[:, k0 // 64:k0 // 64 + nb].unsqueeze(2).broadcast_to([128, nb, 64])
                            st.do("vector", "tensor_tensor", out=S_sb[h][:, k0:k0 + w].rearrange("p (b k) -> p b k", k=64),
                                  in0=ps[si][:, 0:w].rearrange("p (b k) -> p b k", k=64), in1=mbb, op=ALU.add,
                                  reads=[f"ps{si}", "mb"], writes=[sk])
                        else:
                            st.do("scalar", "copy", out=S_sb[h][:, k0:k0 + w], in_=ps[si][:, 0:w], reads=[f"ps{si}"], writes=[sk])
                    st.do("vector", "tensor_tensor", out=S_sb[h][:, L - 128:L], in0=S_sb[h][:, L - 128:L], in1=cst["tri"][:], op=ALU.add,
                          reads=[sk, "c_tri"], writes=[sk])

            def slc_p2():
                for h in range(4):
                    st.do("vector", "reduce_max", out=mx[h][:], in_=S_sb[h][:, 0:L], axis=AX.X, negate=True, reads=[f"S_sb{h}"], writes=[f"mx{h}"])

            def slc_p3():
                for h in range(4):
                    sidx = h * 3 + 1
                    st.do("scalar", "activation", out=P_sb[h][:, 0:L], in_=S_sb[h][:, 0:L], func=AF.Exp, bias=mx[h][:, 0:1],
                          accum_out=sums[cb][:, sidx:sidx + 1], reads=[f"S_sb{h}", f"mx{h}"], writes=[f"P_sb{h}", (f"sums{cb}", sidx)])

            def slc_p4():
                tail_group([(P_sb[h], f"P_sb{h}", PT4[:, :, h, :], "PT4", L, None, "VS", None, None, None) for h in range(4)],
                           act_only=True, no_pv=True)
                nkt = c + 1
                for kt in range(nkt):
                    st.do("tensor", "matmul", poS[0:64, :], VS[:, kt, :], PT4[:, kt, :, :].rearrange("p h t -> p (h t)"),
                          start=(kt == 0), stop=(kt == nkt - 1), reads=["PT4", "VS"], writes=["poS"])
                st.do("scalar", "copy", out=oT_sb[:], in_=poS[0:64, :], reads=["poS"], writes=["oT_sb"])
                b = cnt["tb"] % 2
                cnt["tb"] += 1
                slc_back["b"] = b
                for h in range(4):
                    st.do("tensor", "transpose", out=pT[b][:, h, 0:64], in_=oT_sb[:, h * 128:(h + 1) * 128], identity=ident[0:64, 0:64],
                          reads=["oT_sb", "ident"], writes=[f"pT{b}"])

            def win_p1():
                for h in range(4):
                    for k0 in range(0, Lw, 512):
                        w = min(512, Lw - k0)
                        si = next_ps()
                        st.do("tensor", "matmul", ps[si][:, 0:w], QTb[qb][:, h, :], KW[:, k0w + k0:k0w + k0 + w], start=True, stop=True,
                              reads=[qk, "KW"], writes=[f"ps{si}"])
                        st.do("vector", "tensor_tensor", out=Sw[h][:, k0:k0 + w], in0=ps[si][:, 0:w],
                              in1=cst["winbias"][:, boff + k0:boff + k0 + w], op=ALU.add, reads=[f"ps{si}", "c_winbias"], writes=[f"Sw{h}"])

            def win_p2():
                for h in range(4):
                    st.do("vector", "reduce_max", out=mxw[h][:], in_=Sw[h][:, 0:Lw], axis=AX.X, negate=True, reads=[f"Sw{h}"], writes=[f"mxw{h}"])

            def win_p3():
                for h in range(4):
                    sidx = h * 3 + 2
                    st.do("scalar", "activation", out=Pw[h][:, 0:Lw], in_=Sw[h][:, 0:Lw], func=AF.Exp, bias=mxw[h][:, 0:1],
                          accum_out=sums[cb][:, sidx:sidx + 1], reads=[f"Sw{h}", f"mxw{h}"], writes=[f"Pw{h}", (f"sums{cb}", sidx)])

            def win_p4():
                tail_group([(Pw[h], f"Pw{h}", PTw[h % 2], f"PTw{h % 2}", Lw, lambda kt, nj, kt0=kt0: VW[:, kt0 + kt, :], "VW",
                             po2[:, h * 64:(h + 1) * 64], "po_win", None) for h in range(4)])

            slc_back = {}

            def X():
                st.load("sync", QTb[qb][:], scr["QT"][g * 256:(g + 1) * 256, cblk].rearrange("(h d) t -> d h t", d=64), qk)
                for h in range(4):
                    si = next_ps()
                    st.do("tensor", "matmul", ps[si][:, 0:ncmp], QTb[qb][:, h, :], KC[:, 0:ncmp], start=True, stop=True,
                          reads=[qk, "KC"], writes=[f"ps{si}"])
                    if ncmp > 8:
                        st.do("scalar", "copy", out=Sc[h][:, 0:ncmp - 8], in_=ps[si][:, 0:ncmp - 8], reads=[f"ps{si}"], writes=[f"Sc{h}"])
                        st.do("vector", "tensor_tensor", out=Sc[h][:, ncmp - 8:ncmp], in0=ps[si][:, ncmp - 8:ncmp],
                              in1=cst["cmpbias"][:, 0:8], op=ALU.add, reads=[f"ps{si}", "c_cmpbias"], writes=[f"Sc{h}"])
                    else:
                        st.do("vector", "tensor_tensor", out=Sc[h][:, 0:7], in0=ps[si][:, 0:7],
                              in1=cst["cmpbias"][:, 1:8], op=ALU.add, reads=[f"ps{si}", "c_cmpbias"], writes=[f"Sc{h}"])
                for h in range(4):
                    st.do("vector", "reduce_max", out=mxc[h][:], in_=Sc[h][:, 0:ncmp], axis=AX.X, negate=True, reads=[f"Sc{h}"], writes=[f"mxc{h}"])
                for h in range(4):
                    sidx = h * 3
                    st.do("scalar", "activation", out=Sc[h][:, 0:ncmp], in_=Sc[h][:, 0:ncmp], func=AF.Exp, bias=mxc[h][:, 0:1],
                          accum_out=sums[cb][:, sidx:sidx + 1], reads=[f"Sc{h}", f"mxc{h}"], writes=[f"Sc{h}", (f"sums{cb}", sidx)])
                for h in range(4):
                    sidx = h * 3
                    st.do("vector", "reciprocal", out=mxc[h][:], in_=sums[cb][:, sidx:sidx + 1], reads=[(f"sums{cb}", sidx)], writes=[f"mxc{h}"])
                    if c == 0:
                        st.do("vector", "tensor_tensor", out=mxc[h][:], in0=mxc[h][:], in1=cst["rowvalid"][:], op=ALU.mult,
                              reads=[f"mxc{h}", "c_rowvalid"], writes=[f"mxc{h}"])
                for h in range(4):
                    st.do("gpsimd", "memset", Pc[h][:, ncmp:ncp32], 0.0, reads=[], writes=[f"Pc{h}"])
                    st.do("vector", "tensor_scalar", out=Pc[h][:, 0:ncmp], in0=Sc[h][:, 0:ncmp], scalar1=mxc[h][:, 0:1], scalar2=None,
                          op0=ALU.mult, reads=[f"Sc{h}", f"mxc{h}"], writes=[f"Pc{h}"])
                items = []
                for h in range(4):
                    def extra(kt, nj, nkt, h=h):
                        st.do("tensor", "matmul", pimp[:, 0:NSLC], PTw[h % 2][0:nj, kt, :], ovl[0:nj, kt, :],
                              start=(h == 0 and kt == 0), stop=(h == 3 and kt == nkt - 1), reads=[f"PTw{h % 2}", "ovl"], writes=["pimp"])
                    items.append((Pc[h], f"Pc{h}", PTw[h % 2], f"PTw{h % 2}", ncp32, lambda kt, nj: VC[0:nj, kt, :], "VC",
                                  po[:, h * 64:(h + 1) * 64], "po", extra))
                tail_group(items)
                use_sel = (2 * c + 2) > 16
                if use_sel:
                    st.do("vector", "tensor_copy", out=impS[:], in_=pimp[:, 0:NSLC], reads=["pimp"], writes=["impS"])
                    if 2 * c + 2 < NSLC:
                        st.do("vector", "memset", impS[:, 2 * c + 2:NSLC], -1e30, reads=[], writes=["impS"])
                    lo = 2 * c - 1
                    st.do("vector", "tensor_tensor", out=impS[:, lo:lo + 3], in0=impS[:, lo:lo + 3], in1=cst["selK"][:, 0:3], op=ALU.mult,
                          reads=["impS", "c_selK"], writes=["impS"])
                    st.do("vector", "tensor_tensor", out=impS[:, lo:lo + 3], in0=impS[:, lo:lo + 3], in1=cst["selB"][:, 0:3], op=ALU.add,
                          reads=["impS", "c_selB"], writes=["impS"])
                    st.do("vector", "memset", impS[:, 0:1], 1e4, reads=[], writes=["impS"])
                    st.do("vector", "max", out=m8a[:], in_=impS[:], reads=["impS"], writes=["m8a"])
                    st.do("vector", "match_replace", out=imp2[:], in_to_replace=m8a[:], in_values=impS[:], imm_value=-3e38,
                          reads=["impS", "m8a"], writes=["imp2"])
                    st.do("vector", "max", out=m8b[:], in_=imp2[:], reads=["imp2"], writes=["m8b"])
                    st.do("vector", "tensor_scalar", out=mb[:], in0=impS[:], scalar1=m8b[:, 7:8], scalar2=-NEG, op0=ALU.is_ge, op1=ALU.mult,
                          reads=["impS", "m8b"], writes=["mb"])
                    st.do("vector", "tensor_scalar", out=mb[:], in0=mb[:], scalar1=NEG, scalar2=None, op0=ALU.add,
                          reads=["mb"], writes=["mb"])
                st.do("scalar", "copy", out=ocmp[cb][:], in_=po[:, 0:256], reads=["po"], writes=[f"ocmp{cb}"])
                win_p1()
                slc_p1()
                win_p2()
                slc_p2()

            def Yexp():
                win_p3()
                slc_p3()

            def Yrest():
                win_p4()
                slc_p4()
                sumkeys = [(f"sums{cb}", i) for i in range(12)]
                st.do("vector", "reciprocal", out=rr[cb][:], in_=sums[cb][:], reads=sumkeys, writes=[f"rr{cb}"])
                st.do("vector", "memset", rr[cb][:].rearrange("p (h b) -> p h b", b=3)[:, :, 0], 1.0, reads=[], writes=[f"rr{cb}"])
                st.do("vector", "tensor_tensor", out=rr[cb][:], in0=rr[cb][:], in1=gates[:, c, 12 * g:12 * g + 12], op=ALU.mult,
                      reads=[f"rr{cb}", "gates"], writes=[f"rr{cb}"])
                ob = c % 2
                for h in range(4):
                    hs = slice(h * 64, (h + 1) * 64)
                    st.do("vector", "tensor_scalar", out=acc[:, hs], in0=ocmp[cb][:, hs], scalar1=rr[cb][:, 3 * h:3 * h + 1], scalar2=None,
                          op0=ALU.mult, reads=[f"ocmp{cb}", f"rr{cb}"], writes=[("acc", h)])
                for h in range(4):
                    hs = slice(h * 64, (h + 1) * 64)
                    st.do("vector", "scalar_tensor_tensor", out=acc[:, hs], in0=pT[slc_back["b"]][:, h, 0:64],
                          scalar=rr[cb][:, 3 * h + 1:3 * h + 2], in1=acc[:, hs], op0=ALU.mult, op1=ALU.add,
                          reads=[f"pT{slc_back['b']}", f"rr{cb}", ("acc", h)], writes=[("acc", h)])
                for h in range(4):
                    hs = slice(h * 64, (h + 1) * 64)
                    st.do("vector", "scalar_tensor_tensor", out=oout[ob][:, hs], in0=po2[:, hs],
                          scalar=rr[cb][:, 3 * h + 2:3 * h + 3], in1=acc[:, hs], op0=ALU.mult, op1=ALU.add,
                          reads=["po_win", f"rr{cb}", ("acc", h)], writes=[f"oout{ob}"])
                st.store("sync", scr["ONSA"][cblk, g * 256:(g + 1) * 256], oout[ob][:], f"oout{ob}")

            return X, Yexp, Yrest

        blocks = [make_block(c) for c in range(NT)]
        blocks[0][0]()
        for c in range(NT):
            blocks[c][1]()
            if c + 1 < NT:
                blocks[c + 1][0]()
            blocks[c][2]()

    st.emit()


def stage_D(nc, S, T, C, scr):
    st = Stage(nc, "D")
    NCK = S // 64
    NC4 = S // 512
    ident = st.sb("ident", [128, 128], BF16)
    st.dma("gpsimd", lambda e: e.dma_start(out=ident[:], in_=C["ident"]), "ident", writes=["ident"])
    gmask = st.sb("gmask", [64, 64], F32)
    st.load("sync", gmask[:], C["glamask"], "gmask")
    alT = st.sb("alT", [16, S], BF16)
    st.load("sync", alT[:], scr["ALT"], "alT")
    wa2 = st.sb("wa2", [16, 256], BF16)
    st.dma("gpsimd", lambda e: e.dma_start(out=wa2[:], in_=T["gla_w_a2"][0]), "wa2", writes=["wa2"])
    rmask = st.sb("rmask", [128, S], BF16)
    st.do("vector", "memset", rmask[:], 1.0, reads=[], writes=["rmask"])
    st.do("vector", "memset", rmask[:, 0:S:64], 0.0, reads=[], writes=["rmask"])

    bufA = st.sb("bufA", [128, S], F32)
    bufB = st.sb("bufB", [128, S], F32)
    qT = st.sb("qT", [128, S], BF16)
    kT = st.sb("kT", [128, S], BF16)
    kdec = st.sb("kdec", [128, S], BF16)
    V64 = st.sb("V64", [64, NCK, 256], BF16)
    Sf = [st.sb(f"Sf{i}", [128, 256], F32) for i in range(2)]
    Sb = st.sb("Sb", [128, NCK, 256], BF16)
    Qbd = st.sb("Qbd", [128, NCK, 128], BF16)
    nb = st.sb("nb", [128, 1], F32)
    gb = st.sb("gb", [128, 128], F32)
    tmp = [st.sb(f"tmp{i}", [128, 512], F32) for i in range(2)]
    pkv_ = [st.ps(f"pkv{i}", [128, 512], F32) for i in range(2)]
    pkv = [p[:, 0:256] for p in pkv_]
    pxa = pkv
    pTk_ = [st.ps(f"pTk{i}", [128, 1024], BF16) for i in range(2)]
    pTk = [pTk_[0][0:64, 0:128], pTk_[1][0:64, 0:128]]
    pA_ = [st.ps(f"pA{i}", [128, 512], F32) for i in range(2)]
    pA = [p[0:64, 0:128] for p in pA_]
    pO_ = [st.ps(f"pO{i}", [128, 512], F32) for i in range(2)]
    pO = [p[:, 0:256] for p in pO_]
    kdT = [st.sb(f"kdT{i}", [64, 128], BF16) for i in range(2)]
    ATs = [st.sb(f"ATs{i}", [64, 128], BF16) for i in range(2)]
    R64 = [st.sb(f"R64{i}", [128, 128], BF16) for i in range(2)]
    gg = [st.sb(f"gg{i}", [128, 128], F32) for i in range(2)]
    sqj = [st.sb(f"sqj{i}", [128, 128], F32) for i in range(2)]
    ssq = [st.sb(f"ssq{i}", [128, 1], F32) for i in range(2)]
    og = [st.sb(f"og{i}", [128, 128], BF16) for i in range(2)]

    for hp in range(2):
        rows = slice(hp * 128, (hp + 1) * 128)
        cols = slice(hp * 256, (hp + 1) * 256)
        st.load("sync", qT[:], scr["GQT"][rows, :], "qT")
        st.load("sync", kT[:], scr["GKT"][rows, :], "kT")
        st.load("gpsimd", V64[:], scr["GV"][:, cols].rearrange("(c s) e -> s c e", s=64), "V64")
        st.load("sync", nb[:], T["gla_b_a"][:, rows].rearrange("o p -> p o"), "nb")
        st.do("vector", "tensor_scalar", out=nb[:], in0=nb[:], scalar1=-1.0, scalar2=None, op0=ALU.mult, reads=["nb"], writes=["nb"])
        for tc in range(S // 256):
            i = tc % 2
            tsl = slice(tc * 256, (tc + 1) * 256)
            st.do("tensor", "matmul", pxa[i], wa2[:, rows], alT[:, tsl], start=True, stop=True,
                  reads=["wa2", "alT"], writes=[f"pkv{i}"])
            st.do("scalar", "activation", out=tmp[i][:, 0:256], in_=pxa[i], func=AF.Exp, bias=nb[:, 0:1], scale=-1.0,
                  reads=[f"pkv{i}", "nb"], writes=[f"tmp{i}"])
            st.do("vector", "tensor_scalar", out=tmp[i][:, 0:256], in0=tmp[i][:, 0:256], scalar1=1.0, scalar2=None, op0=ALU.add,
                  reads=[f"tmp{i}"], writes=[f"tmp{i}"])
            st.do("scalar", "activation", out=bufA[:, tsl], in_=tmp[i][:, 0:256], func=AF.Ln, reads=[f"tmp{i}"], writes=["bufA"])
        st.do("vector", "tensor_tensor_scan", out=bufB[:], data0=rmask[:], data1=bufA[:], initial=0.0, op0=ALU.mult, op1=ALU.add,
              reads=["rmask", "bufA"], writes=["bufB"])
        st.do("scalar", "activation", out=bufA[:], in_=bufB[:], func=AF.Exp, scale=-1.0 / 16.0, reads=["bufB"], writes=["bufA"])
        st.do("scalar", "activation", out=bufB[:], in_=bufB[:], func=AF.Exp, scale=1.0 / 16.0, reads=["bufB"], writes=["bufB"])
        st.do("vector", "tensor_tensor", out=qT[:], in0=qT[:], in1=bufA[:], op=ALU.mult, reads=["qT", "bufA"], writes=["qT"])
        st.do("vector", "tensor_tensor", out=kT[:], in0=kT[:], in1=bufB[:], op=ALU.mult, reads=["kT", "bufB"], writes=["kT"])
        dec = bufA[:, 63:63 + 64 * (NCK - 1) + 1:64]
        st.do("vector", "tensor_tensor", out=kdec[:].rearrange("p (c s) -> p c s", s=64),
              in0=kT[:].rearrange("p (c s) -> p c s", s=64), in1=dec.unsqueeze(2).broadcast_to([128, NCK, 64]), op=ALU.mult,
              reads=["kT", "bufA"], writes=["kdec"])
        st.do("gpsimd", "memset", Qbd[:], 0.0, reads=[], writes=["Qbd"])
        for h in range(2):
            hr = slice(h * 64, (h + 1) * 64)
            st.do("gpsimd", "tensor_copy", out=Qbd[hr, :, h * 64:(h + 1) * 64], in_=qT[hr, :].rearrange("p (c s) -> p c s", s=64),
                  reads=["qT"], writes=["Qbd"])
        st.do("vector", "memset", Sf[0][:], 0.0, reads=[], writes=["Sf0"])
        def rec_T(c):
            i = c % 2
            csl = slice(c * 64, (c + 1) * 64)
            st.do("tensor", "transpose", out=pTk[i], in_=kdec[:, csl], identity=ident[:], reads=["kdec", "ident"], writes=[f"pTk{i}"])
            st.do("scalar", "copy", out=kdT[i][:], in_=pTk[i], reads=[f"pTk{i}"], writes=[f"kdT{i}"])

        rec_T(0)
        for c in range(NCK):
            i = c % 2
            if c + 1 < NCK:
                rec_T(c + 1)
            st.do("tensor", "matmul", pkv[i], kdT[i][:], V64[:, c, :], start=True, stop=True,
                  reads=[f"kdT{i}", "V64"], writes=[f"pkv{i}"])
            st.do("gpsimd", "tensor_copy", out=Sb[:, c, :], in_=Sf[i][:], reads=[f"Sf{i}"], writes=[("Sb", c)])
            st.do("vector", "scalar_tensor_tensor", out=Sf[1 - i][:], in0=Sf[i][:], scalar=dec[:, c:c + 1],
                  in1=pkv[i], op0=ALU.mult, op1=ALU.add, reads=[f"Sf{i}", "bufA", f"pkv{i}"], writes=[f"Sf{1 - i}"])
        if DBG_D == 2:
            continue
        for h in range(2):
            st.load("sync", gb[h * 64:(h + 1) * 64, :], bcast_rows(T["gla_norm_g"][:, hp * 256 + h * 128:hp * 256 + (h + 1) * 128], 64), "gb")
        def out_A(c):
            i = c % 2
            csl = slice(c * 64, (c + 1) * 64)
            st.do("tensor", "matmul", pA[i], kT[:, csl], Qbd[:, c, :], start=True, stop=True, reads=["kT", "Qbd"], writes=[f"pA{i}"])
            st.do("vector", "tensor_tensor", out=ATs[i][:].rearrange("p (h t) -> p h t", h=2), in0=pA[i].rearrange("p (h t) -> p h t", h=2),
                  in1=gmask[:].unsqueeze(1).broadcast_to([64, 2, 64]), op=ALU.mult, reads=[f"pA{i}", "gmask"], writes=[f"ATs{i}"])

        for c in range(NCK):
            i = c % 2
            csl = slice(c * 64, (c + 1) * 64)
            for h in range(2):
                st.load("sync", R64[i][h * 64:(h + 1) * 64, :], scr["GR"][csl, hp * 256 + h * 128:hp * 256 + (h + 1) * 128], f"R64{i}")
            st.do("gpsimd", "tensor_tensor", out=gg[i][:], in0=R64[i][:], in1=gb[:], op=ALU.mult, reads=[f"R64{i}", "gb"], writes=[f"gg{i}"])
            if c == 0:
                out_A(0)
            if c + 1 < NCK:
                out_A(c + 1)
            st.do("tensor", "matmul", pO[i], Qbd[:, c, :], Sb[:, c, :], start=True, stop=False, reads=["Qbd", ("Sb", c)], writes=[f"pO{i}"])
            st.do("tensor", "matmul", pO[i], ATs[i][:], V64[:, c, :], start=False, stop=True, reads=[f"ATs{i}", "V64"], writes=[f"pO{i}"])
            for h in range(2):
                hr = slice(h * 64, (h + 1) * 64)
                st.do("scalar", "activation", out=sqj[i][hr, :], in_=pO[i][hr, h * 128:(h + 1) * 128], func=AF.Square,
                      accum_out=ssq[i][hr, 0:1], reads=[f"pO{i}"], writes=[(f"sqj{i}", h), (f"ssq{i}", h)])
            st.do("vector", "tensor_scalar", out=ssq[i][:], in0=ssq[i][:], scalar1=1.0 / 128.0, scalar2=1e-6, op0=ALU.mult, op1=ALU.add,
                  reads=[(f"ssq{i}", 0), (f"ssq{i}", 1)], writes=[f"rs{i}"])
            st.do("scalar", "activation", out=ssq[i][:], in_=ssq[i][:], func=AF.Sqrt, reads=[f"rs{i}"], writes=[f"rs{i}"])
            st.do("vector", "reciprocal", out=ssq[i][:], in_=ssq[i][:], reads=[f"rs{i}"], writes=[f"rs{i}", (f"ssq{i}", 0), (f"ssq{i}", 1)])
            for h in range(2):
                hr = slice(h * 64, (h + 1) * 64)
                st.do("vector", "scalar_tensor_tensor", out=og[i][hr, :], in0=pO[i][hr, h * 128:(h + 1) * 128],
                      scalar=ssq[i][hr, 0:1], in1=gg[i][hr, :], op0=ALU.mult, op1=ALU.mult,
                      reads=[f"pO{i}", f"rs{i}", (f"ssq{i}", 0), (f"ssq{i}", 1), f"gg{i}"], writes=[f"og{i}"])
            for h in range(2):
                st.store("sync", scr["OGLA"][csl, hp * 256 + h * 128:hp * 256 + (h + 1) * 128], og[i][h * 64:(h + 1) * 64, :], f"og{i}")
    st.emit()


def rms_rstd(st, src_ap, src_key, sq, ss, rs, i, dim):
    st.do("scalar", "activation", out=sq[i][:], in_=src_ap, func=AF.Square, accum_out=ss[i][:],
          reads=[src_key], writes=[f"sq{i}", f"ss{i}"])
    st.do("vector", "tensor_scalar", out=rs[i][:], in0=ss[i][:], scalar1=1.0 / dim, scalar2=1e-6, op0=ALU.mult, op1=ALU.add,
          reads=[f"ss{i}"], writes=[f"rs{i}"])
    st.do("scalar", "activation", out=rs[i][:], in_=rs[i][:], func=AF.Sqrt, reads=[f"rs{i}"], writes=[f"rs{i}"])
    st.do("vector", "reciprocal", out=rs[i][:], in_=rs[i][:], reads=[f"rs{i}"], writes=[f"rs{i}"])


def stage_E0(nc, S, T, C, scr):
    st = Stage(nc, "E0")
    TH = S // 2
    NTH = TH // 128
    ident = st.sb("ident", [128, 128], BF16)
    st.dma("gpsimd", lambda e: e.dma_start(out=ident[:], in_=C["ident"]), "ident", writes=["ident"])
    gm = st.sb("gm", [128, 8], F32)
    st.load("sync", gm[:], T["g_mix"].rearrange("o (kt p) -> p (o kt)", p=128), "gm")
    Wm = st.sb("Wm", [128, 8, 2048], BF16)
    wst = [st.sb(f"wst{i}", [128, 2048], F32) for i in range(2)]
    for kt in range(8):
        i = kt % 2
        st.load("sync" if i == 0 else "gpsimd", wst[i][:], T["w_in"][0, kt * 128:(kt + 1) * 128, 2856:4904], f"wst{i}")
        st.do("vector" if i == 0 else "gpsimd", "tensor_scalar", out=Wm[:, kt, :], in0=wst[i][:], scalar1=gm[:, kt:kt + 1], scalar2=None,
              op0=ALU.mult, reads=[f"wst{i}", "gm"], writes=[("Wm", kt)])
    xt = [st.sb(f"xt{i}", [128, D], F32) for i in range(2)]
    sq = [st.sb(f"sq{i}", [128, D], BF16) for i in range(2)]
    xn = [st.sb(f"xn{i}", [128, D], BF16) for i in range(2)]
    ss = [st.sb(f"ss{i}", [128, 1], F32) for i in range(2)]
    rs = [st.sb(f"rs{i}", [128, 1], F32) for i in range(2)]
    uTt = [st.sb(f"uTt{i}", [128, 8, 128], BF16) for i in range(2)]
    sg = [st.sb(f"sg{i}", [128, 2048], BF16) for i in range(2)]
    ptr = [st.ps(f"ptr{i}", [128, 8, 128], BF16) for i in range(2)]
    pm = [st.ps(f"pm{i}", [128, 512], F32) for i in range(4)]
    for t in range(NTH):
        i = t % 2
        st.load("sync", xt[i][:], T["xh"][t * 128:(t + 1) * 128, :], f"xt{i}")
        rms_rstd(st, xt[i][:], f"xt{i}", sq, ss, rs, i, D)
        st.do("vector", "tensor_scalar", out=xn[i][:], in0=xt[i][:], scalar1=rs[i][:, 0:1], scalar2=None, op0=ALU.mult,
              reads=[f"xt{i}", f"rs{i}"], writes=[f"xn{i}"])
        for kt in range(8):
            st.do("tensor", "transpose", out=ptr[i][:, kt, :], in_=xn[i][:, kt * 128:(kt + 1) * 128], identity=ident[:],
                  reads=[f"xn{i}", "ident"], writes=[f"ptr{i}"])
        st.do("scalar", "copy", out=uTt[i][:], in_=ptr[i][:], reads=[f"ptr{i}"], writes=[f"uTt{i}"])
        for cc in range(4):
            for kt in range(8):
                st.do("tensor", "matmul", pm[cc][:], uTt[i][:, kt, :], Wm[:, kt, cc * 512:(cc + 1) * 512], start=(kt == 0), stop=(kt == 7),
                      reads=[f"uTt{i}", ("Wm", kt)], writes=[f"pm{cc}"])
            st.do("scalar", "activation", out=sg[i][:, cc * 512:(cc + 1) * 512], in_=pm[cc][:], func=AF.Sigmoid,
                  reads=[f"pm{cc}"], writes=[f"sg{i}"])
        st.store("sync", scr["SIGM"][t * 128:(t + 1) * 128, :], sg[i][:], f"sg{i}")
    st.emit()


def stage_E1(nc, S, T, C, scr, P, ncores):
    st = Stage(nc, "E1")
    TH = S // 2
    NTH = TH // 128
    h1, vT, combT = P["h1"], P["vT"], P["combT"]
    identb = st.sb("identb", [128, 128], BF16)
    st.dma("gpsimd", lambda e: e.dma_start(out=identb[:], in_=C["ident"]), "identb", writes=["identb"])
    Wpn = st.sb("Wpn", [128, 4, D], BF16)
    Wpg = st.sb("Wpg", [128, 4, D], BF16)
    Wo = st.sb("Wo", [128, 8, D], BF16)
    st.dma("gpsimd", lambda e: e.dma_start(out=Wpn[:], in_=T["w_proj_nsa"][0].rearrange("(j p) d -> p j d", p=128)), "Wpn", writes=["Wpn"])
    st.dma("gpsimd", lambda e: e.dma_start(out=Wpg[:], in_=T["w_proj_gla"][0].rearrange("(j p) d -> p j d", p=128)), "Wpg", writes=["Wpg"])
    st.dma("gpsimd", lambda e: e.dma_start(out=Wo[:], in_=T["w_out"][0].rearrange("(j p) d -> p j d", p=128)), "Wo", writes=["Wo"])
    Wr = st.sb("Wr", [128, 8, 36], BF16)
    st.dma("gpsimd", lambda e: e.dma_start(out=Wr[:, :, 0:4], in_=T["w_grp"][0].rearrange("(j p) g -> p j g", p=128)), "Wr", writes=["Wr"])
    st.dma("gpsimd", lambda e: e.dma_start(out=Wr[:, :, 4:36], in_=T["w_exp"][0].rearrange("(j p) g -> p j g", p=128)), "Wr", writes=["Wr"])
    br = st.sb("br", [128, 36], F32)
    st.load("sync", br[:, 0:4], bcast_rows(T["b_grp"], 128), "br")
    st.load("sync", br[:, 4:36], bcast_rows(T["b_exp"], 128), "br")
    gfb = st.sb("gfb", [128, D], F32)
    st.load("sync", gfb[:], bcast_rows(T["g_ffn"], 128), "gfb")

    xt = [st.sb(f"xt{i}", [128, D], F32) for i in range(2)]
    on = [st.sb(f"on{i}", [128, 512], BF16) for i in range(2)]
    og = [st.sb(f"og{i}", [128, 512], BF16) for i in range(2)]
    sgm = [st.sb(f"sgm{i}", [128, 2048], BF16) for i in range(2)]
    onT = [st.sb(f"onT{i}", [128, 8, 128], BF16) for i in range(2)]
    m1 = st.sb("m1", [128, D], F32)
    m2 = st.sb("m2", [128, D], F32)
    mx_ = st.sb("mixed", [128, D], BF16)
    mT = st.sb("mT", [128, 8, 128], BF16)
    sq = [st.sb(f"sq{i}", [128, D], BF16) for i in range(2)]
    ss = [st.sb(f"ss{i}", [128, 1], F32) for i in range(2)]
    rs = [st.sb(f"rs{i}", [128, 1], F32) for i in range(2)]
    vnb = st.sb("vnb", [128, D], BF16)
    combb = st.sb("combb", [128, 32], BF16)
    lg = st.sb("lg", [128, 36], F32)
    sm = st.sb("sm", [128, 8], F32)
    bg = st.sb("bg", [128, 4], F32)
    lem = st.sb("lem", [128, 32], F32)
    top8 = st.sb("top8", [128, 8], F32)
    msk = st.sb("msk", [128, 32], F32)
    ex = st.sb("ex", [128, 32], F32)
    comb = st.sb("comb", [128, 32], F32)
    eg = st.sb("eg", [128, 4], F32)

    pT = [st.ps(f"pT{i}", [128, 8, 128], BF16) for i in range(2)]
    py = [st.ps(f"py{i}", [128, 512], F32) for i in range(4)]

    for t in range(NTH):
        i = t % 2
        rows = slice(t * 128, (t + 1) * 128)
        st.load("sync", xt[i][:], T["xh"][rows, :], f"xt{i}")
        st.load("sync", sgm[i][:], scr["SIGM"][rows, :], f"sgm{i}")
        st.dma_percore(ncores, lambda e, k, i=i, t=t: e.dma_start(out=on[i][:], in_=scr["ONSA"][(k % 2) * TH + t * 128:(k % 2) * TH + (t + 1) * 128, :]),
                       f"on{i}", writes=[f"on{i}"])
        st.dma_percore(ncores, lambda e, k, i=i, t=t: e.dma_start(out=og[i][:], in_=scr["OGLA"][(k % 2) * TH + t * 128:(k % 2) * TH + (t + 1) * 128, :]),
                       f"og{i}", writes=[f"og{i}"])
        for j in range(4):
            st.do("tensor", "transpose", out=pT[0][:, j, :], in_=on[i][:, j * 128:(j + 1) * 128], identity=identb[:],
                  reads=[f"on{i}", "identb"], writes=["pT0"])
        for j in range(4):
            st.do("tensor", "transpose", out=pT[0][:, 4 + j, :], in_=og[i][:, j * 128:(j + 1) * 128], identity=identb[:],
                  reads=[f"og{i}", "identb"], writes=["pT0"])
        st.do("scalar", "copy", out=onT[i][:], in_=pT[0][:], reads=["pT0"], writes=[f"onT{i}"])
        for hf in range(2):
            cs_ = slice(hf * 512, (hf + 1) * 512)
            for j in range(4):
                st.do("tensor", "matmul", py[hf][:], onT[i][:, j, :], Wpn[:, j, cs_], start=(j == 0), stop=(j == 3),
                      reads=[f"onT{i}", "Wpn"], writes=[f"py{hf}"])
            for j in range(4):
                st.do("tensor", "matmul", py[2 + hf][:], onT[i][:, 4 + j, :], Wpg[:, j, cs_], start=(j == 0), stop=(j == 3),
                      reads=[f"onT{i}", "Wpg"], writes=[f"py{2 + hf}"])
            st.do("vector", "tensor_tensor", out=m1[:, cs_], in0=py[hf][:], in1=sgm[i][:, hf * 512:(hf + 1) * 512], op=ALU.mult,
                  reads=[f"py{hf}", f"sgm{i}"], writes=[("m1", hf)])
            st.do("vector", "tensor_tensor", out=m2[:, cs_], in0=py[2 + hf][:], in1=sgm[i][:, 1024 + hf * 512:1024 + (hf + 1) * 512], op=ALU.mult,
                  reads=[f"py{2 + hf}", f"sgm{i}"], writes=[("m2", hf)])
            st.do("gpsimd", "tensor_tensor", out=mx_[:, cs_], in0=m1[:, cs_], in1=m2[:, cs_], op=ALU.add,
                  reads=[("m1", hf), ("m2", hf)], writes=[("mixed", hf)])
        for kt in range(8):
            st.do("tensor", "transpose", out=pT[1][:, kt, :], in_=mx_[:, kt * 128:(kt + 1) * 128], identity=identb[:],
                  reads=[("mixed", kt // 4), "identb"], writes=["pT1"])
        st.do("scalar", "copy", out=mT[:], in_=pT[1][:], reads=["pT1"], writes=["mT"])
        for hf in range(2):
            cs_ = slice(hf * 512, (hf + 1) * 512)
            for kt in range(8):
                st.do("tensor", "matmul", py[hf][:], mT[:, kt, :], Wo[:, kt, cs_], start=(kt == 0), stop=(kt == 7),
                      reads=["mT", "Wo"], writes=[f"py{hf}"])
            st.do("vector", "tensor_tensor", out=h1[:, t, cs_], in0=py[hf][:], in1=xt[i][:, cs_], op=ALU.add,
                  reads=[f"py{hf}", f"xt{i}"], writes=[("h1", t, hf)])
        st.do("scalar", "activation", out=sq[i][:], in_=h1[:, t, :], func=AF.Square, accum_out=ss[i][:],
              reads=[("h1", t, 0), ("h1", t, 1)], writes=[f"sq{i}", f"ss{i}"])
        st.do("vector", "tensor_scalar", out=rs[i][:], in0=ss[i][:], scalar1=1.0 / D, scalar2=1e-6, op0=ALU.mult, op1=ALU.add,
              reads=[f"ss{i}"], writes=[f"rs{i}"])
        st.do("scalar", "activation", out=rs[i][:], in_=rs[i][:], func=AF.Sqrt, reads=[f"rs{i}"], writes=[f"rs{i}"])
        st.do("vector", "reciprocal", out=rs[i][:], in_=rs[i][:], reads=[f"rs{i}"], writes=[f"rs{i}"])
        st.do("vector", "scalar_tensor_tensor", out=vnb[:], in0=h1[:, t, :], scalar=rs[i][:, 0:1], in1=gfb[:], op0=ALU.mult, op1=ALU.mult,
              reads=[("h1", t, 0), ("h1", t, 1), f"rs{i}", "gfb"], writes=["vn"])
        for kt in range(8):
            st.do("tensor", "transpose", out=pT[0][:, kt, :], in_=vnb[:, kt * 128:(kt + 1) * 128], identity=identb[:],
                  reads=["vn", "identb"], writes=["pT0"])
        st.do("scalar", "copy", out=vT[:, :, rows], in_=pT[0][:], reads=["pT0"], writes=[("vT", t, 0), ("vT", t, 1)])
        for kt in range(8):
            st.do("tensor", "matmul", py[2][:, 0:36], vT[:, kt, rows], Wr[:, kt, :], start=(kt == 0), stop=(kt == 7),
                  reads=[("vT", t, 0), ("vT", t, 1), "Wr"], writes=["py2"])
        st.do("vector", "tensor_tensor", out=lg[:], in0=py[2][:, 0:36], in1=br[:], op=ALU.add, reads=["py2", "br"], writes=["lg"])
        st.do("vector", "reduce_max", out=sm[:, 0:1], in_=lg[:, 0:4], axis=AX.X, reads=["lg"], writes=["sm0"])
        st.do("vector", "tensor_scalar", out=bg[:], in0=lg[:, 0:4], scalar1=sm[:, 0:1], scalar2=-NEG, op0=ALU.is_ge, op1=ALU.mult,
              reads=["lg", "sm0"], writes=["bg"])
        st.do("vector", "tensor_scalar", out=bg[:], in0=bg[:], scalar1=NEG, scalar2=None, op0=ALU.add, reads=["bg"], writes=["bg"])
        st.do("vector", "tensor_scalar", out=sm[:, 1:2], in0=sm[:, 0:1], scalar1=-1.0, scalar2=None, op0=ALU.mult, reads=["sm0"], writes=["sm1"])
        st.do("scalar", "activation", out=eg[:], in_=lg[:, 0:4], func=AF.Exp, bias=sm[:, 1:2], accum_out=sm[:, 2:3],
              reads=["lg", "sm1"], writes=["eg", "sm2"])
        st.do("vector", "tensor_tensor", out=lem[:].rearrange("p (g e) -> p g e", e=8), in0=lg[:, 4:36].rearrange("p (g e) -> p g e", e=8),
              in1=bg[:].unsqueeze(2).broadcast_to([128, 4, 8]), op=ALU.add, reads=["lg", "bg"], writes=["lem"])
        st.do("vector", "max", out=top8[:], in_=lem[:], reads=["lem"], writes=["top8"])
        st.do("vector", "tensor_scalar", out=msk[:], in0=lem[:], scalar1=top8[:, 1:2], scalar2=None, op0=ALU.is_ge, reads=["lem", "top8"], writes=["msk"])
        st.do("vector", "tensor_scalar", out=sm[:, 3:4], in0=top8[:, 0:1], scalar1=-1.0, scalar2=None, op0=ALU.mult, reads=["top8"], writes=["sm3"])
        st.do("scalar", "activation", out=ex[:], in_=lem[:], func=AF.Exp, bias=sm[:, 3:4], reads=["lem", "sm3"], writes=["ex"])
        st.do("vector", "tensor_tensor", out=ex[:], in0=ex[:], in1=msk[:], op=ALU.mult, reads=["ex", "msk"], writes=["ex"])
        st.do("vector", "reduce_sum", out=sm[:, 4:5], in_=ex[:], axis=AX.X, reads=["ex"], writes=["sm4"])
        st.do("vector", "tensor_tensor", out=sm[:, 5:6], in0=sm[:, 4:5], in1=sm[:, 2:3], op=ALU.mult, reads=["sm4", "sm2"], writes=["sm5"])
        st.do("vector", "reciprocal", out=sm[:, 5:6], in_=sm[:, 5:6], reads=["sm5"], writes=["sm5"])
        st.do("vector", "tensor_scalar", out=comb[:], in0=ex[:], scalar1=sm[:, 5:6], scalar2=None, op0=ALU.mult, reads=["ex", "sm5"], writes=["comb"])
        st.do("vector", "tensor_copy", out=combb[:], in_=comb[:], reads=["comb"], writes=["combb"])
        st.do("tensor", "transpose", out=pT[1][0:32, 0, :], in_=combb[:], identity=identb[:], reads=["combb", "identb"], writes=["pT1"])
        st.do("scalar", "copy", out=combT[:, rows], in_=pT[1][0:32, 0, :], reads=["pT1"], writes=[("combT", t)])
    st.emit()


def stage_E2(nc, S, T, C, scr, P, out):
    st = Stage(nc, "E2")
    TH = S // 2
    NTH = TH // 128
    CH = min(512, TH)
    NCH = TH // CH
    TPC = CH // 128
    h1, vT, combT = P["h1"], P["vT"], P["combT"]
    identb = st.sb("identb", [128, 128], BF16)
    st.dma("gpsimd", lambda e: e.dma_start(out=identb[:], in_=C["ident"]), "identb", writes=["identb"])
    selall = st.sb("selall", [32, 32, 128], BF16)
    st.do("vector", "tensor_copy", out=selall[:], in_=identb[0:32, 0:32].unsqueeze(2).broadcast_to([32, 32, 128]),
          reads=["identb"], writes=["selall"])
    Wg = [st.sb(f"Wg{i}", [128, 8, 512], BF16) for i in range(2)]
    Wu = [st.sb(f"Wu{i}", [128, 8, 512], BF16) for i in range(2)]
    Wd = [st.sb(f"Wd{i}", [128, 4, D], BF16) for i in range(2)]
    cB = [st.sb(f"cB{i}", [128, CH], BF16) for i in range(2)]
    sgl = [st.sb(f"sgl{i}", [128, CH], BF16) for i in range(2)]
    tu = [st.sb(f"tu{i}", [128, CH], BF16) for i in range(2)]
    hT = [st.sb(f"hT{i}", [128, 4, CH], BF16) for i in range(2)]
    pcb = st.ps("pcb", [128, 512], F32)
    pG = [st.ps(f"pG{i}", [128, 512], F32) for i in range(2)]
    pU = [st.ps(f"pU{i}", [128, 512], F32) for i in range(2)]
    py = [st.ps(f"py{i}", [128, 512], F32) for i in range(2)]
    gu = 0
    hb = 0
    def load_expert(ex_):
        w = ex_ % 2
        st.dma("gpsimd", lambda e, w=w, ex_=ex_: e.dma_start(out=Wg[w][:], in_=T["w_gate"][0, ex_].rearrange("(kt p) f -> p kt f", p=128)),
               f"Wg{w}", writes=[f"Wg{w}"])
        st.dma("gpsimd", lambda e, w=w, ex_=ex_: e.dma_start(out=Wu[w][:], in_=T["w_up"][0, ex_].rearrange("(kt p) f -> p kt f", p=128)),
               f"Wu{w}", writes=[f"Wu{w}"])
        st.dma("gpsimd", lambda e, w=w, ex_=ex_: e.dma_start(out=Wd[w][:], in_=T["w_down"][0, ex_].rearrange("(fc p) d -> p fc d", p=128)),
               f"Wd{w}", writes=[f"Wd{w}"])

    load_expert(0)
    for ex_ in range(32):
        w = ex_ % 2
        if ex_ + 1 < 32:
            load_expert(ex_ + 1)
        for tc in range(NCH):
            tsl = slice(tc * CH, (tc + 1) * CH)
            cbi = hb % 2
            hb += 1
            ckeys = [("combT", tc * TPC + j) for j in range(TPC)]
            vkeys = [("vT", tc * TPC + j, q) for j in range(TPC) for q in range(2)]
            st.do("tensor", "matmul", pcb[:, 0:CH], selall[:, ex_, :], combT[:, tsl], start=True, stop=True,
                  reads=["selall"] + ckeys, writes=["pcb"])
            st.do("scalar", "copy", out=cB[cbi][:], in_=pcb[:, 0:CH], reads=["pcb"], writes=[f"cB{cbi}"])
            for fc in range(4):
                g = gu % 2
                gu += 1
                fsl = slice(fc * 128, (fc + 1) * 128)
                for kt in range(8):
                    st.do("tensor", "matmul", pG[g][:, 0:CH], Wg[w][:, kt, fsl], vT[:, kt, tsl], start=(kt == 0), stop=(kt == 7),
                          reads=[f"Wg{w}"] + vkeys, writes=[f"pG{g}"])
                for kt in range(8):
                    st.do("tensor", "matmul", pU[g][:, 0:CH], Wu[w][:, kt, fsl], vT[:, kt, tsl], start=(kt == 0), stop=(kt == 7),
                          reads=[f"Wu{w}"] + vkeys, writes=[f"pU{g}"])
                st.do("scalar", "activation", out=sgl[g][:], in_=pG[g][:, 0:CH], func=AF.Silu, reads=[f"pG{g}"], writes=[f"sgl{g}"])
                st.do("vector", "tensor_tensor", out=tu[g][:], in0=pU[g][:, 0:CH], in1=cB[cbi][:], op=ALU.mult,
                      reads=[f"pU{g}", f"cB{cbi}"], writes=[f"tu{g}"])
                st.do("gpsimd", "tensor_tensor", out=hT[cbi][:, fc, :], in0=sgl[g][:], in1=tu[g][:], op=ALU.mult,
                      reads=[f"sgl{g}", f"tu{g}"], writes=[(f"hT{cbi}", fc)])
            for tt in range(TPC):
                t = tc * TPC + tt
                for hf in range(2):
                    cs_ = slice(hf * 512, (hf + 1) * 512)
                    for fc in range(4):
                        st.do("tensor", "matmul", py[hf][:], hT[cbi][:, fc, tt * 128:(tt + 1) * 128], Wd[w][:, fc, cs_],
                              start=(fc == 0), stop=(fc == 3), reads=[(f"hT{cbi}", f) for f in range(4)] + [f"Wd{w}"], writes=[f"py{hf}"])
                    st.do("vector", "tensor_tensor", out=h1[:, t, cs_], in0=py[hf][:], in1=h1[:, t, cs_], op=ALU.add,
                          reads=[f"py{hf}", ("h1", t, hf)], writes=[("h1", t, hf)])
    gfin = st.sb("gfin", [128, D], F32)
    st.load("sync", gfin[:], bcast_rows(T["g_final"], 128), "gfin")
    sq = [st.sb(f"sq{i}", [128, D], BF16) for i in range(2)]
    ss = [st.sb(f"ss{i}", [128, 1], F32) for i in range(2)]
    rs = [st.sb(f"rs{i}", [128, 1], F32) for i in range(2)]
    ot = [st.sb(f"ot{i}", [128, D], F32) for i in range(2)]
    for t in range(NTH):
        i = t % 2
        st.do("scalar", "activation", out=sq[i][:], in_=h1[:, t, :], func=AF.Square, accum_out=ss[i][:],
              reads=[("h1", t, 0), ("h1", t, 1)], writes=[f"sq{i}", f"ss{i}"])
        st.do("vector", "tensor_scalar", out=rs[i][:], in0=ss[i][:], scalar1=1.0 / D, scalar2=1e-6, op0=ALU.mult, op1=ALU.add,
              reads=[f"ss{i}"], writes=[f"rs{i}"])
        st.do("scalar", "activation", out=rs[i][:], in_=rs[i][:], func=AF.Sqrt, reads=[f"rs{i}"], writes=[f"rs{i}"])
        st.do("vector", "reciprocal", out=rs[i][:], in_=rs[i][:], reads=[f"rs{i}"], writes=[f"rs{i}"])
        st.do("vector", "scalar_tensor_tensor", out=ot[i][:], in0=h1[:, t, :], scalar=rs[i][:, 0:1], in1=gfin[:], op0=ALU.mult, op1=ALU.mult,
              reads=[("h1", t, 0), ("h1", t, 1), f"rs{i}", "gfin"], writes=[f"ot{i}"])
        st.store("sync", out[t * 128:(t + 1) * 128, :], ot[i][:], f"ot{i}")
    st.emit()


def build_full(S, ncores, stages="RABCDE", debug=(), scr_in=()):
    nc = bass.Bass("TRN2", target_bir_lowering=False)
    T = {k: nc.dram_tensor(k, shp, dt, kind="ExternalInput").ap() for k, (shp, dt) in IN_SHAPES(S).items()}
    C = {k: nc.dram_tensor("c_" + k, shp, F32, kind="ExternalInput").ap() for k, shp in CONST_SHAPES.items()}
    scr = {}
    spec = dict(SCR_SPEC(S))
    spec["SIGM"] = ([S // 2, 2048], BF16)
    for k, (shp, dt) in spec.items():
        kind = "ExternalOutput" if k in debug else ("ExternalInput" if k in scr_in else "Internal")
        scr[k] = nc.dram_tensor("scr_" + k, shp, dt, kind=kind).ap()
    out = nc.dram_tensor("out", [S // 2, D], F32, kind="ExternalOutput").ap()
    TH = S // 2
    with nc.allow_non_contiguous_dma(reason="small strided parameter loads"), ExitStack() as es:
        if "R" in stages:
            stage_R(nc, S, T, C, scr)
        if "A" in stages:
            stage_A(nc, S, T, C, scr)
        if "B" in stages:
            stage_B(nc, S, T, C, scr)
        if "C" in stages:
            stage_C(nc, S, T, C, scr)
        if "D" in stages:
            stage_D(nc, S, T, C, scr)
        if "E" in stages:
            if "0" in DBG_E:
                stage_E0(nc, S, T, C, scr)
            P = {"h1": es.enter_context(nc.sbuf_tensor("P_h1", [128, TH // 128, D], F32)),
                 "vT": es.enter_context(nc.sbuf_tensor("P_vT", [128, 8, TH], BF16)),
                 "combT": es.enter_context(nc.sbuf_tensor("P_combT", [32, TH], BF16))}
            if "1" in DBG_E:
                stage_E1(nc, S, T, C, scr, P, ncores)
            if "2" in DBG_E:
                stage_E2(nc, S, T, C, scr, P, out)
    return nc


SEQ = 4096
BATCH = 4
_CACHE = {}


def kernel(**inputs):
    S = SEQ
    ncores = 8
    if "nc" not in _CACHE:
        _CACHE["nc"] = build_full(S, ncores)
    nc = _CACHE["nc"]
    consts = host_consts(S)
    shapes = IN_SHAPES(S)
    x = np.ascontiguousarray(np.asarray(inputs["x"], dtype=np.float32))
    pos = np.ascontiguousarray(np.asarray(inputs["positions"]).astype(np.int32))
    in_maps = []
    for core in range(ncores):
        b, half = core // 2, core % 2
        m = {}
        for k, (shp, dt) in shapes.items():
            if k == "x":
                m[k] = x[b]
            elif k == "xh":
                m[k] = np.ascontiguousarray(x[b, half * S // 2:(half + 1) * S // 2])
            elif k == "positions":
                m[k] = pos[b:b + 1]
            elif k == "halfidx":
                m[k] = np.array([[half]], np.int32)
            elif k == "g_final":
                m[k] = np.asarray(inputs[k], dtype=np.float32).reshape(1, -1)
            else:
                m[k] = np.ascontiguousarray(np.asarray(inputs[k], dtype=np.float32))
        for k, v in consts.items():
            m["c_" + k] = v
        in_maps.append(m)
    res = run_bass_kernel_spmd(nc, in_maps, core_ids=list(range(ncores)))
    out = np.empty((BATCH, S, D), np.float32)
    for core in range(ncores):
        b, half = core // 2, core % 2
        out[b, half * S // 2:(half + 1) * S // 2] = res.results[core]["out"]
    return out
```

```python
import math
import numpy as np
from contextlib import ExitStack
import concourse.bass as bass
import concourse.mybir as mybir
from concourse.bass_utils import run_bass_kernel_spmd

F32 = mybir.dt.float32
BF16 = mybir.dt.bfloat16
I32 = mybir.dt.int32
AF = mybir.ActivationFunctionType
ALU = mybir.AluOpType
AX = mybir.AxisListType

SAME_ENGINE_SYNC = True
EPOCH = 12000
NEG = -30000.0
DBG_BR = None
DBG_D = 3
DBG_NOIF = False
DBG_E = '012'
D = 1024


class Stage:
    ENGS = ("tensor", "vector", "scalar", "gpsimd", "sync")

    def __init__(self, nc, name):
        self.nc = nc
        self.name = name
        self.es = ExitStack()
        self.ops = {e: [] for e in self.ENGS}
        self.last_write = {}
        self.readers = {}
        self.seen = {e: {} for e in self.ENGS}
        self.sems = {}
        self.cnt = {}
        self.epoch = {e: 0 for e in self.ENGS}
        self.dma_sems_by_eng = {e: set() for e in self.ENGS}
        self.nalloc = 0

    def sb(self, name, shape, dtype):
        return self.es.enter_context(self.nc.sbuf_tensor(f"{self.name}_{name}", list(shape), dtype))

    def ps(self, name, shape, dtype=F32):
        return self.es.enter_context(self.nc.psum_tensor(f"{self.name}_{name}", list(shape), dtype))

    def _sem(self, semkey):
        if semkey not in self.sems:
            self.nalloc += 1
            self.sems[semkey] = self.nc.alloc_semaphore(name=f"{self.name}_s{self.nalloc}")
            self.cnt[semkey] = 0
        return self.sems[semkey]

    def _deps(self, eng, reads, writes):
        need = {}

        def want(c):
            if c is None:
                return
            sk, v = c
            if need.get(sk, 0) < v:
                need[sk] = v

        for r in reads:
            want(self.last_write.get(r))
        for w in writes:
            want(self.last_write.get(w))
            for c in self.readers.get(w, ()):
                want(c)
        waits = []
        for sk, v in need.items():
            if (not SAME_ENGINE_SYNC or eng == "tensor") and sk[0] == "E" and sk[1] == eng:
                continue
            if self.seen[eng].get(sk, 0) >= v:
                continue
            self.seen[eng][sk] = v
            waits.append((sk, v))
        return waits

    def _commit(self, comp, reads, writes):
        for w in writes:
            self.last_write[w] = comp
            self.readers[w] = []
        for r in reads:
            if r in writes:
                continue
            self.readers.setdefault(r, []).append(comp)

    def op(self, eng, fn, reads=(), writes=()):
        reads = list(reads)
        writes = list(writes)
        waits = self._deps(eng, reads, writes)
        sk = ("E", eng, self.epoch[eng])
        self._sem(sk)
        self.cnt[sk] += 1
        comp = (sk, self.cnt[sk])
        if self.cnt[sk] >= EPOCH:
            self.epoch[eng] += 1
        self.ops[eng].append((fn, waits, sk, 1))
        self._commit(comp, reads, writes)
        return comp

    def dma(self, eng, fn, semkey, reads=(), writes=()):
        reads = list(reads)
        writes = list(writes)
        waits = self._deps(eng, reads, writes)
        sk = ("D", semkey)
        self._sem(sk)
        self.cnt[sk] += 16
        comp = (sk, self.cnt[sk])
        self.ops[eng].append((fn, waits, sk, 16))
        self.dma_sems_by_eng[eng].add(sk)
        self._commit(comp, reads, writes)
        return comp

    def do(self, eng, method, *args, reads=(), writes=(), **kw):
        return self.op(eng, lambda e: getattr(e, method)(*args, **kw), reads=reads, writes=writes)

    def dma_percore(self, ncores, mk, semkey, reads=(), writes=()):
        if DBG_NOIF:
            return self.dma("gpsimd", lambda e: mk(e, 0), semkey, reads=reads, writes=writes)
        sk = ("D", semkey)
        self._sem(sk)

        class _Done:
            def then_inc(self_, *a):
                return self_

        def fn(e):
            if getattr(self, "_pid", None) is None:
                self._pid = e.partition_id()
            pid = self._pid
            for k in range(ncores):
                with e.If(pid == k):
                    mk(e, k).then_inc(self.sems[sk], 16)
            return _Done()

        return self.dma("gpsimd", fn, semkey, reads=reads, writes=writes)

    def load(self, eng, out_ap, in_ap, key):
        return self.dma(eng, lambda e: e.dma_start(out=out_ap, in_=in_ap), key, writes=[key])

    def store(self, eng, out_ap, in_ap, key):
        return self.dma(eng, lambda e: e.dma_start(out=out_ap, in_=in_ap), key, reads=[key])

    def emit(self):
        nc = self.nc
        with nc.Block() as block:
            for ename in self.ENGS:
                ops = self.ops[ename]
                if not ops:
                    continue
                final = []
                for sk in self.dma_sems_by_eng[ename]:
                    final.append((sk, self.cnt[sk]))
                for ep in range(self.epoch[ename] + 1):
                    sk = ("E", ename, ep)
                    if sk in self.cnt and self.cnt[sk] > 0:
                        final.append((sk, self.cnt[sk]))

                def body(e, ops=ops, final=final):
                    for fn, waits, sk, inc in ops:
                        for wk, wv in waits:
                            e.wait_ge(self.sems[wk], wv)
                        fn(e).then_inc(self.sems[sk], inc)
                    for wk, wv in final:
                        e.wait_ge(self.sems[wk], wv)

                getattr(block, ename)(body)
        self.nc.clear_and_free_semaphores(list(self.sems.values()))
        self.nc.all_engine_barrier()
        self.es.close()


def host_consts(S):
    c = {}
    c["ident"] = np.eye(128, dtype=np.float32)
    half = 32
    inv = 1.0 / (10000.0 ** (np.arange(half, dtype=np.float32) / half))
    inv64 = np.concatenate([inv, inv]).astype(np.float32)
    c["invs"] = (np.concatenate([inv64, inv64]) / np.float32(2 * math.pi)).astype(np.float32).reshape(128, 1)
    tl = np.arange(128)[:, None]
    npr = np.arange(-1, 7)[None, :]
    c["cmpbias"] = np.where(16 * npr + 31 <= tl, 0.0, NEG).astype(np.float32)
    kk = np.arange(128)[None, :]
    c["tri"] = np.where(kk <= tl, 0.0, NEG).astype(np.float32)
    kw = np.arange(640)[None, :]
    c["winbias"] = np.where((kw > tl) & (kw <= tl + 512), 0.0, NEG).astype(np.float32)
    n = np.arange(256)[:, None]
    j = np.arange(64)[None, :]
    ov = np.clip(np.minimum(16 * n + 32, 64 * j + 64) - np.maximum(16 * n, 64 * j), 0, None) / 32.0
    ov[255] = 0.0
    c["overlap"] = ov.astype(np.float32)
    K = np.zeros((128, 3), np.float32)
    B = np.zeros((128, 3), np.float32)
    lo = np.arange(128) < 64
    K[lo] = [0, 0, 0]
    B[lo] = [1e4, 1e4, -1e30]
    K[~lo] = [1, 0, 0]
    B[~lo] = [0, 1e4, 1e4]
    c["selK"] = K
    c["selB"] = B
    c["rowvalid"] = (np.arange(128) >= 31).astype(np.float32).reshape(128, 1)
    s_ = np.arange(64)[:, None]
    t_ = np.arange(64)[None, :]
    c["glamask"] = (s_ <= t_).astype(np.float32)
    return c


CONST_SHAPES = {"ident": [128, 128], "invs": [128, 1], "cmpbias": [128, 8], "tri": [128, 128],
                "winbias": [128, 640], "overlap": [256, 64], "selK": [128, 3], "selB": [128, 3],
                "rowvalid": [128, 1], "glamask": [64, 64]}

WA_COLS = 3624
SRC = dict(q=0, kc=512, vc=640, ks=768, vs=896, kw=1024, vw=1152, gates=1280, gq=1304, gk=1560,
           gv=1816, al=2328, gr=2344, ma=2856, mb=3880)
DST = dict(q=0, qR=512, ks=1024, ksR=1152, kw=1280, kwR=1408, kc=1536, vc=1664, gq=1792, gk=2048,
           al=2304, vs=2320, vw=2448, gates=2576, gv=2600, gr=3112)


def bcast_rows(ap_row, nparts):
    return ap_row.broadcast_to([nparts, ap_row.shape[-1]])


def stage_R(nc, S, T, C, scr):
    st = Stage(nc, "R")
    pos = T["positions"]
    invs = st.sb("invs", [64, 1], F32)
    cosT = st.sb("cosT", [64, S], F32)
    sinT = st.sb("sinT", [64, S], F32)
    posi = st.sb("posi", [64, S], I32)
    tq = st.sb("tq", [64, S], F32)
    t2 = st.sb("t2", [64, S], F32)
    t3 = st.sb("t3", [64, S], F32)
    ni = st.sb("ni", [64, S], I32)
    st.load("sync", invs[:], C["invs"][0:64, :], "invs")
    st.load("sync", posi[:], bcast_rows(pos, 64), "posi")
    st.op("vector", lambda e: e.tensor_copy(out=tq[:], in_=posi[:]), reads=["posi"], writes=["tq"])
    st.op("vector", lambda e: e.tensor_scalar(out=tq[:], in0=tq[:], scalar1=invs[:, 0:1], scalar2=None, op0=ALU.mult),
          reads=["tq", "invs"], writes=["tq"])

    def table(dst, shift):
        st.op("vector", lambda e: e.tensor_scalar(out=t2[:], in0=tq[:], scalar1=float(shift), scalar2=None, op0=ALU.add),
              reads=["tq"], writes=["t2"])
        st.op("vector", lambda e: e.tensor_copy(out=ni[:], in_=t2[:]), reads=["t2"], writes=["ni"])
        st.op("vector", lambda e: e.tensor_copy(out=t3[:], in_=ni[:]), reads=["ni"], writes=["t3"])
        st.op("vector", lambda e: e.tensor_tensor(out=t2[:], in0=t2[:], in1=t3[:], op=ALU.subtract),
              reads=["t2", "t3"], writes=["t2"])
        st.op("vector", lambda e: e.tensor_scalar(out=t3[:], in0=t2[:], scalar1=0.5, scalar2=None, op0=ALU.is_gt),
              reads=["t2"], writes=["t3"])
        st.op("vector", lambda e: e.tensor_tensor(out=t2[:], in0=t2[:], in1=t3[:], op=ALU.subtract),
              reads=["t2", "t3"], writes=["t2"])
        st.op("vector", lambda e: e.tensor_scalar(out=t3[:], in0=t2[:], scalar1=-0.5, scalar2=None, op0=ALU.is_lt),
              reads=["t2"], writes=["t3"])
        st.op("vector", lambda e: e.tensor_tensor(out=t2[:], in0=t2[:], in1=t3[:], op=ALU.add),
              reads=["t2", "t3"], writes=["t2"])
        st.op("scalar", lambda e: e.activation(out=dst[:], in_=t2[:], func=AF.Sin, scale=6.283185),
              reads=["t2"], writes=[dst.name])

    table(sinT, 0.0)
    table(cosT, 0.25)
    st.store("sync", scr["SIN"], sinT[:], sinT.name)
    st.store("sync", scr["COS"], cosT[:], cosT.name)
    st.emit()


def stage_A(nc, S, T, C, scr):
    st = Stage(nc, "A")
    SH = S // 2
    NT = SH // 128
    NC4 = SH // 512
    x, w_in = T["x"], T["w_in"]
    uT = st.sb("uT", [128, 8, SH], BF16)
    WA = st.sb("WA", [128, 8, WA_COLS], BF16)
    ident = st.sb("ident", [128, 128], BF16)
    gm = st.sb("gm", [128, 8], F32)
    cosT = st.sb("cosT", [128, SH], F32)
    sinT = st.sb("sinT", [128, SH], F32)
    st.dma("gpsimd", lambda e: e.dma_start(out=ident[:], in_=C["ident"]), "ident", writes=["ident"])
    st.dma("sync", lambda e: e.dma_start(out=gm[:], in_=T["g_mix"].rearrange("o (kt p) -> p (o kt)", p=128)),
           "gm", writes=["gm"])

    wst = [st.sb(f"wst{i}", [128, 2856], F32) for i in range(2)]
    for kt in range(8):
        i = kt % 2
        wk = f"wst{i}"
        st.load("sync" if kt % 2 == 0 else "gpsimd", wst[i][:], w_in[0, kt * 128:(kt + 1) * 128, 0:2856], wk)
        g = gm[:, kt:kt + 1]
        wkey = ("WA", kt)

        def cp(eng, dst0, src0, n, mul=1.0, i=i, kt=kt, g=g, wk=wk, wkey=wkey):
            st.op(eng, lambda e: e.tensor_scalar(out=WA[:, kt, dst0:dst0 + n], in0=wst[i][:, src0:src0 + n],
                                                 scalar1=g, scalar2=float(mul), op0=ALU.mult, op1=ALU.mult),
                  reads=[wk, "gm"], writes=[wkey])

        def rot(eng, dst0, src0, nh, mul=1.0, i=i, kt=kt, g=g, wk=wk, wkey=wkey):
            s4 = wst[i][:, src0:src0 + nh * 64].rearrange("p (h two j) -> p h two j", two=2, j=32)
            d4 = WA[:, kt, dst0:dst0 + nh * 64].rearrange("p (h two j) -> p h two j", two=2, j=32)
            st.op(eng, lambda e: e.tensor_scalar(out=d4[:, :, 0, :], in0=s4[:, :, 1, :], scalar1=g, scalar2=float(-mul),
                                                 op0=ALU.mult, op1=ALU.mult), reads=[wk, "gm"], writes=[wkey])
            st.op(eng, lambda e: e.tensor_scalar(out=d4[:, :, 1, :], in0=s4[:, :, 0, :], scalar1=g, scalar2=float(mul),
                                                 op0=ALU.mult, op1=ALU.mult), reads=[wk, "gm"], writes=[wkey])

        cp("vector", DST["q"], SRC["q"], 512, 0.125)
        rot("gpsimd", DST["qR"], SRC["q"], 8, 0.125)
        cp("vector", DST["ks"], SRC["ks"], 128)
        rot("gpsimd", DST["ksR"], SRC["ks"], 2)
        cp("vector", DST["kw"], SRC["kw"], 128)
        rot("gpsimd", DST["kwR"], SRC["kw"], 2)
        cp("vector", DST["kc"], SRC["kc"], 256)
        cp("gpsimd", DST["gq"], SRC["gq"], 256, 0.125)
        cp("gpsimd", DST["gk"], SRC["gk"], 256)
        cp("vector", DST["al"], SRC["al"], 16)
        cp("vector", DST["vs"], SRC["vs"], 128)
        cp("vector", DST["vw"], SRC["vw"], 128)
        cp("vector", DST["gates"], SRC["gates"], 24)
        cp("gpsimd", DST["gv"], SRC["gv"], 512)
        cp("vector", DST["gr"], SRC["gr"], 512)
    WAK = [("WA", kt) for kt in range(8)]

    xt = [st.sb(f"xt{i}", [128, D], F32) for i in range(2)]
    sq = [st.sb(f"sq{i}", [128, D], BF16) for i in range(2)]
    xn = [st.sb(f"xn{i}", [128, D], BF16) for i in range(2)]
    ss = [st.sb(f"ss{i}", [128, 1], F32) for i in range(2)]
    rs = [st.sb(f"rs{i}", [128, 1], F32) for i in range(2)]
    ptr = [st.ps(f"ptr{i}", [128, 8, 128], BF16) for i in range(2)]
    pa = [st.ps(f"pa{i}", [128, 512], F32) for i in range(2)]
    pb = [st.ps(f"pb{i}", [128, 512], F32) for i in range(2)]
    r1 = [st.sb(f"r1{i}", [128, 512], F32) for i in range(2)]
    r2 = [st.sb(f"r2{i}", [128, 512], F32) for i in range(2)]
    fo = [st.sb(f"fo{i}", [128, 512], BF16) for i in range(3)]
    pt = [st.ps(f"pt{i}", [128, 512], F32) for i in range(2)]
    tA = [st.sb(f"tA{i}", [128, 256], BF16) for i in range(2)]
    tG = [st.sb(f"tG{i}", [128, 24], F32) for i in range(2)]
    tV = [st.sb(f"tV{i}", [128, 512], BF16) for i in range(2)]
    tR = [st.sb(f"tR{i}", [128, 512], BF16) for i in range(2)]
    for hs in range(2):
        H0 = hs * SH
        for hh in range(2):
            st.load("sync", cosT[hh * 64:(hh + 1) * 64, :], scr["COS"][:, H0:H0 + SH], cosT.name)
            st.load("sync", sinT[hh * 64:(hh + 1) * 64, :], scr["SIN"][:, H0:H0 + SH], sinT.name)
        for t in range(NT):
            i = t % 2
            st.load("sync", xt[i][:], x[H0 + t * 128:H0 + (t + 1) * 128, :], f"xt{i}")
            st.op("scalar", lambda e, i=i: e.activation(out=sq[i][:], in_=xt[i][:], func=AF.Square, accum_out=ss[i][:]),
                  reads=[f"xt{i}"], writes=[f"sq{i}", f"ss{i}"])
            st.op("vector", lambda e, i=i: e.tensor_scalar(out=rs[i][:], in0=ss[i][:], scalar1=1.0 / D, scalar2=1e-6,
                                                           op0=ALU.mult, op1=ALU.add), reads=[f"ss{i}"], writes=[f"rs{i}"])
            st.op("scalar", lambda e, i=i: e.activation(out=rs[i][:], in_=rs[i][:], func=AF.Sqrt),
                  reads=[f"rs{i}"], writes=[f"rs{i}"])
            st.op("vector", lambda e, i=i: e.reciprocal(out=rs[i][:], in_=rs[i][:]), reads=[f"rs{i}"], writes=[f"rs{i}"])
            st.op("vector", lambda e, i=i: e.tensor_scalar(out=xn[i][:], in0=xt[i][:], scalar1=rs[i][:, 0:1], scalar2=None,
                                                           op0=ALU.mult), reads=[f"xt{i}", f"rs{i}"], writes=[f"xn{i}"])
            for kt in range(8):
                st.op("tensor", lambda e, i=i, kt=kt: e.transpose(out=ptr[i][:, kt, :], in_=xn[i][:, kt * 128:(kt + 1) * 128],
                                                                  identity=ident[:]),
                      reads=[f"xn{i}", "ident"], writes=[f"ptr{i}"])
            st.op("scalar", lambda e, i=i, t=t: e.copy(out=uT[:, :, t * 128:(t + 1) * 128], in_=ptr[i][:]),
                  reads=[f"ptr{i}"], writes=[("uT", t)])

        rope_groups = [(DST["q"] + 128 * p, DST["qR"] + 128 * p, scr["QT"], 128 * p) for p in range(4)]
        rope_groups += [(DST["ks"], DST["ksR"], scr["KST"], 0), (DST["kw"], DST["kwR"], scr["KWT"], 0)]
        plain_groups = [(DST["kc"], 128, scr["KCT"], 0), (DST["vc"], 128, scr["VCT"], 0),
                        (DST["gq"], 128, scr["GQT"], 0), (DST["gq"] + 128, 128, scr["GQT"], 128),
                        (DST["gk"], 128, scr["GKT"], 0), (DST["gk"] + 128, 128, scr["GKT"], 128),
                        (DST["al"], 16, scr["ALT"], 0)]
        it = 0
        fi = 0
        for tc in range(NC4):
            tsl = slice(tc * 512, (tc + 1) * 512)
            ukeys = [("uT", tc * 4 + j) for j in range(4)]
            for (ca, cb, dst, row0) in rope_groups:
                i = it % 2
                it += 1
                f = fi % 3
                fi += 1
                for kt in range(8):
                    st.op("tensor", lambda e, i=i, kt=kt, ca=ca, tsl=tsl: e.matmul(pa[i][:], WA[:, kt, ca:ca + 128], uT[:, kt, tsl],
                                                                          start=(kt == 0), stop=(kt == 7)),
                          reads=ukeys + [("WA", kt)], writes=[f"pa{i}"])
                for kt in range(8):
                    st.op("tensor", lambda e, i=i, kt=kt, cb=cb, tsl=tsl: e.matmul(pb[i][:], WA[:, kt, cb:cb + 128], uT[:, kt, tsl],
                                                                          start=(kt == 0), stop=(kt == 7)),
                          reads=ukeys + [("WA", kt)], writes=[f"pb{i}"])
                st.op("vector", lambda e, i=i, tsl=tsl: e.tensor_tensor(out=r1[i][:], in0=pa[i][:], in1=cosT[:, tsl], op=ALU.mult),
                      reads=[f"pa{i}", cosT.name], writes=[f"r1{i}"])
                st.op("vector", lambda e, i=i, tsl=tsl: e.tensor_tensor(out=r2[i][:], in0=pb[i][:], in1=sinT[:, tsl], op=ALU.mult),
                      reads=[f"pb{i}", sinT.name], writes=[f"r2{i}"])
                st.op("gpsimd", lambda e, i=i, f=f: e.tensor_tensor(out=fo[f][:], in0=r1[i][:], in1=r2[i][:], op=ALU.add),
                      reads=[f"r1{i}", f"r2{i}"], writes=[f"fo{f}"])
                st.store("sync", dst[row0:row0 + 128, H0 + tc * 512:H0 + (tc + 1) * 512], fo[f][:], f"fo{f}")
            for (ca, n, dst, row0) in plain_groups:
                i = it % 2
                it += 1
                f = fi % 3
                fi += 1
                for kt in range(8):
                    st.op("tensor", lambda e, i=i, kt=kt, ca=ca, n=n, tsl=tsl: e.matmul(pa[i][0:n, :], WA[:, kt, ca:ca + n], uT[:, kt, tsl],
                                                                               start=(kt == 0), stop=(kt == 7)),
                          reads=ukeys + [("WA", kt)], writes=[f"pa{i}"])
                st.op("scalar", lambda e, i=i, f=f, n=n: e.copy(out=fo[f][0:n, :], in_=pa[i][0:n, :]),
                      reads=[f"pa{i}"], writes=[f"fo{f}"])
                st.store("sync", dst[row0:row0 + n, H0 + tc * 512:H0 + (tc + 1) * 512], fo[f][0:n, :], f"fo{f}")

        it = 0
        for t in range(NT):
            rows = slice(t * 128, (t + 1) * 128)
            drows = slice(H0 + t * 128, H0 + (t + 1) * 128)
            b = t % 2
            for (c0, n, kind) in [(DST["vs"], 280, 0), (DST["gv"], 512, 1), (DST["gr"], 512, 2)]:
                i = it % 2
                it += 1
                for kt in range(8):
                    st.op("tensor", lambda e, i=i, kt=kt, c0=c0, n=n, rows=rows: e.matmul(pt[i][:, 0:n], uT[:, kt, rows], WA[:, kt, c0:c0 + n],
                                                                               start=(kt == 0), stop=(kt == 7)),
                          reads=[("uT", t), ("WA", kt)], writes=[f"pt{i}"])
                if kind == 0:
                    st.op("vector", lambda e, i=i, b=b: e.tensor_copy(out=tA[b][:], in_=pt[i][:, 0:256]),
                          reads=[f"pt{i}"], writes=[f"tA{b}"])
                    st.op("scalar", lambda e, i=i, b=b: e.activation(out=tG[b][:], in_=pt[i][:, 256:280], func=AF.Sigmoid),
                          reads=[f"pt{i}"], writes=[f"tG{b}"])
                    st.store("gpsimd", scr["VS"][drows, :], tA[b][:, 0:128], f"tA{b}")
                    st.store("gpsimd", scr["VW"][drows, :], tA[b][:, 128:256], f"tA{b}")
                    st.store("gpsimd", scr["GATE"][drows, :], tG[b][:], f"tG{b}")
                elif kind == 1:
                    st.op("vector", lambda e, i=i, b=b: e.tensor_copy(out=tV[b][:], in_=pt[i][:]),
                          reads=[f"pt{i}"], writes=[f"tV{b}"])
                    st.store("gpsimd", scr["GV"][drows, :], tV[b][:], f"tV{b}")
                else:
                    st.op("scalar", lambda e, i=i, b=b: e.activation(out=tR[b][:], in_=pt[i][:], func=AF.Silu),
                          reads=[f"pt{i}"], writes=[f"tR{b}"])
                    st.store("gpsimd", scr["GR"][drows, :], tR[b][:], f"tR{b}")

    st.emit()


SCR_SPEC = lambda S: {
    "SIN": ([64, S], F32), "COS": ([64, S], F32),
    "QT": ([512, S], BF16), "KST": ([128, S], BF16), "KWT": ([128, S], BF16),
    "KCT": ([128, S], BF16), "VCT": ([128, S], BF16), "GQT": ([256, S], BF16), "GKT": ([256, S], BF16),
    "ALT": ([16, S], BF16), "VS": ([S, 128], BF16), "VW": ([S, 128], BF16), "GATE": ([S, 24], F32),
    "GV": ([S, 512], BF16), "GR": ([S, 512], BF16),
    "KCMPT": ([128, S // 16], BF16), "VCMP": ([2, S // 16, 64], BF16),
    "ONSA": ([S, 512], BF16), "OGLA": ([S, 512], BF16),
}

IN_SHAPES = lambda S: {
    "x": ([S, D], F32), "xh": ([S // 2, D], F32), "positions": ([1, S], I32), "g_mix": ([1, D], F32),
    "w_in": ([1, D, 4904], F32),
    "cmp_pos_k": ([1, 32, 64], F32), "cmp_w1_k": ([1, 2048, 256], F32), "cmp_b1_k": ([1, 256], F32),
    "cmp_w2_k": ([1, 256, 64], F32), "cmp_b2_k": ([1, 64], F32),
    "cmp_pos_v": ([1, 32, 64], F32), "cmp_w1_v": ([1, 2048, 256], F32), "cmp_b1_v": ([1, 256], F32),
    "cmp_w2_v": ([1, 256, 64], F32), "cmp_b2_v": ([1, 64], F32),
    "gla_w_a2": ([1, 16, 256], F32), "gla_b_a": ([1, 256], F32), "gla_norm_g": ([1, 512], F32),
    "w_proj_nsa": ([1, 512, D], F32), "w_proj_gla": ([1, 512, D], F32), "w_out": ([1, D, D], F32),
    "g_ffn": ([1, D], F32), "w_grp": ([1, D, 4], F32), "b_grp": ([1, 4], F32), "w_exp": ([1, D, 32], F32),
    "b_exp": ([1, 32], F32), "w_gate": ([1, 32, D, 512], F32), "w_up": ([1, 32, D, 512], F32),
    "w_down": ([1, 32, 512, D], F32), "g_final": ([1, D], F32), "halfidx": ([1, 1], I32),
}


def build(S, stages="ABCDE", debug=(), scr_in=(), ncores=2):
    return build_full(S, ncores, stages=("R" + stages) if "A" in stages else stages, debug=debug, scr_in=scr_in)


def stage_B(nc, S, T, C, scr):
    st = Stage(nc, "B")
    NCMP = S // 16 - 1
    srcT = {"k": st.sb("kcT", [128, S], BF16), "v": st.sb("vcT", [128, S], BF16)}
    st.load("sync", srcT["k"][:], scr["KCT"], "kcT")
    st.load("sync", srcT["v"][:], scr["VCT"], "vcT")
    cosf = st.sb("cosf", [64, S], F32)
    sinf = st.sb("sinf", [64, S], F32)
    st.load("sync", cosf[:], scr["COS"], "cosf")
    st.load("sync", sinf[:], scr["SIN"], "sinf")
    cos_e = cosf[:, 31:31 + 16 * (NCMP - 1) + 1:16]
    sin_e = sinf[:, 31:31 + 16 * (NCMP - 1) + 1:16]
    ph = [st.ps(f"ph{i}", [128, 512], F32) for i in range(2)]
    pbias = st.ps("pbias", [128, 2], F32)
    pk = [st.ps(f"pk{i}", [64, 512], F32) for i in range(2)]
    pv = st.ps("pv", [128, 64], F32)
    for kv in ("k", "v"):
        W1 = st.sb(f"W1{kv}", [128, 32, 256], BF16)
        w1src = T[f"cmp_w1_{kv}"][0].rearrange("(i d) h -> d i h", d=64)
        st.dma("gpsimd", lambda e, W1=W1, w1src=w1src: e.dma_start(out=W1[0:64], in_=w1src), f"W1{kv}", writes=[f"W1{kv}"])
        st.dma("gpsimd", lambda e, W1=W1, w1src=w1src: e.dma_start(out=W1[64:128], in_=w1src), f"W1{kv}", writes=[f"W1{kv}"])
        posf = st.sb(f"posf{kv}", [64, 32], F32)
        posb = st.sb(f"posb{kv}", [64, 32], BF16)
        st.load("sync", posf[:], T[f"cmp_pos_{kv}"][0].rearrange("i d -> d i"), f"posf{kv}")
        st.do("vector", "tensor_copy", out=posb[:], in_=posf[:], reads=[f"posf{kv}"], writes=[f"posb{kv}"])
        b1 = st.sb(f"b1{kv}", [128, 2], F32)
        st.load("sync", b1[:], T[f"cmp_b1_{kv}"].rearrange("o (hh p) -> p (o hh)", p=128), f"b1{kv}")
        w2f = st.sb(f"w2f{kv}", [128, 2, 64], F32)
        st.load("sync", w2f[:], T[f"cmp_w2_{kv}"][0].rearrange("(hh p) d -> p hh d", p=128), f"w2f{kv}")
        W2 = st.sb(f"W2{kv}", [128, 2, 64], BF16)
        st.do("vector", "tensor_copy", out=W2[:], in_=w2f[:], reads=[f"w2f{kv}"], writes=[f"W2{kv}"])
        bias1 = st.sb(f"bias1{kv}", [128, 2], F32)
        for hh in range(2):
            for i in range(32):
                st.do("tensor", "matmul", pbias[:, hh:hh + 1], W1[0:64, i, hh * 128:(hh + 1) * 128], posb[:, i:i + 1],
                      start=(i == 0), stop=(i == 31), reads=[f"W1{kv}", f"posb{kv}"], writes=["pbias"])
        st.do("vector", "tensor_tensor", out=bias1[:], in0=pbias[:], in1=b1[:], op=ALU.add,
              reads=["pbias", f"b1{kv}"], writes=[f"bias1{kv}"])
        if kv == "k":
            W2R = st.sb("W2R", [128, 2, 64], BF16)
            for hh in range(2):
                st.do("vector", "tensor_scalar", out=W2R[:, hh, 0:32], in0=w2f[:, hh, 32:64], scalar1=-1.0, scalar2=None,
                      op0=ALU.mult, reads=["w2fk"], writes=["W2R"])
                st.do("vector", "tensor_copy", out=W2R[:, hh, 32:64], in_=w2f[:, hh, 0:32], reads=["w2fk"], writes=["W2R"])
            b2 = st.sb("b2k", [64, 1], F32)
            b2R = st.sb("b2R", [64, 1], F32)
            b2src = T["cmp_b2_k"].rearrange("o d -> d o")
            st.load("sync", b2[:], b2src, "b2k")
            st.load("sync", b2R[0:32], b2src[32:64], "b2R")
            st.load("sync", b2R[32:64], b2src[0:32], "b2R")
            st.do("vector", "tensor_scalar", out=b2R[0:32], in0=b2R[0:32], scalar1=-1.0, scalar2=None, op0=ALU.mult,
                  reads=["b2R"], writes=["b2R"])
        else:
            b2v = st.sb("b2v", [128, 64], F32)
            st.load("sync", b2v[:], bcast_rows(T["cmp_b2_v"], 128), "b2v")
        for g in range(2):
            gp = slice(g * 64, (g + 1) * 64)
            h1T = [st.sb(f"h1T{kv}{g}{hh}", [128, 256 if NCMP <= 256 else NCMP], BF16) for hh in range(2)]
            for hh in range(2):
                hk = f"h1T{kv}{g}{hh}"
                for i in range(32):
                    st.do("tensor", "matmul", ph[hh][:, 0:NCMP], W1[gp, i, hh * 128:(hh + 1) * 128],
                          srcT[kv][gp, i:i + 16 * (NCMP - 1) + 1:16], start=(i == 0), stop=(i == 31),
                          reads=[f"W1{kv}", f"{kv}cT"], writes=[f"ph{hh}"])
                xh = st.sb(f"xh{kv}{g}{hh}", [128, NCMP], F32)
                t1 = st.sb(f"t1{kv}{g}{hh}", [128, NCMP], F32)
                xk, tk = f"xh{kv}{g}{hh}", f"t1{kv}{g}{hh}"
                st.do("scalar", "activation", out=xh[:], in_=ph[hh][:, 0:NCMP], func=AF.Identity, bias=bias1[:, hh:hh + 1],
                      reads=[f"ph{hh}", f"bias1{kv}"], writes=[xk])
                st.do("vector", "tensor_tensor", out=t1[:], in0=xh[:], in1=xh[:], op=ALU.mult, reads=[xk], writes=[tk])
                st.do("vector", "tensor_scalar", out=t1[:], in0=t1[:], scalar1=0.044715, scalar2=1.0, op0=ALU.mult, op1=ALU.add,
                      reads=[tk], writes=[tk])
                st.do("vector", "tensor_tensor", out=t1[:], in0=t1[:], in1=xh[:], op=ALU.mult, reads=[tk, xk], writes=[tk])
                st.do("scalar", "activation", out=t1[:], in_=t1[:], func=AF.Tanh, scale=0.7978845608028654,
                      reads=[tk], writes=[tk])
                st.do("vector", "tensor_scalar", out=t1[:], in0=t1[:], scalar1=1.0, scalar2=0.5, op0=ALU.add, op1=ALU.mult,
                      reads=[tk], writes=[tk])
                st.do("vector", "tensor_tensor", out=h1T[hh][:, 0:NCMP], in0=t1[:], in1=xh[:], op=ALU.mult,
                      reads=[tk, xk], writes=[hk])
            hks = [f"h1T{kv}{g}{hh}" for hh in range(2)]
            if kv == "k":
                for hh in range(2):
                    st.do("tensor", "matmul", pk[0][:, 0:NCMP], W2[:, hh, :], h1T[hh][:, 0:NCMP], start=(hh == 0), stop=(hh == 1),
                          reads=hks + ["W2k"], writes=["pk0"])
                for hh in range(2):
                    st.do("tensor", "matmul", pk[1][:, 0:NCMP], W2R[:, hh, :], h1T[hh][:, 0:NCMP], start=(hh == 0), stop=(hh == 1),
                          reads=hks + ["W2R"], writes=["pk1"])
                ka = st.sb(f"ka{g}", [64, NCMP], F32)
                kb = st.sb(f"kb{g}", [64, NCMP], F32)
                ko = st.sb(f"ko{g}", [64, NCMP], BF16)
                st.do("scalar", "activation", out=ka[:], in_=pk[0][:, 0:NCMP], func=AF.Identity, bias=b2[:, 0:1],
                      reads=["pk0", "b2k"], writes=[f"ka{g}"])
                st.do("scalar", "activation", out=kb[:], in_=pk[1][:, 0:NCMP], func=AF.Identity, bias=b2R[:, 0:1],
                      reads=["pk1", "b2R"], writes=[f"kb{g}"])
                st.do("vector", "tensor_tensor", out=ka[:], in0=ka[:], in1=cos_e, op=ALU.mult, reads=[f"ka{g}", "cosf"], writes=[f"ka{g}"])
                st.do("vector", "tensor_tensor", out=kb[:], in0=kb[:], in1=sin_e, op=ALU.mult, reads=[f"kb{g}", "sinf"], writes=[f"kb{g}"])
                st.do("vector", "tensor_tensor", out=ko[:], in0=ka[:], in1=kb[:], op=ALU.add, reads=[f"ka{g}", f"kb{g}"], writes=[f"ko{g}"])
                st.store("sync", scr["KCMPT"][gp, 0:NCMP], ko[:], f"ko{g}")
            else:
                for ci, n0 in enumerate(range(0, NCMP, 128)):
                    n = min(128, NCMP - n0)
                    for hh in range(2):
                        st.do("tensor", "matmul", pv[0:n, :], h1T[hh][:, n0:n0 + n], W2[:, hh, :], start=(hh == 0), stop=(hh == 1),
                              reads=hks + ["W2v"], writes=["pv"])
                    vo = st.sb(f"vo{g}{ci}", [128, 64], BF16)
                    st.do("vector", "tensor_tensor", out=vo[0:n, :], in0=pv[0:n, :], in1=b2v[0:n, :], op=ALU.add,
                          reads=["pv", "b2v"], writes=[f"vo{g}{ci}"])
                    st.store("sync", scr["VCMP"][g, n0:n0 + n, :], vo[0:n, :], f"vo{g}{ci}")
    st.emit()


def stage_C(nc, S, T, C, scr):
    st = Stage(nc, "C")
    NT = S // 128
    NSLC = S // 64
    NCP = S // 16
    NCH = (NCP + 127) // 128
    SW = 640
    ident = st.sb("ident", [128, 128], BF16)
    st.dma("gpsimd", lambda e: e.dma_start(out=ident[:], in_=C["ident"]), "ident", writes=["ident"])
    ovl = st.sb("ovl", [128, NCH, NSLC], BF16)
    for j in range(NCH):
        n = min(128, NCP - j * 128)
        st.dma("gpsimd", lambda e, j=j, n=n: e.dma_start(out=ovl[0:n, j, :], in_=C["overlap"][j * 128:j * 128 + n, 0:NSLC]),
               "ovl", writes=["ovl"])
    cst = {}
    for k in ("cmpbias", "tri", "winbias", "selK", "selB", "rowvalid"):
        cst[k] = st.sb("c_" + k, CONST_SHAPES[k], F32)
        st.load("sync", cst[k][:], C[k], "c_" + k)
    gates = st.sb("gates", [128, NT, 24], F32)
    st.load("sync", gates[:], scr["GATE"].rearrange("(c p) k -> p c k", p=128), "gates")

    ps = [st.ps(f"ps{i}", [128, 512], F32) for i in range(3)]
    pT = [st.ps(f"pT{i}", [128, 8, 128], BF16) for i in range(2)]
    po = st.ps("po", [128, 512], F32)
    po2 = st.ps("po2", [128, 512], F32)
    pimp = st.ps("pimp", [128, 512], F32)

    S_sb = [st.sb(f"S_sb{i}", [128, S], F32) for i in range(4)]
    P_sb = [st.sb(f"P_sb{i}", [128, S], BF16) for i in range(4)]
    PT_sb = [st.sb(f"PT_sb{i}", [128, NT, 128], BF16) for i in range(2)]
    Sw = [st.sb(f"Sw{i}", [128, SW], F32) for i in range(4)]
    Pw = [st.sb(f"Pw{i}", [128, SW], BF16) for i in range(4)]
    PTw = [st.sb(f"PTw{i}", [128, 5, 128], BF16) for i in range(2)]
    mx = [st.sb(f"mx{i}", [128, 1], F32) for i in range(4)]
    mxw = [st.sb(f"mxw{i}", [128, 1], F32) for i in range(4)]
    sums = [st.sb(f"sums{i}", [128, 12], F32) for i in range(2)]
    rr = [st.sb(f"rr{i}", [128, 12], F32) for i in range(2)]
    impS = st.sb("impS", [128, NSLC], F32)
    imp2 = st.sb("imp2", [128, NSLC], F32)
    m8a = st.sb("m8a", [128, 8], F32)
    m8b = st.sb("m8b", [128, 8], F32)
    mb = st.sb("mb", [128, NSLC], F32)
    acc = st.sb("acc", [128, 256], F32)
    oout = [st.sb(f"oout{i}", [128, 256], BF16) for i in range(2)]
    QTb = [st.sb(f"QTb{i}", [64, 4, 128], BF16) for i in range(2)]
    KS = st.sb("KS", [64, S], BF16)
    KW = st.sb("KW", [64, S], BF16)
    VS = st.sb("VS", [128, NT, 64], BF16)
    VW = st.sb("VW", [128, NT, 64], BF16)
    KC = st.sb("KC", [64, NCP], BF16)
    VC = st.sb("VC", [128, NCH, 64], BF16)
    cnt = {"tb": 0, "sb": 0, "cp": 0}

    def next_ps():
        i = cnt["sb"] % 3
        cnt["sb"] += 1
        return i

    def tail_group(items, act_only=False):
        rounds = []
        for it in items:
            L = it[4]
            nkt = (L + 127) // 128
            for k0 in range(0, nkt, 8):
                rounds.append((it, k0, min(8, nkt - k0), nkt))

        def emit_T(r):
            (Pt, pkey, PTt, ptkey, L, Vt, vkey, po_ap, pokey, extra), k0, nb, nkt = r
            b = cnt["tb"] % 2
            cnt["tb"] += 1
            for kk in range(nb):
                kt = k0 + kk
                nj = min(128, L - kt * 128)
                st.do("tensor", "transpose", out=pT[b][0:nj, kk, :], in_=Pt[:, kt * 128:kt * 128 + nj], identity=ident[:],
                      reads=[pkey, "ident"], writes=[f"pT{b}"])
            njl = min(128, L - (k0 + nb - 1) * 128)
            cnt["cp"] += 1
            full = nb if njl == 128 else nb - 1
            if act_only or cnt["cp"] % 2 == 0:
                eng, meth = "scalar", "copy"
            else:
                eng, meth = "vector", "tensor_copy"
            if full > 0:
                st.do(eng, meth, out=PTt[:, k0:k0 + full, :], in_=pT[b][:, 0:full, :], reads=[f"pT{b}"], writes=[(ptkey, k0, 0)])
            if full < nb:
                st.do(eng, meth, out=PTt[0:njl, k0 + nb - 1, :], in_=pT[b][0:njl, nb - 1, :], reads=[f"pT{b}"], writes=[(ptkey, k0, 1)])

        def emit_PV(r):
            (Pt, pkey, PTt, ptkey, L, Vt, vkey, po_ap, pokey, extra), k0, nb, nkt = r
            for kk in range(nb):
                kt = k0 + kk
                nj = min(128, L - kt * 128)
                st.do("tensor", "matmul", po_ap, PTt[0:nj, kt, :], Vt(kt, nj), start=(kt == 0), stop=(kt == nkt - 1),
                      reads=[(ptkey, k0, 0), (ptkey, k0, 1), vkey], writes=[pokey])
                if extra is not None:
                    extra(kt, nj, nkt)

        for i, r in enumerate(rounds):
            emit_T(r)
            if i >= 1:
                emit_PV(rounds[i - 1])
        emit_PV(rounds[-1])

    for g in range(2):
        gp = slice(g * 64, (g + 1) * 64)
        st.load("sync", KS[:], scr["KST"][gp, :], "KS")
        st.load("sync", KW[:], scr["KWT"][gp, :], "KW")
        st.load("gpsimd", VS[:], scr["VS"][:, gp].rearrange("(kt p) d -> p kt d", p=128), "VS")
        st.load("gpsimd", VW[:], scr["VW"][:, gp].rearrange("(kt p) d -> p kt d", p=128), "VW")
        st.load("sync", KC[:, 0:NCP - 1], scr["KCMPT"][gp, 0:NCP - 1], "KC")
        st.do("vector", "memset", VC[:], 0.0, reads=[], writes=["VC"])
        for j in range(NCH):
            n = min(128, NCP - 1 - j * 128)
            st.load("sync", VC[0:n, j, :], scr["VCMP"][g, j * 128:j * 128 + n, :], "VC")

        for c in range(NT):
            cblk = slice(c * 128, (c + 1) * 128)
            cb = c % 2
            qb = c % 2
            qk = f"QTb{qb}"
            st.load("sync", QTb[qb][:], scr["QT"][g * 256:(g + 1) * 256, cblk].rearrange("(h d) t -> d h t", d=64), qk)
            ncmp = 8 * c + 7
            ncp32 = ((ncmp + 31) // 32) * 32
            for h in range(4):
                si = next_ps()
                st.do("tensor", "matmul", ps[si][:, 0:ncmp], QTb[qb][:, h, :], KC[:, 0:ncmp], start=True, stop=True,
                      reads=[qk, "KC"], writes=[f"ps{si}"])
                if ncmp > 8:
                    st.do("scalar", "copy", out=Sw[h][:, 0:ncmp - 8], in_=ps[si][:, 0:ncmp - 8], reads=[f"ps{si}"], writes=[f"Sw{h}"])
                    st.do("vector", "tensor_tensor", out=Sw[h][:, ncmp - 8:ncmp], in0=ps[si][:, ncmp - 8:ncmp],
                          in1=cst["cmpbias"][:, 0:8], op=ALU.add, reads=[f"ps{si}", "c_cmpbias"], writes=[f"Sw{h}"])
                else:
                    st.do("vector", "tensor_tensor", out=Sw[h][:, 0:7], in0=ps[si][:, 0:7],
                          in1=cst["cmpbias"][:, 1:8], op=ALU.add, reads=[f"ps{si}", "c_cmpbias"], writes=[f"Sw{h}"])
            for h in range(4):
                st.do("vector", "reduce_max", out=mxw[h][:], in_=Sw[h][:, 0:ncmp], axis=AX.X, negate=True, reads=[f"Sw{h}"], writes=[f"mxw{h}"])
            for h in range(4):
                sidx = h * 3
                st.do("scalar", "activation", out=Sw[h][:, 0:ncmp], in_=Sw[h][:, 0:ncmp], func=AF.Exp, bias=mxw[h][:, 0:1],
                      accum_out=sums[cb][:, sidx:sidx + 1], reads=[f"Sw{h}", f"mxw{h}"], writes=[f"Sw{h}", (f"sums{cb}", sidx)])
            for h in range(4):
                sidx = h * 3
                st.do("vector", "reciprocal", out=mxw[h][:], in_=sums[cb][:, sidx:sidx + 1], reads=[(f"sums{cb}", sidx)], writes=[f"mxw{h}"])
                if c == 0:
                    st.do("vector", "tensor_tensor", out=mxw[h][:], in0=mxw[h][:], in1=cst["rowvalid"][:], op=ALU.mult,
                          reads=[f"mxw{h}", "c_rowvalid"], writes=[f"mxw{h}"])
            for h in range(4):
                st.do("gpsimd", "memset", Pw[h][:, ncmp:ncp32], 0.0, reads=[], writes=[f"Pw{h}"])
                st.do("vector", "tensor_scalar", out=Pw[h][:, 0:ncmp], in0=Sw[h][:, 0:ncmp], scalar1=mxw[h][:, 0:1], scalar2=None,
                      op0=ALU.mult, reads=[f"Sw{h}", f"mxw{h}"], writes=[f"Pw{h}"])
            items = []
            for h in range(4):
                def extra(kt, nj, nkt, h=h):
                    st.do("tensor", "matmul", pimp[:, 0:NSLC], PTw[h % 2][0:nj, kt, :], ovl[0:nj, kt, :],
                          start=(h == 0 and kt == 0), stop=(h == 3 and kt == nkt - 1), reads=[(f"PTw{h % 2}", 0, 0), (f"PTw{h % 2}", 0, 1), "ovl"], writes=["pimp"])
                items.append((Pw[h], f"Pw{h}", PTw[h % 2], f"PTw{h % 2}", ncp32, lambda kt, nj: VC[0:nj, kt, :], "VC",
                              po[:, h * 64:(h + 1) * 64], "po", extra))
            tail_group(items)
            use_sel = (2 * c + 2) > 16
            if use_sel:
                st.do("vector", "tensor_copy", out=impS[:], in_=pimp[:, 0:NSLC], reads=["pimp"], writes=["impS"])
                if 2 * c + 2 < NSLC:
                    st.do("vector", "memset", impS[:, 2 * c + 2:NSLC], -1e30, reads=[], writes=["impS"])
                lo = 2 * c - 1
                st.do("vector", "tensor_tensor", out=impS[:, lo:lo + 3], in0=impS[:, lo:lo + 3], in1=cst["selK"][:, 0:3], op=ALU.mult,
                      reads=["impS", "c_selK"], writes=["impS"])
                st.do("vector", "tensor_tensor", out=impS[:, lo:lo + 3], in0=impS[:, lo:lo + 3], in1=cst["selB"][:, 0:3], op=ALU.add,
                      reads=["impS", "c_selB"], writes=["impS"])
                st.do("vector", "memset", impS[:, 0:1], 1e4, reads=[], writes=["impS"])
                st.do("vector", "max", out=m8a[:], in_=impS[:], reads=["impS"], writes=["m8a"])
                st.do("vector", "match_replace", out=imp2[:], in_to_replace=m8a[:], in_values=impS[:], imm_value=-3e38,
                      reads=["impS", "m8a"], writes=["imp2"])
                st.do("vector", "max", out=m8b[:], in_=imp2[:], reads=["imp2"], writes=["m8b"])
                st.do("vector", "tensor_scalar", out=mb[:], in0=impS[:], scalar1=m8b[:, 7:8], scalar2=-NEG, op0=ALU.is_ge, op1=ALU.mult,
                      reads=["impS", "m8b"], writes=["mb"])
                st.do("vector", "tensor_scalar", out=mb[:], in0=mb[:], scalar1=NEG, scalar2=None, op0=ALU.add,
                      reads=["mb"], writes=["mb"])
            L = 128 * (c + 1)
            k0w = max(0, 128 * c - 512)
            Lw = L - k0w
            boff = k0w - (128 * c - 512)
            kt0 = k0w // 128

            def slc_p1():
                for h in range(4):
                    sk = f"S_sb{h}"
                    for k0 in range(0, L, 512):
                        w = min(512, L - k0)
                        si = next_ps()
                        st.do("tensor", "matmul", ps[si][:, 0:w], QTb[qb][:, h, :], KS[:, k0:k0 + w], start=True, stop=True,
                              reads=[qk, "KS"], writes=[f"ps{si}"])
                        if use_sel:
                            nb = w // 64
                            mbb = mb[:, k0 // 64:k0 // 64 + nb].unsqueeze(2).broadcast_to([128, nb, 64])
                            st.do("vector", "tensor_tensor", out=S_sb[h][:, k0:k0 + w].rearrange("p (b k) -> p b k", k=64),
                                  in0=ps[si][:, 0:w].rearrange("p (b k) -> p b k", k=64), in1=mbb, op=ALU.add,
                                  reads=[f"ps{si}", "mb"], writes=[(sk, k0 // 512)])
                        else:
                            st.do("scalar", "copy", out=S_sb[h][:, k0:k0 + w], in_=ps[si][:, 0:w], reads=[f"ps{si}"], writes=[(sk, k0 // 512)])
                    lk = (sk, (L - 128) // 512)
                    st.do("vector", "tensor_tensor", out=S_sb[h][:, L - 128:L], in0=S_sb[h][:, L - 128:L], in1=cst["tri"][:], op=ALU.add,
                          reads=[lk, "c_tri"], writes=[lk])

            def slc_p2():
                for h in range(4):
                    st.do("vector", "reduce_max", out=mx[h][:], in_=S_sb[h][:, 0:L], axis=AX.X, negate=True,
                          reads=[(f"S_sb{h}", k) for k in range((L + 511) // 512)], writes=[f"mx{h}"])

            def slc_p3():
                for h in range(4):
                    sidx = h * 3 + 1
                    st.do("scalar", "activation", out=P_sb[h][:, 0:L], in_=S_sb[h][:, 0:L], func=AF.Exp, bias=mx[h][:, 0:1],
                          accum_out=sums[cb][:, sidx:sidx + 1], reads=[(f"S_sb{h}", k) for k in range((L + 511) // 512)] + [f"mx{h}"],
                          writes=[f"P_sb{h}", (f"sums{cb}", sidx)])

            def slc_p4():
                tail_group([(P_sb[h], f"P_sb{h}", PT_sb[h % 2], f"PT_sb{h % 2}", L, lambda kt, nj: VS[:, kt, :], "VS",
                             po[:, 256 + h * 64:256 + (h + 1) * 64], "po", None) for h in range(4)], act_only=True)

            def win_p1():
                for h in range(4):
                    for k0 in range(0, Lw, 512):
                        w = min(512, Lw - k0)
                        si = next_ps()
                        st.do("tensor", "matmul", ps[si][:, 0:w], QTb[qb][:, h, :], KW[:, k0w + k0:k0w + k0 + w], start=True, stop=True,
                              reads=[qk, "KW"], writes=[f"ps{si}"])
                        st.do("vector", "tensor_tensor", out=Sw[h][:, k0:k0 + w], in0=ps[si][:, 0:w],
                              in1=cst["winbias"][:, boff + k0:boff + k0 + w], op=ALU.add, reads=[f"ps{si}", "c_winbias"], writes=[f"Sw{h}"])

            def win_p2():
                for h in range(4):
                    st.do("vector", "reduce_max", out=mxw[h][:], in_=Sw[h][:, 0:Lw], axis=AX.X, negate=True, reads=[f"Sw{h}"], writes=[f"mxw{h}"])

            def win_p3():
                for h in range(4):
                    sidx = h * 3 + 2
                    st.do("scalar", "activation", out=Pw[h][:, 0:Lw], in_=Sw[h][:, 0:Lw], func=AF.Exp, bias=mxw[h][:, 0:1],
                          accum_out=sums[cb][:, sidx:sidx + 1], reads=[f"Sw{h}", f"mxw{h}"], writes=[f"Pw{h}", (f"sums{cb}", sidx)])

            def win_p4():
                tail_group([(Pw[h], f"Pw{h}", PTw[h % 2], f"PTw{h % 2}", Lw, lambda kt, nj, kt0=kt0: VW[:, kt0 + kt, :], "VW",
                             po2[:, h * 64:(h + 1) * 64], "po_win", None) for h in range(4)])

            win_p1()
            slc_p1()
            win_p2()
            win_p3()
            slc_p2()
            slc_p3()
            win_p4()
            slc_p4()
            sumkeys = [(f"sums{cb}", i) for i in range(12)]
            st.do("vector", "reciprocal", out=rr[cb][:], in_=sums[cb][:], reads=sumkeys, writes=[f"rr{cb}"])
            st.do("vector", "memset", rr[cb][:].rearrange("p (h b) -> p h b", b=3)[:, :, 0], 1.0, reads=[], writes=[f"rr{cb}"])
            st.do("vector", "tensor_tensor", out=rr[cb][:], in0=rr[cb][:], in1=gates[:, c, 12 * g:12 * g + 12], op=ALU.mult,
                  reads=[f"rr{cb}", "gates"], writes=[f"rr{cb}"])
            ob = c % 2
            for h in range(4):
                hs = slice(h * 64, (h + 1) * 64)
                st.do("vector", "tensor_scalar", out=acc[:, hs], in0=po[:, hs], scalar1=rr[cb][:, 3 * h:3 * h + 1], scalar2=None,
                      op0=ALU.mult, reads=["po", f"rr{cb}"], writes=[("acc", h)])
            for h in range(4):
                hs = slice(h * 64, (h + 1) * 64)
                st.do("vector", "scalar_tensor_tensor", out=acc[:, hs], in0=po[:, 256 + h * 64:256 + (h + 1) * 64],
                      scalar=rr[cb][:, 3 * h + 1:3 * h + 2], in1=acc[:, hs], op0=ALU.mult, op1=ALU.add,
                      reads=["po", f"rr{cb}", ("acc", h)], writes=[("acc", h)])
            for h in range(4):
                hs = slice(h * 64, (h + 1) * 64)
                st.do("vector", "scalar_tensor_tensor", out=oout[ob][:, hs], in0=po2[:, hs],
                      scalar=rr[cb][:, 3 * h + 2:3 * h + 3], in1=acc[:, hs], op0=ALU.mult, op1=ALU.add,
                      reads=["po_win", f"rr{cb}", ("acc", h)], writes=[f"oout{ob}"])
            st.store("sync", scr["ONSA"][cblk, g * 256:(g + 1) * 256], oout[ob][:], f"oout{ob}")
    st.emit()


def stage_D(nc, S, T, C, scr):
    st = Stage(nc, "D")
    NCK = S // 64
    NC4 = S // 512
    ident = st.sb("ident", [128, 128], BF16)
    st.dma("gpsimd", lambda e: e.dma_start(out=ident[:], in_=C["ident"]), "ident", writes=["ident"])
    gmask = st.sb("gmask", [64, 64], F32)
    st.load("sync", gmask[:], C["glamask"], "gmask")
    alT = st.sb("alT", [16, S], BF16)
    st.load("sync", alT[:], scr["ALT"], "alT")
    wa2 = st.sb("wa2", [16, 256], BF16)
    st.dma("gpsimd", lambda e: e.dma_start(out=wa2[:], in_=T["gla_w_a2"][0]), "wa2", writes=["wa2"])
    rmask = st.sb("rmask", [128, S], BF16)
    st.do("vector", "memset", rmask[:], 1.0, reads=[], writes=["rmask"])
    st.do("vector", "memset", rmask[:, 0:S:64], 0.0, reads=[], writes=["rmask"])

    bufA = st.sb("bufA", [128, S], F32)
    bufB = st.sb("bufB", [128, S], F32)
    qT = st.sb("qT", [128, S], BF16)
    kT = st.sb("kT", [128, S], BF16)
    kdec = st.sb("kdec", [128, S], BF16)
    V64 = st.sb("V64", [64, NCK, 256], BF16)
    Sf = [st.sb(f"Sf{i}", [128, 256], F32) for i in range(2)]
    Sb = st.sb("Sb", [128, NCK, 256], BF16)
    Qbd = st.sb("Qbd", [128, NCK, 128], BF16)
    nb = st.sb("nb", [128, 1], F32)
    gb = st.sb("gb", [128, 128], F32)
    tmp = [st.sb(f"tmp{i}", [128, 512], F32) for i in range(2)]
    pkv_ = [st.ps(f"pkv{i}", [128, 512], F32) for i in range(2)]
    pkv = [p[:, 0:256] for p in pkv_]
    pxa = pkv
    pTk_ = [st.ps(f"pTk{i}", [128, 1024], BF16) for i in range(2)]
    pTk = [pTk_[0][0:64, 0:128], pTk_[1][0:64, 0:128]]
    pA_ = [st.ps(f"pA{i}", [128, 512], F32) for i in range(2)]
    pA = [p[0:64, 0:128] for p in pA_]
    pO_ = [st.ps(f"pO{i}", [128, 512], F32) for i in range(2)]
    pO = [p[:, 0:256] for p in pO_]
    kdT = [st.sb(f"kdT{i}", [64, 128], BF16) for i in range(2)]
    ATs = [st.sb(f"ATs{i}", [64, 128], BF16) for i in range(2)]
    R64 = [st.sb(f"R64{i}", [128, 128], BF16) for i in range(2)]
    gg = [st.sb(f"gg{i}", [128, 128], F32) for i in range(2)]
    sqj = [st.sb(f"sqj{i}", [128, 128], F32) for i in range(2)]
    ssq = [st.sb(f"ssq{i}", [128, 1], F32) for i in range(2)]
    og = [st.sb(f"og{i}", [128, 128], BF16) for i in range(2)]

    for hp in range(2):
        rows = slice(hp * 128, (hp + 1) * 128)
        cols = slice(hp * 256, (hp + 1) * 256)
        st.load("sync", qT[:], scr["GQT"][rows, :], "qT")
        st.load("sync", kT[:], scr["GKT"][rows, :], "kT")
        st.load("gpsimd", V64[:], scr["GV"][:, cols].rearrange("(c s) e -> s c e", s=64), "V64")
        st.load("sync", nb[:], T["gla_b_a"][:, rows].rearrange("o p -> p o"), "nb")
        st.do("vector", "tensor_scalar", out=nb[:], in0=nb[:], scalar1=-1.0, scalar2=None, op0=ALU.mult, reads=["nb"], writes=["nb"])
        for tc in range(S // 256):
            i = tc % 2
            tsl = slice(tc * 256, (tc + 1) * 256)
            st.do("tensor", "matmul", pxa[i], wa2[:, rows], alT[:, tsl], start=True, stop=True,
                  reads=["wa2", "alT"], writes=[f"pkv{i}"])
            st.do("scalar", "activation", out=tmp[i][:, 0:256], in_=pxa[i], func=AF.Exp, bias=nb[:, 0:1], scale=-1.0,
                  reads=[f"pkv{i}", "nb"], writes=[f"tmp{i}"])
            st.do("vector", "tensor_scalar", out=tmp[i][:, 0:256], in0=tmp[i][:, 0:256], scalar1=1.0, scalar2=None, op0=ALU.add,
                  reads=[f"tmp{i}"], writes=[f"tmp{i}"])
            st.do("scalar", "activation", out=bufA[:, tsl], in_=tmp[i][:, 0:256], func=AF.Ln, reads=[f"tmp{i}"], writes=["bufA"])
        st.do("vector", "tensor_tensor_scan", out=bufB[:], data0=rmask[:], data1=bufA[:], initial=0.0, op0=ALU.mult, op1=ALU.add,
              reads=["rmask", "bufA"], writes=["bufB"])
        st.do("scalar", "activation", out=bufA[:], in_=bufB[:], func=AF.Exp, scale=-1.0 / 16.0, reads=["bufB"], writes=["bufA"])
        st.do("scalar", "activation", out=bufB[:], in_=bufB[:], func=AF.Exp, scale=1.0 / 16.0, reads=["bufB"], writes=["bufB"])
        st.do("vector", "tensor_tensor", out=qT[:], in0=qT[:], in1=bufA[:], op=ALU.mult, reads=["qT", "bufA"], writes=["qT"])
        st.do("vector", "tensor_tensor", out=kT[:], in0=kT[:], in1=bufB[:], op=ALU.mult, reads=["kT", "bufB"], writes=["kT"])
        dec = bufA[:, 63:63 + 64 * (NCK - 1) + 1:64]
        st.do("vector", "tensor_tensor", out=kdec[:].rearrange("p (c s) -> p c s", s=64),
              in0=kT[:].rearrange("p (c s) -> p c s", s=64), in1=dec.unsqueeze(2).broadcast_to([128, NCK, 64]), op=ALU.mult,
              reads=["kT", "bufA"], writes=["kdec"])
        st.do("gpsimd", "memset", Qbd[:], 0.0, reads=[], writes=["Qbd"])
        for h in range(2):
            hr = slice(h * 64, (h + 1) * 64)
            st.do("gpsimd", "tensor_copy", out=Qbd[hr, :, h * 64:(h + 1) * 64], in_=qT[hr, :].rearrange("p (c s) -> p c s", s=64),
                  reads=["qT"], writes=["Qbd"])
        st.do("vector", "memset", Sf[0][:], 0.0, reads=[], writes=["Sf0"])
        def rec_T(c):
            i = c % 2
            csl = slice(c * 64, (c + 1) * 64)
            st.do("tensor", "transpose", out=pTk[i], in_=kdec[:, csl], identity=ident[:], reads=["kdec", "ident"], writes=[f"pTk{i}"])
            st.do("scalar", "copy", out=kdT[i][:], in_=pTk[i], reads=[f"pTk{i}"], writes=[f"kdT{i}"])

        rec_T(0)
        for c in range(NCK):
            i = c % 2
            if c + 1 < NCK:
                rec_T(c + 1)
            st.do("tensor", "matmul", pkv[i], kdT[i][:], V64[:, c, :], start=True, stop=True,
                  reads=[f"kdT{i}", "V64"], writes=[f"pkv{i}"])
            st.do("gpsimd", "tensor_copy", out=Sb[:, c, :], in_=Sf[i][:], reads=[f"Sf{i}"], writes=[("Sb", c)])
            st.do("vector", "scalar_tensor_tensor", out=Sf[1 - i][:], in0=Sf[i][:], scalar=dec[:, c:c + 1],
                  in1=pkv[i], op0=ALU.mult, op1=ALU.add, reads=[f"Sf{i}", "bufA", f"pkv{i}"], writes=[f"Sf{1 - i}"])
        if DBG_D == 2:
            continue
        for h in range(2):
            st.load("sync", gb[h * 64:(h + 1) * 64, :], bcast_rows(T["gla_norm_g"][:, hp * 256 + h * 128:hp * 256 + (h + 1) * 128], 64), "gb")
        def out_A(c):
            i = c % 2
            csl = slice(c * 64, (c + 1) * 64)
            st.do("tensor", "matmul", pA[i], kT[:, csl], Qbd[:, c, :], start=True, stop=True, reads=["kT", "Qbd"], writes=[f"pA{i}"])
            st.do("vector", "tensor_tensor", out=ATs[i][:].rearrange("p (h t) -> p h t", h=2), in0=pA[i].rearrange("p (h t) -> p h t", h=2),
                  in1=gmask[:].unsqueeze(1).broadcast_to([64, 2, 64]), op=ALU.mult, reads=[f"pA{i}", "gmask"], writes=[f"ATs{i}"])

        for c in range(NCK):
            i = c % 2
            csl = slice(c * 64, (c + 1) * 64)
            for h in range(2):
                st.load("sync", R64[i][h * 64:(h + 1) * 64, :], scr["GR"][csl, hp * 256 + h * 128:hp * 256 + (h + 1) * 128], f"R64{i}")
            st.do("gpsimd", "tensor_tensor", out=gg[i][:], in0=R64[i][:], in1=gb[:], op=ALU.mult, reads=[f"R64{i}", "gb"], writes=[f"gg{i}"])
            if c == 0:
                out_A(0)
            if c + 1 < NCK:
                out_A(c + 1)
            st.do("tensor", "matmul", pO[i], Qbd[:, c, :], Sb[:, c, :], start=True, stop=False, reads=["Qbd", ("Sb", c)], writes=[f"pO{i}"])
            st.do("tensor", "matmul", pO[i], ATs[i][:], V64[:, c, :], start=False, stop=True, reads=[f"ATs{i}", "V64"], writes=[f"pO{i}"])
            for h in range(2):
                hr = slice(h * 64, (h + 1) * 64)
                st.do("scalar", "activation", out=sqj[i][hr, :], in_=pO[i][hr, h * 128:(h + 1) * 128], func=AF.Square,
                      accum_out=ssq[i][hr, 0:1], reads=[f"pO{i}"], writes=[(f"sqj{i}", h), (f"ssq{i}", h)])
            st.do("vector", "tensor_scalar", out=ssq[i][:], in0=ssq[i][:], scalar1=1.0 / 128.0, scalar2=1e-6, op0=ALU.mult, op1=ALU.add,
                  reads=[(f"ssq{i}", 0), (f"ssq{i}", 1)], writes=[f"rs{i}"])
            st.do("scalar", "activation", out=ssq[i][:], in_=ssq[i][:], func=AF.Sqrt, reads=[f"rs{i}"], writes=[f"rs{i}"])
            st.do("vector", "reciprocal", out=ssq[i][:], in_=ssq[i][:], reads=[f"rs{i}"], writes=[f"rs{i}", (f"ssq{i}", 0), (f"ssq{i}", 1)])
            for h in range(2):
                hr = slice(h * 64, (h + 1) * 64)
                st.do("vector", "scalar_tensor_tensor", out=og[i][hr, :], in0=pO[i][hr, h * 128:(h + 1) * 128],
                      scalar=ssq[i][hr, 0:1], in1=gg[i][hr, :], op0=ALU.mult, op1=ALU.mult,
                      reads=[f"pO{i}", f"rs{i}", (f"ssq{i}", 0), (f"ssq{i}", 1), f"gg{i}"], writes=[f"og{i}"])
            for h in range(2):
                st.store("sync", scr["OGLA"][csl, hp * 256 + h * 128:hp * 256 + (h + 1) * 128], og[i][h * 64:(h + 1) * 64, :], f"og{i}")
    st.emit()


def rms_rstd(st, src_ap, src_key, sq, ss, rs, i, dim):
    st.do("scalar", "activation", out=sq[i][:], in_=src_ap, func=AF.Square, accum_out=ss[i][:],
          reads=[src_key], writes=[f"sq{i}", f"ss{i}"])
    st.do("vector", "tensor_scalar", out=rs[i][:], in0=ss[i][:], scalar1=1.0 / dim, scalar2=1e-6, op0=ALU.mult, op1=ALU.add,
          reads=[f"ss{i}"], writes=[f"rs{i}"])
    st.do("scalar", "activation", out=rs[i][:], in_=rs[i][:], func=AF.Sqrt, reads=[f"rs{i}"], writes=[f"rs{i}"])
    st.do("vector", "reciprocal", out=rs[i][:], in_=rs[i][:], reads=[f"rs{i}"], writes=[f"rs{i}"])


def stage_E0(nc, S, T, C, scr):
    st = Stage(nc, "E0")
    TH = S // 2
    NTH = TH // 128
    ident = st.sb("ident", [128, 128], BF16)
    st.dma("gpsimd", lambda e: e.dma_start(out=ident[:], in_=C["ident"]), "ident", writes=["ident"])
    gm = st.sb("gm", [128, 8], F32)
    st.load("sync", gm[:], T["g_mix"].rearrange("o (kt p) -> p (o kt)", p=128), "gm")
    Wm = st.sb("Wm", [128, 8, 2048], BF16)
    wst = [st.sb(f"wst{i}", [128, 2048], F32) for i in range(2)]
    for kt in range(8):
        i = kt % 2
        st.load("sync" if i == 0 else "gpsimd", wst[i][:], T["w_in"][0, kt * 128:(kt + 1) * 128, 2856:4904], f"wst{i}")
        st.do("vector" if i == 0 else "gpsimd", "tensor_scalar", out=Wm[:, kt, :], in0=wst[i][:], scalar1=gm[:, kt:kt + 1], scalar2=None,
              op0=ALU.mult, reads=[f"wst{i}", "gm"], writes=[("Wm", kt)])
    xt = [st.sb(f"xt{i}", [128, D], F32) for i in range(2)]
    sq = [st.sb(f"sq{i}", [128, D], BF16) for i in range(2)]
    xn = [st.sb(f"xn{i}", [128, D], BF16) for i in range(2)]
    ss = [st.sb(f"ss{i}", [128, 1], F32) for i in range(2)]
    rs = [st.sb(f"rs{i}", [128, 1], F32) for i in range(2)]
    uTt = [st.sb(f"uTt{i}", [128, 8, 128], BF16) for i in range(2)]
    sg = [st.sb(f"sg{i}", [128, 2048], BF16) for i in range(2)]
    ptr = [st.ps(f"ptr{i}", [128, 8, 128], BF16) for i in range(2)]
    pm = [st.ps(f"pm{i}", [128, 512], F32) for i in range(4)]
    for t in range(NTH):
        i = t % 2
        st.load("sync", xt[i][:], T["xh"][t * 128:(t + 1) * 128, :], f"xt{i}")
        rms_rstd(st, xt[i][:], f"xt{i}", sq, ss, rs, i, D)
        st.do("vector", "tensor_scalar", out=xn[i][:], in0=xt[i][:], scalar1=rs[i][:, 0:1], scalar2=None, op0=ALU.mult,
              reads=[f"xt{i}", f"rs{i}"], writes=[f"xn{i}"])
        for kt in range(8):
            st.do("tensor", "transpose", out=ptr[i][:, kt, :], in_=xn[i][:, kt * 128:(kt + 1) * 128], identity=ident[:],
                  reads=[f"xn{i}", "ident"], writes=[f"ptr{i}"])
        st.do("scalar", "copy", out=uTt[i][:], in_=ptr[i][:], reads=[f"ptr{i}"], writes=[f"uTt{i}"])
        for cc in range(4):
            for kt in range(8):
                st.do("tensor", "matmul", pm[cc][:], uTt[i][:, kt, :], Wm[:, kt, cc * 512:(cc + 1) * 512], start=(kt == 0), stop=(kt == 7),
                      reads=[f"uTt{i}", ("Wm", kt)], writes=[f"pm{cc}"])
            st.do("scalar", "activation", out=sg[i][:, cc * 512:(cc + 1) * 512], in_=pm[cc][:], func=AF.Sigmoid,
                  reads=[f"pm{cc}"], writes=[f"sg{i}"])
        st.store("sync", scr["SIGM"][t * 128:(t + 1) * 128, :], sg[i][:], f"sg{i}")
    st.emit()


def stage_E1(nc, S, T, C, scr, P, ncores):
    st = Stage(nc, "E1")
    TH = S // 2
    NTH = TH // 128
    h1, vT, combT = P["h1"], P["vT"], P["combT"]
    identb = st.sb("identb", [128, 128], BF16)
    st.dma("gpsimd", lambda e: e.dma_start(out=identb[:], in_=C["ident"]), "identb", writes=["identb"])
    Wpn = st.sb("Wpn", [128, 4, D], BF16)
    Wpg = st.sb("Wpg", [128, 4, D], BF16)
    Wo = st.sb("Wo", [128, 8, D], BF16)
    st.dma("gpsimd", lambda e: e.dma_start(out=Wpn[:], in_=T["w_proj_nsa"][0].rearrange("(j p) d -> p j d", p=128)), "Wpn", writes=["Wpn"])
    st.dma("gpsimd", lambda e: e.dma_start(out=Wpg[:], in_=T["w_proj_gla"][0].rearrange("(j p) d -> p j d", p=128)), "Wpg", writes=["Wpg"])
    st.dma("gpsimd", lambda e: e.dma_start(out=Wo[:], in_=T["w_out"][0].rearrange("(j p) d -> p j d", p=128)), "Wo", writes=["Wo"])
    Wr = st.sb("Wr", [128, 8, 36], BF16)
    st.dma("gpsimd", lambda e: e.dma_start(out=Wr[:, :, 0:4], in_=T["w_grp"][0].rearrange("(j p) g -> p j g", p=128)), "Wr", writes=["Wr"])
    st.dma("gpsimd", lambda e: e.dma_start(out=Wr[:, :, 4:36], in_=T["w_exp"][0].rearrange("(j p) g -> p j g", p=128)), "Wr", writes=["Wr"])
    br = st.sb("br", [128, 36], F32)
    st.load("sync", br[:, 0:4], bcast_rows(T["b_grp"], 128), "br")
    st.load("sync", br[:, 4:36], bcast_rows(T["b_exp"], 128), "br")
    gfb = st.sb("gfb", [128, D], F32)
    st.load("sync", gfb[:], bcast_rows(T["g_ffn"], 128), "gfb")

    xt = [st.sb(f"xt{i}", [128, D], F32) for i in range(2)]
    on = [st.sb(f"on{i}", [128, 512], BF16) for i in range(2)]
    og = [st.sb(f"og{i}", [128, 512], BF16) for i in range(2)]
    sgm = [st.sb(f"sgm{i}", [128, 2048], BF16) for i in range(2)]
    onT = [st.sb(f"onT{i}", [128, 8, 128], BF16) for i in range(2)]
    m1 = st.sb("m1", [128, D], F32)
    m2 = st.sb("m2", [128, D], F32)
    mx_ = st.sb("mixed", [128, D], BF16)
    mT = st.sb("mT", [128, 8, 128], BF16)
    sq = [st.sb(f"sq{i}", [128, D], BF16) for i in range(2)]
    ss = [st.sb(f"ss{i}", [128, 1], F32) for i in range(2)]
    rs = [st.sb(f"rs{i}", [128, 1], F32) for i in range(2)]
    vnb = st.sb("vnb", [128, D], BF16)
    combb = st.sb("combb", [128, 32], BF16)
    lg = st.sb("lg", [128, 36], F32)
    sm = st.sb("sm", [128, 8], F32)
    bg = st.sb("bg", [128, 4], F32)
    lem = st.sb("lem", [128, 32], F32)
    top8 = st.sb("top8", [128, 8], F32)
    msk = st.sb("msk", [128, 32], F32)
    ex = st.sb("ex", [128, 32], F32)
    comb = st.sb("comb", [128, 32], F32)
    eg = st.sb("eg", [128, 4], F32)

    pT = [st.ps(f"pT{i}", [128, 8, 128], BF16) for i in range(2)]
    py = [st.ps(f"py{i}", [128, 512], F32) for i in range(4)]

    for t in range(NTH):
        i = t % 2
        rows = slice(t * 128, (t + 1) * 128)
        st.load("sync", xt[i][:], T["xh"][rows, :], f"xt{i}")
        st.load("sync", sgm[i][:], scr["SIGM"][rows, :], f"sgm{i}")
        st.dma_percore(ncores, lambda e, k, i=i, t=t: e.dma_start(out=on[i][:], in_=scr["ONSA"][(k % 2) * TH + t * 128:(k % 2) * TH + (t + 1) * 128, :]),
                       f"on{i}", writes=[f"on{i}"])
        st.dma_percore(ncores, lambda e, k, i=i, t=t: e.dma_start(out=og[i][:], in_=scr["OGLA"][(k % 2) * TH + t * 128:(k % 2) * TH + (t + 1) * 128, :]),
                       f"og{i}", writes=[f"og{i}"])
        for j in range(4):
            st.do("tensor", "transpose", out=pT[0][:, j, :], in_=on[i][:, j * 128:(j + 1) * 128], identity=identb[:],
                  reads=[f"on{i}", "identb"], writes=["pT0"])
        for j in range(4):
            st.do("tensor", "transpose", out=pT[0][:, 4 + j, :], in_=og[i][:, j * 128:(j + 1) * 128], identity=identb[:],
                  reads=[f"og{i}", "identb"], writes=["pT0"])
        st.do("scalar", "copy", out=onT[i][:], in_=pT[0][:], reads=["pT0"], writes=[f"onT{i}"])
        for hf in range(2):
            cs_ = slice(hf * 512, (hf + 1) * 512)
            for j in range(4):
                st.do("tensor", "matmul", py[hf][:], onT[i][:, j, :], Wpn[:, j, cs_], start=(j == 0), stop=(j == 3),
                      reads=[f"onT{i}", "Wpn"], writes=[f"py{hf}"])
            for j in range(4):
                st.do("tensor", "matmul", py[2 + hf][:], onT[i][:, 4 + j, :], Wpg[:, j, cs_], start=(j == 0), stop=(j == 3),
                      reads=[f"onT{i}", "Wpg"], writes=[f"py{2 + hf}"])
            st.do("vector", "tensor_tensor", out=m1[:, cs_], in0=py[hf][:], in1=sgm[i][:, hf * 512:(hf + 1) * 512], op=ALU.mult,
                  reads=[f"py{hf}", f"sgm{i}"], writes=[("m1", hf)])
            st.do("vector", "tensor_tensor", out=m2[:, cs_], in0=py[2 + hf][:], in1=sgm[i][:, 1024 + hf * 512:1024 + (hf + 1) * 512], op=ALU.mult,
                  reads=[f"py{2 + hf}", f"sgm{i}"], writes=[("m2", hf)])
            st.do("gpsimd", "tensor_tensor", out=mx_[:, cs_], in0=m1[:, cs_], in1=m2[:, cs_], op=ALU.add,
                  reads=[("m1", hf), ("m2", hf)], writes=[("mixed", hf)])
        for kt in range(8):
            st.do("tensor", "transpose", out=pT[1][:, kt, :], in_=mx_[:, kt * 128:(kt + 1) * 128], identity=identb[:],
                  reads=[("mixed", kt // 4), "identb"], writes=["pT1"])
        st.do("scalar", "copy", out=mT[:], in_=pT[1][:], reads=["pT1"], writes=["mT"])
        for hf in range(2):
            cs_ = slice(hf * 512, (hf + 1) * 512)
            for kt in range(8):
                st.do("tensor", "matmul", py[hf][:], mT[:, kt, :], Wo[:, kt, cs_], start=(kt == 0), stop=(kt == 7),
                      reads=["mT", "Wo"], writes=[f"py{hf}"])
            st.do("vector", "tensor_tensor", out=h1[:, t, cs_], in0=py[hf][:], in1=xt[i][:, cs_], op=ALU.add,
                  reads=[f"py{hf}", f"xt{i}"], writes=[("h1", t, hf)])
        st.do("scalar", "activation", out=sq[i][:], in_=h1[:, t, :], func=AF.Square, accum_out=ss[i][:],
              reads=[("h1", t, 0), ("h1", t, 1)], writes=[f"sq{i}", f"ss{i}"])
        st.do("vector", "tensor_scalar", out=rs[i][:], in0=ss[i][:], scalar1=1.0 / D, scalar2=1e-6, op0=ALU.mult, op1=ALU.add,
              reads=[f"ss{i}"], writes=[f"rs{i}"])
        st.do("scalar", "activation", out=rs[i][:], in_=rs[i][:], func=AF.Sqrt, reads=[f"rs{i}"], writes=[f"rs{i}"])
        st.do("vector", "reciprocal", out=rs[i][:], in_=rs[i][:], reads=[f"rs{i}"], writes=[f"rs{i}"])
        st.do("vector", "scalar_tensor_tensor", out=vnb[:], in0=h1[:, t, :], scalar=rs[i][:, 0:1], in1=gfb[:], op0=ALU.mult, op1=ALU.mult,
              reads=[("h1", t, 0), ("h1", t, 1), f"rs{i}", "gfb"], writes=["vn"])
        for kt in range(8):
            st.do("tensor", "transpose", out=pT[0][:, kt, :], in_=vnb[:, kt * 128:(kt + 1) * 128], identity=identb[:],
                  reads=["vn", "identb"], writes=["pT0"])
        st.do("scalar", "copy", out=vT[:, :, rows], in_=pT[0][:], reads=["pT0"], writes=[("vT", t, 0), ("vT", t, 1)])
        for kt in range(8):
            st.do("tensor", "matmul", py[2][:, 0:36], vT[:, kt, rows], Wr[:, kt, :], start=(kt == 0), stop=(kt == 7),
                  reads=[("vT", t, 0), ("vT", t, 1), "Wr"], writes=["py2"])
        st.do("vector", "tensor_tensor", out=lg[:], in0=py[2][:, 0:36], in1=br[:], op=ALU.add, reads=["py2", "br"], writes=["lg"])
        st.do("vector", "reduce_max", out=sm[:, 0:1], in_=lg[:, 0:4], axis=AX.X, reads=["lg"], writes=["sm0"])
        st.do("vector", "tensor_scalar", out=bg[:], in0=lg[:, 0:4], scalar1=sm[:, 0:1], scalar2=-NEG, op0=ALU.is_ge, op1=ALU.mult,
              reads=["lg", "sm0"], writes=["bg"])
        st.do("vector", "tensor_scalar", out=bg[:], in0=bg[:], scalar1=NEG, scalar2=None, op0=ALU.add, reads=["bg"], writes=["bg"])
        st.do("vector", "tensor_scalar", out=sm[:, 1:2], in0=sm[:, 0:1], scalar1=-1.0, scalar2=None, op0=ALU.mult, reads=["sm0"], writes=["sm1"])
        st.do("scalar", "activation", out=eg[:], in_=lg[:, 0:4], func=AF.Exp, bias=sm[:, 1:2], accum_out=sm[:, 2:3],
              reads=["lg", "sm1"], writes=["eg", "sm2"])
        st.do("vector", "tensor_tensor", out=lem[:].rearrange("p (g e) -> p g e", e=8), in0=lg[:, 4:36].rearrange("p (g e) -> p g e", e=8),
              in1=bg[:].unsqueeze(2).broadcast_to([128, 4, 8]), op=ALU.add, reads=["lg", "bg"], writes=["lem"])
        st.do("vector", "max", out=top8[:], in_=lem[:], reads=["lem"], writes=["top8"])
        st.do("vector", "tensor_scalar", out=msk[:], in0=lem[:], scalar1=top8[:, 1:2], scalar2=None, op0=ALU.is_ge, reads=["lem", "top8"], writes=["msk"])
        st.do("vector", "tensor_scalar", out=sm[:, 3:4], in0=top8[:, 0:1], scalar1=-1.0, scalar2=None, op0=ALU.mult, reads=["top8"], writes=["sm3"])
        st.do("scalar", "activation", out=ex[:], in_=lem[:], func=AF.Exp, bias=sm[:, 3:4], reads=["lem", "sm3"], writes=["ex"])
        st.do("vector", "tensor_tensor", out=ex[:], in0=ex[:], in1=msk[:], op=ALU.mult, reads=["ex", "msk"], writes=["ex"])
        st.do("vector", "reduce_sum", out=sm[:, 4:5], in_=ex[:], axis=AX.X, reads=["ex"], writes=["sm4"])
        st.do("vector", "tensor_tensor", out=sm[:, 5:6], in0=sm[:, 4:5], in1=sm[:, 2:3], op=ALU.mult, reads=["sm4", "sm2"], writes=["sm5"])
        st.do("vector", "reciprocal", out=sm[:, 5:6], in_=sm[:, 5:6], reads=["sm5"], writes=["sm5"])
        st.do("vector", "tensor_scalar", out=comb[:], in0=ex[:], scalar1=sm[:, 5:6], scalar2=None, op0=ALU.mult, reads=["ex", "sm5"], writes=["comb"])
        st.do("vector", "tensor_copy", out=combb[:], in_=comb[:], reads=["comb"], writes=["combb"])
        st.do("tensor", "transpose", out=pT[1][0:32, 0, :], in_=combb[:], identity=identb[:], reads=["combb", "identb"], writes=["pT1"])
        st.do("scalar", "copy", out=combT[:, rows], in_=pT[1][0:32, 0, :], reads=["pT1"], writes=[("combT", t)])
    st.emit()


def stage_E2(nc, S, T, C, scr, P, out):
    st = Stage(nc, "E2")
    TH = S // 2
    NTH = TH // 128
    CH = min(512, TH)
    NCH = TH // CH
    TPC = CH // 128
    h1, vT, combT = P["h1"], P["vT"], P["combT"]
    identb = st.sb("identb", [128, 128], BF16)
    st.dma("gpsimd", lambda e: e.dma_start(out=identb[:], in_=C["ident"]), "identb", writes=["identb"])
    selall = st.sb("selall", [32, 32, 128], BF16)
    st.do("vector", "tensor_copy", out=selall[:], in_=identb[0:32, 0:32].unsqueeze(2).broadcast_to([32, 32, 128]),
          reads=["identb"], writes=["selall"])
    Wg = [st.sb(f"Wg{i}", [128, 8, 512], BF16) for i in range(2)]
    Wu = [st.sb(f"Wu{i}", [128, 8, 512], BF16) for i in range(2)]
    Wd = [st.sb(f"Wd{i}", [128, 4, D], BF16) for i in range(2)]
    cB = [st.sb(f"cB{i}", [128, CH], BF16) for i in range(2)]
    sgl = [st.sb(f"sgl{i}", [128, CH], BF16) for i in range(2)]
    tu = [st.sb(f"tu{i}", [128, CH], BF16) for i in range(2)]
    hT = [st.sb(f"hT{i}", [128, 4, CH], BF16) for i in range(2)]
    pcb = st.ps("pcb", [128, 512], F32)
    pG = [st.ps(f"pG{i}", [128, 512], F32) for i in range(2)]
    pU = [st.ps(f"pU{i}", [128, 512], F32) for i in range(2)]
    py = [st.ps(f"py{i}", [128, 512], F32) for i in range(2)]
    gu = 0
    hb = 0
    def load_expert(ex_):
        w = ex_ % 2
        st.dma("gpsimd", lambda e, w=w, ex_=ex_: e.dma_start(out=Wg[w][:], in_=T["w_gate"][0, ex_].rearrange("(kt p) f -> p kt f", p=128)),
               f"Wg{w}", writes=[f"Wg{w}"])
        st.dma("gpsimd", lambda e, w=w, ex_=ex_: e.dma_start(out=Wu[w][:], in_=T["w_up"][0, ex_].rearrange("(kt p) f -> p kt f", p=128)),
               f"Wu{w}", writes=[f"Wu{w}"])
        st.dma("gpsimd", lambda e, w=w, ex_=ex_: e.dma_start(out=Wd[w][:], in_=T["w_down"][0, ex_].rearrange("(fc p) d -> p fc d", p=128)),
               f"Wd{w}", writes=[f"Wd{w}"])

    load_expert(0)
    for ex_ in range(32):
        w = ex_ % 2
        if ex_ + 1 < 32:
            load_expert(ex_ + 1)
        for tc in range(NCH):
            tsl = slice(tc * CH, (tc + 1) * CH)
            cbi = hb % 2
            hb += 1
            ckeys = [("combT", tc * TPC + j) for j in range(TPC)]
            vkeys = [("vT", tc * TPC + j, q) for j in range(TPC) for q in range(2)]
            st.do("tensor", "matmul", pcb[:, 0:CH], selall[:, ex_, :], combT[:, tsl], start=True, stop=True,
                  reads=["selall"] + ckeys, writes=["pcb"])
            st.do("scalar", "copy", out=cB[cbi][:], in_=pcb[:, 0:CH], reads=["pcb"], writes=[f"cB{cbi}"])
            for fc in range(4):
                g = gu % 2
                gu += 1
                fsl = slice(fc * 128, (fc + 1) * 128)
                for kt in range(8):
                    st.do("tensor", "matmul", pG[g][:, 0:CH], Wg[w][:, kt, fsl], vT[:, kt, tsl], start=(kt == 0), stop=(kt == 7),
                          reads=[f"Wg{w}"] + vkeys, writes=[f"pG{g}"])
                for kt in range(8):
                    st.do("tensor", "matmul", pU[g][:, 0:CH], Wu[w][:, kt, fsl], vT[:, kt, tsl], start=(kt == 0), stop=(kt == 7),
                          reads=[f"Wu{w}"] + vkeys, writes=[f"pU{g}"])
                st.do("scalar", "activation", out=sgl[g][:], in_=pG[g][:, 0:CH], func=AF.Silu, reads=[f"pG{g}"], writes=[f"sgl{g}"])
                st.do("vector", "tensor_tensor", out=tu[g][:], in0=pU[g][:, 0:CH], in1=cB[cbi][:], op=ALU.mult,
                      reads=[f"pU{g}", f"cB{cbi}"], writes=[f"tu{g}"])
                st.do("gpsimd", "tensor_tensor", out=hT[cbi][:, fc, :], in0=sgl[g][:], in1=tu[g][:], op=ALU.mult,
                      reads=[f"sgl{g}", f"tu{g}"], writes=[(f"hT{cbi}", fc)])
            for tt in range(TPC):
                t = tc * TPC + tt
                for hf in range(2):
                    cs_ = slice(hf * 512, (hf + 1) * 512)
                    for fc in range(4):
                        st.do("tensor", "matmul", py[hf][:], hT[cbi][:, fc, tt * 128:(tt + 1) * 128], Wd[w][:, fc, cs_],
                              start=(fc == 0), stop=(fc == 3), reads=[(f"hT{cbi}", f) for f in range(4)] + [f"Wd{w}"], writes=[f"py{hf}"])
                    st.do("vector", "tensor_tensor", out=h1[:, t, cs_], in0=py[hf][:], in1=h1[:, t, cs_], op=ALU.add,
                          reads=[f"py{hf}", ("h1", t, hf)], writes=[("h1", t, hf)])
    gfin = st.sb("gfin", [128, D], F32)
    st.load("sync", gfin[:], bcast_rows(T["g_final"], 128), "gfin")
    sq = [st.sb(f"sq{i}", [128, D], BF16) for i in range(2)]
    ss = [st.sb(f"ss{i}", [128, 1], F32) for i in range(2)]
    rs = [st.sb(f"rs{i}", [128, 1], F32) for i in range(2)]
    ot = [st.sb(f"ot{i}", [128, D], F32) for i in range(2)]
    for t in range(NTH):
        i = t % 2
        st.do("scalar", "activation", out=sq[i][:], in_=h1[:, t, :], func=AF.Square, accum_out=ss[i][:],
              reads=[("h1", t, 0), ("h1", t, 1)], writes=[f"sq{i}", f"ss{i}"])
        st.do("vector", "tensor_scalar", out=rs[i][:], in0=ss[i][:], scalar1=1.0 / D, scalar2=1e-6, op0=ALU.mult, op1=ALU.add,
              reads=[f"ss{i}"], writes=[f"rs{i}"])
        st.do("scalar", "activation", out=rs[i][:], in_=rs[i][:], func=AF.Sqrt, reads=[f"rs{i}"], writes=[f"rs{i}"])
        st.do("vector", "reciprocal", out=rs[i][:], in_=rs[i][:], reads=[f"rs{i}"], writes=[f"rs{i}"])
        st.do("vector", "scalar_tensor_tensor", out=ot[i][:], in0=h1[:, t, :], scalar=rs[i][:, 0:1], in1=gfin[:], op0=ALU.mult, op1=ALU.mult,
              reads=[("h1", t, 0), ("h1", t, 1), f"rs{i}", "gfin"], writes=[f"ot{i}"])
        st.store("sync", out[t * 128:(t + 1) * 128, :], ot[i][:], f"ot{i}")
    st.emit()


def build_full(S, ncores, stages="RABCDE", debug=(), scr_in=()):
    nc = bass.Bass("TRN2", target_bir_lowering=False)
    T = {k: nc.dram_tensor(k, shp, dt, kind="ExternalInput").ap() for k, (shp, dt) in IN_SHAPES(S).items()}
    C = {k: nc.dram_tensor("c_" + k, shp, F32, kind="ExternalInput").ap() for k, shp in CONST_SHAPES.items()}
    scr = {}
    spec = dict(SCR_SPEC(S))
    spec["SIGM"] = ([S // 2, 2048], BF16)
    for k, (shp, dt) in spec.items():
        kind = "ExternalOutput" if k in debug else ("ExternalInput" if k in scr_in else "Internal")
        scr[k] = nc.dram_tensor("scr_" + k, shp, dt, kind=kind).ap()
    out = nc.dram_tensor("out", [S // 2, D], F32, kind="ExternalOutput").ap()
    TH = S // 2
    with nc.allow_non_contiguous_dma(reason="small strided parameter loads"), ExitStack() as es:
        if "R" in stages:
            stage_R(nc, S, T, C, scr)
        if "A" in stages:
            stage_A(nc, S, T, C, scr)
        if "B" in stages:
            stage_B(nc, S, T, C, scr)
        if "C" in stages:
            stage_C(nc, S, T, C, scr)
        if "D" in stages:
            stage_D(nc, S, T, C, scr)
        if "E" in stages:
            if "0" in DBG_E:
                stage_E0(nc, S, T, C, scr)
            P = {"h1": es.enter_context(nc.sbuf_tensor("P_h1", [128, TH // 128, D], F32)),
                 "vT": es.enter_context(nc.sbuf_tensor("P_vT", [128, 8, TH], BF16)),
                 "combT": es.enter_context(nc.sbuf_tensor("P_combT", [32, TH], BF16))}
            if "1" in DBG_E:
                stage_E1(nc, S, T, C, scr, P, ncores)
            if "2" in DBG_E:
                stage_E2(nc, S, T, C, scr, P, out)
    return nc


SEQ = 4096
BATCH = 4
_CACHE = {}


def kernel(**inputs):
    S = SEQ
    ncores = 8
    if "nc" not in _CACHE:
        _CACHE["nc"] = build_full(S, ncores)
    nc = _CACHE["nc"]
    consts = host_consts(S)
    shapes = IN_SHAPES(S)
    x = np.ascontiguousarray(np.asarray(inputs["x"], dtype=np.float32))
    pos = np.ascontiguousarray(np.asarray(inputs["positions"]).astype(np.int32))
    in_maps = []
    for core in range(ncores):
        b, half = core // 2, core % 2
        m = {}
        for k, (shp, dt) in shapes.items():
            if k == "x":
                m[k] = x[b]
            elif k == "xh":
                m[k] = np.ascontiguousarray(x[b, half * S // 2:(half + 1) * S // 2])
            elif k == "positions":
                m[k] = pos[b:b + 1]
            elif k == "halfidx":
                m[k] = np.array([[half]], np.int32)
            elif k == "g_final":
                m[k] = np.asarray(inputs[k], dtype=np.float32).reshape(1, -1)
            else:
                m[k] = np.ascontiguousarray(np.asarray(inputs[k], dtype=np.float32))
        for k, v in consts.items():
            m["c_" + k] = v
        in_maps.append(m)
    res = run_bass_kernel_spmd(nc, in_maps, core_ids=list(range(ncores)))
    out = np.empty((BATCH, S, D), np.float32)
    for core in range(ncores):
        b, half = core // 2, core % 2
        out[b, half * S // 2:(half + 1) * S // 2] = res.results[core]["out"]
    return out
```

```python
import math
import numpy as np
from contextlib import ExitStack
import concourse.bass as bass
import concourse.mybir as mybir
from concourse.bass_utils import run_bass_kernel_spmd

F32 = mybir.dt.float32
BF16 = mybir.dt.bfloat16
I32 = mybir.dt.int32
AF = mybir.ActivationFunctionType
ALU = mybir.AluOpType
AX = mybir.AxisListType

SAME_ENGINE_SYNC = True
EPOCH = 12000
NEG = -30000.0
DBG_BR = None
DBG_D = 3
DBG_NOIF = False
DBG_E = '012'
D = 1024


class Stage:
    ENGS = ("tensor", "vector", "scalar", "gpsimd", "sync")

    def __init__(self, nc, name):
        self.nc = nc
        self.name = name
        self.es = ExitStack()
        self.ops = {e: [] for e in self.ENGS}
        self.last_write = {}
        self.readers = {}
        self.seen = {e: {} for e in self.ENGS}
        self.sems = {}
        self.cnt = {}
        self.epoch = {e: 0 for e in self.ENGS}
        self.dma_sems_by_eng = {e: set() for e in self.ENGS}
        self.nalloc = 0

    def sb(self, name, shape, dtype):
        return self.es.enter_context(self.nc.sbuf_tensor(f"{self.name}_{name}", list(shape), dtype))

    def ps(self, name, shape, dtype=F32):
        return self.es.enter_context(self.nc.psum_tensor(f"{self.name}_{name}", list(shape), dtype))

    def _sem(self, semkey):
        if semkey not in self.sems:
            self.nalloc += 1
            self.sems[semkey] = self.nc.alloc_semaphore(name=f"{self.name}_s{self.nalloc}")
            self.cnt[semkey] = 0
        return self.sems[semkey]

    def _deps(self, eng, reads, writes):
        need = {}

        def want(c):
            if c is None:
                return
            sk, v = c
            if need.get(sk, 0) < v:
                need[sk] = v

        for r in reads:
            want(self.last_write.get(r))
        for w in writes:
            want(self.last_write.get(w))
            for c in self.readers.get(w, ()):
                want(c)
        waits = []
        for sk, v in need.items():
            if (not SAME_ENGINE_SYNC or eng == "tensor") and sk[0] == "E" and sk[1] == eng:
                continue
            if self.seen[eng].get(sk, 0) >= v:
                continue
            self.seen[eng][sk] = v
            waits.append((sk, v))
        return waits

    def _commit(self, comp, reads, writes):
        for w in writes:
            self.last_write[w] = comp
            self.readers[w] = []
        for r in reads:
            if r in writes:
                continue
            self.readers.setdefault(r, []).append(comp)

    def op(self, eng, fn, reads=(), writes=()):
        reads = list(reads)
        writes = list(writes)
        waits = self._deps(eng, reads, writes)
        sk = ("E", eng, self.epoch[eng])
        self._sem(sk)
        self.cnt[sk] += 1
        comp = (sk, self.cnt[sk])
        if self.cnt[sk] >= EPOCH:
            self.epoch[eng] += 1
        self.ops[eng].append((fn, waits, sk, 1))
        self._commit(comp, reads, writes)
        return comp

    def dma(self, eng, fn, semkey, reads=(), writes=()):
        reads = list(reads)
        writes = list(writes)
        waits = self._deps(eng, reads, writes)
        sk = ("D", semkey)
        self._sem(sk)
        self.cnt[sk] += 16
        comp = (sk, self.cnt[sk])
        self.ops[eng].append((fn, waits, sk, 16))
        self.dma_sems_by_eng[eng].add(sk)
        self._commit(comp, reads, writes)
        return comp

    def do(self, eng, method, *args, reads=(), writes=(), **kw):
        return self.op(eng, lambda e: getattr(e, method)(*args, **kw), reads=reads, writes=writes)

    def dma_percore(self, ncores, mk, semkey, reads=(), writes=()):
        if DBG_NOIF:
            return self.dma("gpsimd", lambda e: mk(e, 0), semkey, reads=reads, writes=writes)
        sk = ("D", semkey)
        self._sem(sk)

        class _Done:
            def then_inc(self_, *a):
                return self_

        def fn(e):
            if getattr(self, "_pid", None) is None:
                self._pid = e.partition_id()
            pid = self._pid
            for k in range(ncores):
                with e.If(pid == k):
                    mk(e, k).then_inc(self.sems[sk], 16)
            return _Done()

        return self.dma("gpsimd", fn, semkey, reads=reads, writes=writes)

    def load(self, eng, out_ap, in_ap, key):
        return self.dma(eng, lambda e: e.dma_start(out=out_ap, in_=in_ap), key, writes=[key])

    def store(self, eng, out_ap, in_ap, key):
        return self.dma(eng, lambda e: e.dma_start(out=out_ap, in_=in_ap), key, reads=[key])

    def emit(self):
        nc = self.nc
        with nc.Block() as block:
            for ename in self.ENGS:
                ops = self.ops[ename]
                if not ops:
                    continue
                final = []
                for sk in self.dma_sems_by_eng[ename]:
                    final.append((sk, self.cnt[sk]))
                for ep in range(self.epoch[ename] + 1):
                    sk = ("E", ename, ep)
                    if sk in self.cnt and self.cnt[sk] > 0:
                        final.append((sk, self.cnt[sk]))

                def body(e, ops=ops, final=final):
                    for fn, waits, sk, inc in ops:
                        for wk, wv in waits:
                            e.wait_ge(self.sems[wk], wv)
                        fn(e).then_inc(self.sems[sk], inc)
                    for wk, wv in final:
                        e.wait_ge(self.sems[wk], wv)

                getattr(block, ename)(body)
        self.nc.clear_and_free_semaphores(list(self.sems.values()))
        self.nc.all_engine_barrier()
        self.es.close()


def host_consts(S):
    c = {}
    c["ident"] = np.eye(128, dtype=np.float32)
    half = 32
    inv = 1.0 / (10000.0 ** (np.arange(half, dtype=np.float32) / half))
    inv64 = np.concatenate([inv, inv]).astype(np.float32)
    c["invs"] = (np.concatenate([inv64, inv64]) / np.float32(2 * math.pi)).astype(np.float32).reshape(128, 1)
    tl = np.arange(128)[:, None]
    npr = np.arange(-1, 7)[None, :]
    c["cmpbias"] = np.where(16 * npr + 31 <= tl, 0.0, NEG).astype(np.float32)
    kk = np.arange(128)[None, :]
    c["tri"] = np.where(kk <= tl, 0.0, NEG).astype(np.float32)
    kw = np.arange(640)[None, :]
    c["winbias"] = np.where((kw > tl) & (kw <= tl + 512), 0.0, NEG).astype(np.float32)
    n = np.arange(256)[:, None]
    j = np.arange(64)[None, :]
    ov = np.clip(np.minimum(16 * n + 32, 64 * j + 64) - np.maximum(16 * n, 64 * j), 0, None) / 32.0
    ov[255] = 0.0
    c["overlap"] = ov.astype(np.float32)
    K = np.zeros((128, 3), np.float32)
    B = np.zeros((128, 3), np.float32)
    lo = np.arange(128) < 64
    K[lo] = [0, 0, 0]
    B[lo] = [1e4, 1e4, -1e30]
    K[~lo] = [1, 0, 0]
    B[~lo] = [0, 1e4, 1e4]
    c["selK"] = K
    c["selB"] = B
    c["rowvalid"] = (np.arange(128) >= 31).astype(np.float32).reshape(128, 1)
    s_ = np.arange(64)[:, None]
    t_ = np.arange(64)[None, :]
    c["glamask"] = (s_ <= t_).astype(np.float32)
    return c


CONST_SHAPES = {"ident": [128, 128], "invs": [128, 1], "cmpbias": [128, 8], "tri": [128, 128],
                "winbias": [128, 640], "overlap": [256, 64], "selK": [128, 3], "selB": [128, 3],
                "rowvalid": [128, 1], "glamask": [64, 64]}

WA_COLS = 3624
SRC = dict(q=0, kc=512, vc=640, ks=768, vs=896, kw=1024, vw=1152, gates=1280, gq=1304, gk=1560,
           gv=1816, al=2328, gr=2344, ma=2856, mb=3880)
DST = dict(q=0, qR=512, ks=1024, ksR=1152, kw=1280, kwR=1408, kc=1536, vc=1664, gq=1792, gk=2048,
           al=2304, vs=2320, vw=2448, gates=2576, gv=2600, gr=3112)


def bcast_rows(ap_row, nparts):
    return ap_row.broadcast_to([nparts, ap_row.shape[-1]])


def stage_R(nc, S, T, C, scr):
    st = Stage(nc, "R")
    pos = T["positions"]
    invs = st.sb("invs", [64, 1], F32)
    cosT = st.sb("cosT", [64, S], F32)
    sinT = st.sb("sinT", [64, S], F32)
    posi = st.sb("posi", [64, S], I32)
    tq = st.sb("tq", [64, S], F32)
    t2 = st.sb("t2", [64, S], F32)
    t3 = st.sb("t3", [64, S], F32)
    ni = st.sb("ni", [64, S], I32)
    st.load("sync", invs[:], C["invs"][0:64, :], "invs")
    st.load("sync", posi[:], bcast_rows(pos, 64), "posi")
    st.op("vector", lambda e: e.tensor_copy(out=tq[:], in_=posi[:]), reads=["posi"], writes=["tq"])
    st.op("vector", lambda e: e.tensor_scalar(out=tq[:], in0=tq[:], scalar1=invs[:, 0:1], scalar2=None, op0=ALU.mult),
          reads=["tq", "invs"], writes=["tq"])

    def table(dst, shift):
        st.op("vector", lambda e: e.tensor_scalar(out=t2[:], in0=tq[:], scalar1=float(shift), scalar2=None, op0=ALU.add),
              reads=["tq"], writes=["t2"])
        st.op("vector", lambda e: e.tensor_copy(out=ni[:], in_=t2[:]), reads=["t2"], writes=["ni"])
        st.op("vector", lambda e: e.tensor_copy(out=t3[:], in_=ni[:]), reads=["ni"], writes=["t3"])
        st.op("vector", lambda e: e.tensor_tensor(out=t2[:], in0=t2[:], in1=t3[:], op=ALU.subtract),
              reads=["t2", "t3"], writes=["t2"])
        st.op("vector", lambda e: e.tensor_scalar(out=t3[:], in0=t2[:], scalar1=0.5, scalar2=None, op0=ALU.is_gt),
              reads=["t2"], writes=["t3"])
        st.op("vector", lambda e: e.tensor_tensor(out=t2[:], in0=t2[:], in1=t3[:], op=ALU.subtract),
              reads=["t2", "t3"], writes=["t2"])
        st.op("vector", lambda e: e.tensor_scalar(out=t3[:], in0=t2[:], scalar1=-0.5, scalar2=None, op0=ALU.is_lt),
              reads=["t2"], writes=["t3"])
        st.op("vector", lambda e: e.tensor_tensor(out=t2[:], in0=t2[:], in1=t3[:], op=ALU.add),
              reads=["t2", "t3"], writes=["t2"])
        st.op("scalar", lambda e: e.activation(out=dst[:], in_=t2[:], func=AF.Sin, scale=6.283185),
              reads=["t2"], writes=[dst.name])

    table(sinT, 0.0)
    table(cosT, 0.25)
    st.store("sync", scr["SIN"], sinT[:], sinT.name)
    st.store("sync", scr["COS"], cosT[:], cosT.name)
    st.emit()


def stage_A(nc, S, T, C, scr):
    st = Stage(nc, "A")
    SH = S // 2
    NT = SH // 128
    NC4 = SH // 512
    x, w_in = T["x"], T["w_in"]
    uT = st.sb("uT", [128, 8, SH], BF16)
    WA = st.sb("WA", [128, 8, WA_COLS], BF16)
    ident = st.sb("ident", [128, 128], BF16)
    gm = st.sb("gm", [128, 8], F32)
    cosT = st.sb("cosT", [128, SH], F32)
    sinT = st.sb("sinT", [128, SH], F32)
    st.dma("gpsimd", lambda e: e.dma_start(out=ident[:], in_=C["ident"]), "ident", writes=["ident"])
    st.dma("sync", lambda e: e.dma_start(out=gm[:], in_=T["g_mix"].rearrange("o (kt p) -> p (o kt)", p=128)),
           "gm", writes=["gm"])

    wst = [st.sb(f"wst{i}", [128, 2856], F32) for i in range(2)]
    WASUB = {}
    for kt in range(8):
        i = kt % 2
        wk = f"wst{i}"
        st.load("sync" if kt % 2 == 0 else "gpsimd", wst[i][:], w_in[0, kt * 128:(kt + 1) * 128, 0:2856], wk)
        g = gm[:, kt:kt + 1]
        wkey = ("WA", kt)

        def cp(eng, dst0, src0, n, mul=1.0, i=i, kt=kt, g=g, wk=wk, wkey=wkey):
            wkey = ("WA", kt, dst0)
            WASUB.setdefault(kt, []).append(wkey)
            st.op(eng, lambda e: e.tensor_scalar(out=WA[:, kt, dst0:dst0 + n], in0=wst[i][:, src0:src0 + n],
                                                 scalar1=g, scalar2=float(mul), op0=ALU.mult, op1=ALU.mult),
                  reads=[wk, "gm"], writes=[wkey])

        def rot(eng, dst0, src0, nh, mul=1.0, i=i, kt=kt, g=g, wk=wk, wkey=wkey):
            wkey = ("WA", kt, dst0)
            WASUB.setdefault(kt, []).append(wkey)
            s4 = wst[i][:, src0:src0 + nh * 64].rearrange("p (h two j) -> p h two j", two=2, j=32)
            d4 = WA[:, kt, dst0:dst0 + nh * 64].rearrange("p (h two j) -> p h two j", two=2, j=32)
            st.op(eng, lambda e: e.tensor_scalar(out=d4[:, :, 0, :], in0=s4[:, :, 1, :], scalar1=g, scalar2=float(-mul),
                                                 op0=ALU.mult, op1=ALU.mult), reads=[wk, "gm"], writes=[wkey])
            st.op(eng, lambda e: e.tensor_scalar(out=d4[:, :, 1, :], in0=s4[:, :, 0, :], scalar1=g, scalar2=float(mul),
                                                 op0=ALU.mult, op1=ALU.mult), reads=[wk, "gm"], writes=[wkey])

        cp("vector", DST["q"], SRC["q"], 512, 0.125)
        rot("gpsimd", DST["qR"], SRC["q"], 8, 0.125)
        cp("vector", DST["ks"], SRC["ks"], 128)
        rot("gpsimd", DST["ksR"], SRC["ks"], 2)
        cp("vector", DST["kw"], SRC["kw"], 128)
        rot("gpsimd", DST["kwR"], SRC["kw"], 2)
        cp("vector", DST["kc"], SRC["kc"], 256)
        cp("gpsimd", DST["gq"], SRC["gq"], 256, 0.125)
        cp("gpsimd", DST["gk"], SRC["gk"], 256)
        cp("vector", DST["al"], SRC["al"], 16)
        cp("vector", DST["vs"], SRC["vs"], 128)
        cp("vector", DST["vw"], SRC["vw"], 128)
        cp("vector", DST["gates"], SRC["gates"], 24)
        cp("gpsimd", DST["gv"], SRC["gv"], 512)
        cp("vector", DST["gr"], SRC["gr"], 512)
    WAK = [("WA", kt) for kt in range(8)]

    xt = [st.sb(f"xt{i}", [128, D], F32) for i in range(2)]
    sq = [st.sb(f"sq{i}", [128, D], BF16) for i in range(2)]
    xn = [st.sb(f"xn{i}", [128, D], BF16) for i in range(2)]
    ss = [st.sb(f"ss{i}", [128, 1], F32) for i in range(2)]
    rs = [st.sb(f"rs{i}", [128, 1], F32) for i in range(2)]
    ptr = [st.ps(f"ptr{i}", [128, 8, 128], BF16) for i in range(2)]
    pa = [st.ps(f"pa{i}", [128, 512], F32) for i in range(2)]
    pb = [st.ps(f"pb{i}", [128, 512], F32) for i in range(2)]
    r1 = [st.sb(f"r1{i}", [128, 512], F32) for i in range(2)]
    r2 = [st.sb(f"r2{i}", [128, 512], F32) for i in range(2)]
    fo = [st.sb(f"fo{i}", [128, 512], BF16) for i in range(3)]
    pt = [st.ps(f"pt{i}", [128, 512], F32) for i in range(2)]
    tA = [st.sb(f"tA{i}", [128, 256], BF16) for i in range(2)]
    tG = [st.sb(f"tG{i}", [128, 24], F32) for i in range(2)]
    tV = [st.sb(f"tV{i}", [128, 512], BF16) for i in range(2)]
    tR = [st.sb(f"tR{i}", [128, 512], BF16) for i in range(2)]
    for hs in range(2):
        H0 = hs * SH
        for hh in range(2):
            st.load("sync", cosT[hh * 64:(hh + 1) * 64, :], scr["COS"][:, H0:H0 + SH], cosT.name)
            st.load("sync", sinT[hh * 64:(hh + 1) * 64, :], scr["SIN"][:, H0:H0 + SH], sinT.name)
        for t in range(NT):
            i = t % 2
            st.load("sync", xt[i][:], x[H0 + t * 128:H0 + (t + 1) * 128, :], f"xt{i}")
            st.op("scalar", lambda e, i=i: e.activation(out=sq[i][:], in_=xt[i][:], func=AF.Square, accum_out=ss[i][:]),
                  reads=[f"xt{i}"], writes=[f"sq{i}", f"ss{i}"])
            st.op("vector", lambda e, i=i: e.tensor_scalar(out=rs[i][:], in0=ss[i][:], scalar1=1.0 / D, scalar2=1e-6,
                                                           op0=ALU.mult, op1=ALU.add), reads=[f"ss{i}"], writes=[f"rs{i}"])
            st.op("scalar", lambda e, i=i: e.activation(out=rs[i][:], in_=rs[i][:], func=AF.Sqrt),
                  reads=[f"rs{i}"], writes=[f"rs{i}"])
            st.op("vector", lambda e, i=i: e.reciprocal(out=rs[i][:], in_=rs[i][:]), reads=[f"rs{i}"], writes=[f"rs{i}"])
            st.op("vector", lambda e, i=i: e.tensor_scalar(out=xn[i][:], in0=xt[i][:], scalar1=rs[i][:, 0:1], scalar2=None,
                                                           op0=ALU.mult), reads=[f"xt{i}", f"rs{i}"], writes=[f"xn{i}"])
            for kt in range(8):
                st.op("tensor", lambda e, i=i, kt=kt: e.transpose(out=ptr[i][:, kt, :], in_=xn[i][:, kt * 128:(kt + 1) * 128],
                                                                  identity=ident[:]),
                      reads=[f"xn{i}", "ident"], writes=[f"ptr{i}"])
            st.op("scalar", lambda e, i=i, t=t: e.copy(out=uT[:, :, t * 128:(t + 1) * 128], in_=ptr[i][:]),
                  reads=[f"ptr{i}"], writes=[("uT", t)])

        rope_groups = [(DST["q"] + 128 * p, DST["qR"] + 128 * p, scr["QT"], 128 * p) for p in range(4)]
        rope_groups += [(DST["ks"], DST["ksR"], scr["KST"], 0), (DST["kw"], DST["kwR"], scr["KWT"], 0)]
        plain_groups = [(DST["kc"], 128, scr["KCT"], 0), (DST["vc"], 128, scr["VCT"], 0),
                        (DST["gq"], 128, scr["GQT"], 0), (DST["gq"] + 128, 128, scr["GQT"], 128),
                        (DST["gk"], 128, scr["GKT"], 0), (DST["gk"] + 128, 128, scr["GKT"], 128),
                        (DST["al"], 16, scr["ALT"], 0)]
        it = 0
        fi = 0
        for tc in range(NC4):
            tsl = slice(tc * 512, (tc + 1) * 512)
            ukeys = [("uT", tc * 4 + j) for j in range(4)]
            for (ca, cb, dst, row0) in rope_groups:
                i = it % 2
                it += 1
                f = fi % 3
                fi += 1
                for kt in range(8):
                    st.op("tensor", lambda e, i=i, kt=kt, ca=ca, tsl=tsl: e.matmul(pa[i][:], WA[:, kt, ca:ca + 128], uT[:, kt, tsl],
                                                                          start=(kt == 0), stop=(kt == 7)),
                          reads=ukeys + WASUB[kt], writes=[f"pa{i}"])
                for kt in range(8):
                    st.op("tensor", lambda e, i=i, kt=kt, cb=cb, tsl=tsl: e.matmul(pb[i][:], WA[:, kt, cb:cb + 128], uT[:, kt, tsl],
                                                                          start=(kt == 0), stop=(kt == 7)),
                          reads=ukeys + WASUB[kt], writes=[f"pb{i}"])
                st.op("vector", lambda e, i=i, tsl=tsl: e.tensor_tensor(out=r1[i][:], in0=pa[i][:], in1=cosT[:, tsl], op=ALU.mult),
                      reads=[f"pa{i}", cosT.name], writes=[f"r1{i}"])
                st.op("vector", lambda e, i=i, tsl=tsl: e.tensor_tensor(out=r2[i][:], in0=pb[i][:], in1=sinT[:, tsl], op=ALU.mult),
                      reads=[f"pb{i}", sinT.name], writes=[f"r2{i}"])
                st.op("gpsimd", lambda e, i=i, f=f: e.tensor_tensor(out=fo[f][:], in0=r1[i][:], in1=r2[i][:], op=ALU.add),
                      reads=[f"r1{i}", f"r2{i}"], writes=[f"fo{f}"])
                st.store("sync", dst[row0:row0 + 128, H0 + tc * 512:H0 + (tc + 1) * 512], fo[f][:], f"fo{f}")
            for (ca, n, dst, row0) in plain_groups:
                i = it % 2
                it += 1
                f = fi % 3
                fi += 1
                for kt in range(8):
                    st.op("tensor", lambda e, i=i, kt=kt, ca=ca, n=n, tsl=tsl: e.matmul(pa[i][0:n, :], WA[:, kt, ca:ca + n], uT[:, kt, tsl],
                                                                               start=(kt == 0), stop=(kt == 7)),
                          reads=ukeys + WASUB[kt], writes=[f"pa{i}"])
                st.op("scalar", lambda e, i=i, f=f, n=n: e.copy(out=fo[f][0:n, :], in_=pa[i][0:n, :]),
                      reads=[f"pa{i}"], writes=[f"fo{f}"])
                st.store("sync", dst[row0:row0 + n, H0 + tc * 512:H0 + (tc + 1) * 512], fo[f][0:n, :], f"fo{f}")

        it = 0
        for t in range(NT):
            rows = slice(t * 128, (t + 1) * 128)
            drows = slice(H0 + t * 128, H0 + (t + 1) * 128)
            b = t % 2
            for (c0, n, kind) in [(DST["vs"], 280, 0), (DST["gv"], 512, 1), (DST["gr"], 512, 2)]:
                i = it % 2
                it += 1
                for kt in range(8):
                    st.op("tensor", lambda e, i=i, kt=kt, c0=c0, n=n, rows=rows: e.matmul(pt[i][:, 0:n], uT[:, kt, rows], WA[:, kt, c0:c0 + n],
                                                                               start=(kt == 0), stop=(kt == 7)),
                          reads=[("uT", t)] + WASUB[kt], writes=[f"pt{i}"])
                if kind == 0:
                    st.op("vector", lambda e, i=i, b=b: e.tensor_copy(out=tA[b][:], in_=pt[i][:, 0:256]),
                          reads=[f"pt{i}"], writes=[f"tA{b}"])
                    st.op("scalar", lambda e, i=i, b=b: e.activation(out=tG[b][:], in_=pt[i][:, 256:280], func=AF.Sigmoid),
                          reads=[f"pt{i}"], writes=[f"tG{b}"])
                    st.store("gpsimd", scr["VS"][drows, :], tA[b][:, 0:128], f"tA{b}")
                    st.store("gpsimd", scr["VW"][drows, :], tA[b][:, 128:256], f"tA{b}")
                    st.store("gpsimd", scr["GATE"][drows, :], tG[b][:], f"tG{b}")
                elif kind == 1:
                    st.op("vector", lambda e, i=i, b=b: e.tensor_copy(out=tV[b][:], in_=pt[i][:]),
                          reads=[f"pt{i}"], writes=[f"tV{b}"])
                    st.store("gpsimd", scr["GV"][drows, :], tV[b][:], f"tV{b}")
                else:
                    st.op("scalar", lambda e, i=i, b=b: e.activation(out=tR[b][:], in_=pt[i][:], func=AF.Silu),
                          reads=[f"pt{i}"], writes=[f"tR{b}"])
                    st.store("gpsimd", scr["GR"][drows, :], tR[b][:], f"tR{b}")

    st.emit()


SCR_SPEC = lambda S: {
    "SIN": ([64, S], F32), "COS": ([64, S], F32),
    "QT": ([512, S], BF16), "KST": ([128, S], BF16), "KWT": ([128, S], BF16),
    "KCT": ([128, S], BF16), "VCT": ([128, S], BF16), "GQT": ([256, S], BF16), "GKT": ([256, S], BF16),
    "ALT": ([16, S], BF16), "VS": ([S, 128], BF16), "VW": ([S, 128], BF16), "GATE": ([S, 24], F32),
    "GV": ([S, 512], BF16), "GR": ([S, 512], BF16),
    "KCMPT": ([128, S // 16], BF16), "VCMP": ([2, S // 16, 64], BF16),
    "ONSA": ([S, 512], BF16), "OGLA": ([S, 512], BF16),
}

IN_SHAPES = lambda S: {
    "x": ([S, D], F32), "xh": ([S // 2, D], F32), "positions": ([1, S], I32), "g_mix": ([1, D], F32),
    "w_in": ([1, D, 4904], F32),
    "cmp_pos_k": ([1, 32, 64], F32), "cmp_w1_k": ([1, 2048, 256], F32), "cmp_b1_k": ([1, 256], F32),
    "cmp_w2_k": ([1, 256, 64], F32), "cmp_b2_k": ([1, 64], F32),
    "cmp_pos_v": ([1, 32, 64], F32), "cmp_w1_v": ([1, 2048, 256], F32), "cmp_b1_v": ([1, 256], F32),
    "cmp_w2_v": ([1, 256, 64], F32), "cmp_b2_v": ([1, 64], F32),
    "gla_w_a2": ([1, 16, 256], F32), "gla_b_a": ([1, 256], F32), "gla_norm_g": ([1, 512], F32),
    "w_proj_nsa": ([1, 512, D], F32), "w_proj_gla": ([1, 512, D], F32), "w_out": ([1, D, D], F32),
    "g_ffn": ([1, D], F32), "w_grp": ([1, D, 4], F32), "b_grp": ([1, 4], F32), "w_exp": ([1, D, 32], F32),
    "b_exp": ([1, 32], F32), "w_gate": ([1, 32, D, 512], F32), "w_up": ([1, 32, D, 512], F32),
    "w_down": ([1, 32, 512, D], F32), "g_final": ([1, D], F32), "halfidx": ([1, 1], I32),
}


def build(S, stages="ABCDE", debug=(), scr_in=(), ncores=2):
    return build_full(S, ncores, stages=("R" + stages) if "A" in stages else stages, debug=debug, scr_in=scr_in)


def stage_B(nc, S, T, C, scr):
    st = Stage(nc, "B")
    NCMP = S // 16 - 1
    srcT = {"k": st.sb("kcT", [128, S], BF16), "v": st.sb("vcT", [128, S], BF16)}
    st.load("sync", srcT["k"][:], scr["KCT"], "kcT")
    st.load("sync", srcT["v"][:], scr["VCT"], "vcT")
    cosf = st.sb("cosf", [64, S], F32)
    sinf = st.sb("sinf", [64, S], F32)
    st.load("sync", cosf[:], scr["COS"], "cosf")
    st.load("sync", sinf[:], scr["SIN"], "sinf")
    cos_e = cosf[:, 31:31 + 16 * (NCMP - 1) + 1:16]
    sin_e = sinf[:, 31:31 + 16 * (NCMP - 1) + 1:16]
    ph = [st.ps(f"ph{i}", [128, 512], F32) for i in range(2)]
    pbias = st.ps("pbias", [128, 2], F32)
    pk = [st.ps(f"pk{i}", [64, 512], F32) for i in range(2)]
    pv = st.ps("pv", [128, 64], F32)
    for kv in ("k", "v"):
        W1 = st.sb(f"W1{kv}", [128, 32, 256], BF16)
        w1src = T[f"cmp_w1_{kv}"][0].rearrange("(i d) h -> d i h", d=64)
        st.dma("gpsimd", lambda e, W1=W1, w1src=w1src: e.dma_start(out=W1[0:64], in_=w1src), f"W1{kv}", writes=[f"W1{kv}"])
        st.dma("gpsimd", lambda e, W1=W1, w1src=w1src: e.dma_start(out=W1[64:128], in_=w1src), f"W1{kv}", writes=[f"W1{kv}"])
        posf = st.sb(f"posf{kv}", [64, 32], F32)
        posb = st.sb(f"posb{kv}", [64, 32], BF16)
        st.load("sync", posf[:], T[f"cmp_pos_{kv}"][0].rearrange("i d -> d i"), f"posf{kv}")
        st.do("vector", "tensor_copy", out=posb[:], in_=posf[:], reads=[f"posf{kv}"], writes=[f"posb{kv}"])
        b1 = st.sb(f"b1{kv}", [128, 2], F32)
        st.load("sync", b1[:], T[f"cmp_b1_{kv}"].rearrange("o (hh p) -> p (o hh)", p=128), f"b1{kv}")
        w2f = st.sb(f"w2f{kv}", [128, 2, 64], F32)
        st.load("sync", w2f[:], T[f"cmp_w2_{kv}"][0].rearrange("(hh p) d -> p hh d", p=128), f"w2f{kv}")
        W2 = st.sb(f"W2{kv}", [128, 2, 64], BF16)
        st.do("vector", "tensor_copy", out=W2[:], in_=w2f[:], reads=[f"w2f{kv}"], writes=[f"W2{kv}"])
        bias1 = st.sb(f"bias1{kv}", [128, 2], F32)
        for hh in range(2):
            for i in range(32):
                st.do("tensor", "matmul", pbias[:, hh:hh + 1], W1[0:64, i, hh * 128:(hh + 1) * 128], posb[:, i:i + 1],
                      start=(i == 0), stop=(i == 31), reads=[f"W1{kv}", f"posb{kv}"], writes=["pbias"])
        st.do("vector", "tensor_tensor", out=bias1[:], in0=pbias[:], in1=b1[:], op=ALU.add,
              reads=["pbias", f"b1{kv}"], writes=[f"bias1{kv}"])
        if kv == "k":
            W2R = st.sb("W2R", [128, 2, 64], BF16)
            for hh in range(2):
                st.do("vector", "tensor_scalar", out=W2R[:, hh, 0:32], in0=w2f[:, hh, 32:64], scalar1=-1.0, scalar2=None,
                      op0=ALU.mult, reads=["w2fk"], writes=["W2R"])
                st.do("vector", "tensor_copy", out=W2R[:, hh, 32:64], in_=w2f[:, hh, 0:32], reads=["w2fk"], writes=["W2R"])
            b2 = st.sb("b2k", [64, 1], F32)
            b2R = st.sb("b2R", [64, 1], F32)
            b2src = T["cmp_b2_k"].rearrange("o d -> d o")
            st.load("sync", b2[:], b2src, "b2k")
            st.load("sync", b2R[0:32], b2src[32:64], "b2R")
            st.load("sync", b2R[32:64], b2src[0:32], "b2R")
            st.do("vector", "tensor_scalar", out=b2R[0:32], in0=b2R[0:32], scalar1=-1.0, scalar2=None, op0=ALU.mult,
                  reads=["b2R"], writes=["b2R"])
        else:
            b2v = st.sb("b2v", [128, 64], F32)
            st.load("sync", b2v[:], bcast_rows(T["cmp_b2_v"], 128), "b2v")
        for g in range(2):
            gp = slice(g * 64, (g + 1) * 64)
            h1T = [st.sb(f"h1T{kv}{g}{hh}", [128, 256 if NCMP <= 256 else NCMP], BF16) for hh in range(2)]
            for hh in range(2):
                hk = f"h1T{kv}{g}{hh}"
                for i in range(32):
                    st.do("tensor", "matmul", ph[hh][:, 0:NCMP], W1[gp, i, hh * 128:(hh + 1) * 128],
                          srcT[kv][gp, i:i + 16 * (NCMP - 1) + 1:16], start=(i == 0), stop=(i == 31),
                          reads=[f"W1{kv}", f"{kv}cT"], writes=[f"ph{hh}"])
                xh = st.sb(f"xh{kv}{g}{hh}", [128, NCMP], F32)
                t1 = st.sb(f"t1{kv}{g}{hh}", [128, NCMP], F32)
                xk, tk = f"xh{kv}{g}{hh}", f"t1{kv}{g}{hh}"
                st.do("scalar", "activation", out=xh[:], in_=ph[hh][:, 0:NCMP], func=AF.Identity, bias=bias1[:, hh:hh + 1],
                      reads=[f"ph{hh}", f"bias1{kv}"], writes=[xk])
                st.do("vector", "tensor_tensor", out=t1[:], in0=xh[:], in1=xh[:], op=ALU.mult, reads=[xk], writes=[tk])
                st.do("vector", "tensor_scalar", out=t1[:], in0=t1[:], scalar1=0.044715, scalar2=1.0, op0=ALU.mult, op1=ALU.add,
                      reads=[tk], writes=[tk])
                st.do("vector", "tensor_tensor", out=t1[:], in0=t1[:], in1=xh[:], op=ALU.mult, reads=[tk, xk], writes=[tk])
                st.do("scalar", "activation", out=t1[:], in_=t1[:], func=AF.Tanh, scale=0.7978845608028654,
                      reads=[tk], writes=[tk])
                st.do("vector", "tensor_scalar", out=t1[:], in0=t1[:], scalar1=1.0, scalar2=0.5, op0=ALU.add, op1=ALU.mult,
                      reads=[tk], writes=[tk])
                st.do("vector", "tensor_tensor", out=h1T[hh][:, 0:NCMP], in0=t1[:], in1=xh[:], op=ALU.mult,
                      reads=[tk, xk], writes=[hk])
            hks = [f"h1T{kv}{g}{hh}" for hh in range(2)]
            if kv == "k":
                for hh in range(2):
                    st.do("tensor", "matmul", pk[0][:, 0:NCMP], W2[:, hh, :], h1T[hh][:, 0:NCMP], start=(hh == 0), stop=(hh == 1),
                          reads=hks + ["W2k"], writes=["pk0"])
                for hh in range(2):
                    st.do("tensor", "matmul", pk[1][:, 0:NCMP], W2R[:, hh, :], h1T[hh][:, 0:NCMP], start=(hh == 0), stop=(hh == 1),
                          reads=hks + ["W2R"], writes=["pk1"])
                ka = st.sb(f"ka{g}", [64, NCMP], F32)
                kb = st.sb(f"kb{g}", [64, NCMP], F32)
                ko = st.sb(f"ko{g}", [64, NCMP], BF16)
                st.do("scalar", "activation", out=ka[:], in_=pk[0][:, 0:NCMP], func=AF.Identity, bias=b2[:, 0:1],
                      reads=["pk0", "b2k"], writes=[f"ka{g}"])
                st.do("scalar", "activation", out=kb[:], in_=pk[1][:, 0:NCMP], func=AF.Identity, bias=b2R[:, 0:1],
                      reads=["pk1", "b2R"], writes=[f"kb{g}"])
                st.do("vector", "tensor_tensor", out=ka[:], in0=ka[:], in1=cos_e, op=ALU.mult, reads=[f"ka{g}", "cosf"], writes=[f"ka{g}"])
                st.do("vector", "tensor_tensor", out=kb[:], in0=kb[:], in1=sin_e, op=ALU.mult, reads=[f"kb{g}", "sinf"], writes=[f"kb{g}"])
                st.do("vector", "tensor_tensor", out=ko[:], in0=ka[:], in1=kb[:], op=ALU.add, reads=[f"ka{g}", f"kb{g}"], writes=[f"ko{g}"])
                st.store("sync", scr["KCMPT"][gp, 0:NCMP], ko[:], f"ko{g}")
            else:
                for ci, n0 in enumerate(range(0, NCMP, 128)):
                    n = min(128, NCMP - n0)
                    for hh in range(2):
                        st.do("tensor", "matmul", pv[0:n, :], h1T[hh][:, n0:n0 + n], W2[:, hh, :], start=(hh == 0), stop=(hh == 1),
                              reads=hks + ["W2v"], writes=["pv"])
                    vo = st.sb(f"vo{g}{ci}", [128, 64], BF16)
                    st.do("vector", "tensor_tensor", out=vo[0:n, :], in0=pv[0:n, :], in1=b2v[0:n, :], op=ALU.add,
                          reads=["pv", "b2v"], writes=[f"vo{g}{ci}"])
                    st.store("sync", scr["VCMP"][g, n0:n0 + n, :], vo[0:n, :], f"vo{g}{ci}")
    st.emit()


def stage_C(nc, S, T, C, scr):
    st = Stage(nc, "C")
    NT = S // 128
    NSLC = S // 64
    NCP = S // 16
    NCH = (NCP + 127) // 128
    SW = 640
    ident = st.sb("ident", [128, 128], BF16)
    st.dma("gpsimd", lambda e: e.dma_start(out=ident[:], in_=C["ident"]), "ident", writes=["ident"])
    ovl = st.sb("ovl", [128, NCH, NSLC], BF16)
    for j in range(NCH):
        n = min(128, NCP - j * 128)
        st.dma("gpsimd", lambda e, j=j, n=n: e.dma_start(out=ovl[0:n, j, :], in_=C["overlap"][j * 128:j * 128 + n, 0:NSLC]),
               "ovl", writes=["ovl"])
    cst = {}
    for k in ("cmpbias", "tri", "winbias", "selK", "selB", "rowvalid"):
        cst[k] = st.sb("c_" + k, CONST_SHAPES[k], F32)
        st.load("sync", cst[k][:], C[k], "c_" + k)
    gates = st.sb("gates", [128, NT, 24], F32)
    st.load("sync", gates[:], scr["GATE"].rearrange("(c p) k -> p c k", p=128), "gates")

    ps = [st.ps(f"ps{i}", [128, 512], F32) for i in range(3)]
    pT = [st.ps(f"pT{i}", [128, 8, 128], BF16) for i in range(2)]
    po = st.ps("po", [128, 512], F32)
    po2 = st.ps("po2", [128, 512], F32)
    pimp = st.ps("pimp", [128, 512], F32)

    S_sb = [st.sb(f"S_sb{i}", [128, S], F32) for i in range(4)]
    P_sb = [st.sb(f"P_sb{i}", [128, S], BF16) for i in range(4)]
    PT_sb = [st.sb(f"PT_sb{i}", [128, NT, 128], BF16) for i in range(2)]
    Sw = [st.sb(f"Sw{i}", [128, SW], F32) for i in range(4)]
    Pw = [st.sb(f"Pw{i}", [128, SW], BF16) for i in range(4)]
    PTw = [st.sb(f"PTw{i}", [128, 5, 128], BF16) for i in range(2)]
    mx = [st.sb(f"mx{i}", [128, 1], F32) for i in range(4)]
    mxw = [st.sb(f"mxw{i}", [128, 1], F32) for i in range(4)]
    sums = [st.sb(f"sums{i}", [128, 12], F32) for i in range(2)]
    rr = [st.sb(f"rr{i}", [128, 12], F32) for i in range(2)]
    impS = st.sb("impS", [128, NSLC], F32)
    imp2 = st.sb("imp2", [128, NSLC], F32)
    m8a = st.sb("m8a", [128, 8], F32)
    m8b = st.sb("m8b", [128, 8], F32)
    mb = st.sb("mb", [128, NSLC], F32)
    acc = st.sb("acc", [128, 256], F32)
    oout = [st.sb(f"oout{i}", [128, 256], BF16) for i in range(2)]
    QTb = [st.sb(f"QTb{i}", [64, 4, 128], BF16) for i in range(2)]
    KS = st.sb("KS", [64, S], BF16)
    KW = st.sb("KW", [64, S], BF16)
    VS = st.sb("VS", [128, NT, 64], BF16)
    VW = st.sb("VW", [128, NT, 64], BF16)
    KC = st.sb("KC", [64, NCP], BF16)
    VC = st.sb("VC", [128, NCH, 64], BF16)
    cnt = {"tb": 0, "sb": 0, "cp": 0}

    def next_ps():
        i = cnt["sb"] % 3
        cnt["sb"] += 1
        return i

    def tail_group(items, act_only=False):
        rounds = []
        for it in items:
            L = it[4]
            nkt = (L + 127) // 128
            for k0 in range(0, nkt, 8):
                rounds.append((it, k0, min(8, nkt - k0), nkt))

        def emit_T(r):
            (Pt, pkey, PTt, ptkey, L, Vt, vkey, po_ap, pokey, extra), k0, nb, nkt = r
            b = cnt["tb"] % 2
            cnt["tb"] += 1
            for kk in range(nb):
                kt = k0 + kk
                nj = min(128, L - kt * 128)
                st.do("tensor", "transpose", out=pT[b][0:nj, kk, :], in_=Pt[:, kt * 128:kt * 128 + nj], identity=ident[:],
                      reads=[pkey, "ident"], writes=[f"pT{b}"])
            njl = min(128, L - (k0 + nb - 1) * 128)
            cnt["cp"] += 1
            full = nb if njl == 128 else nb - 1
            if act_only or cnt["cp"] % 2 == 0:
                eng, meth = "scalar", "copy"
            else:
                eng, meth = "vector", "tensor_copy"
            if full > 0:
                st.do(eng, meth, out=PTt[:, k0:k0 + full, :], in_=pT[b][:, 0:full, :], reads=[f"pT{b}"], writes=[(ptkey, k0, 0)])
            if full < nb:
                st.do(eng, meth, out=PTt[0:njl, k0 + nb - 1, :], in_=pT[b][0:njl, nb - 1, :], reads=[f"pT{b}"], writes=[(ptkey, k0, 1)])

        def emit_PV(r):
            (Pt, pkey, PTt, ptkey, L, Vt, vkey, po_ap, pokey, extra), k0, nb, nkt = r
            for kk in range(nb):
                kt = k0 + kk
                nj = min(128, L - kt * 128)
                st.do("tensor", "matmul", po_ap, PTt[0:nj, kt, :], Vt(kt, nj), start=(kt == 0), stop=(kt == nkt - 1),
                      reads=[(ptkey, k0, 0), (ptkey, k0, 1), vkey], writes=[pokey])
                if extra is not None:
                    extra(kt, nj, nkt)

        for i, r in enumerate(rounds):
            emit_T(r)
            if i >= 1:
                emit_PV(rounds[i - 1])
        emit_PV(rounds[-1])

    for g in range(2):
        gp = slice(g * 64, (g + 1) * 64)
        st.load("sync", KS[:], scr["KST"][gp, :], "KS")
        st.load("sync", KW[:], scr["KWT"][gp, :], "KW")
        st.load("gpsimd", VS[:], scr["VS"][:, gp].rearrange("(kt p) d -> p kt d", p=128), "VS")
        st.load("gpsimd", VW[:], scr["VW"][:, gp].rearrange("(kt p) d -> p kt d", p=128), "VW")
        st.load("sync", KC[:, 0:NCP - 1], scr["KCMPT"][gp, 0:NCP - 1], "KC")
        st.do("vector", "memset", VC[:], 0.0, reads=[], writes=["VC"])
        for j in range(NCH):
            n = min(128, NCP - 1 - j * 128)
            st.load("sync", VC[0:n, j, :], scr["VCMP"][g, j * 128:j * 128 + n, :], "VC")

        for c in range(NT):
            cblk = slice(c * 128, (c + 1) * 128)
            cb = c % 2
            qb = c % 2
            qk = f"QTb{qb}"
            st.load("sync", QTb[qb][:], scr["QT"][g * 256:(g + 1) * 256, cblk].rearrange("(h d) t -> d h t", d=64), qk)
            ncmp = 8 * c + 7
            ncp32 = ((ncmp + 31) // 32) * 32
            for h in range(4):
                si = next_ps()
                st.do("tensor", "matmul", ps[si][:, 0:ncmp], QTb[qb][:, h, :], KC[:, 0:ncmp], start=True, stop=True,
                      reads=[qk, "KC"], writes=[f"ps{si}"])
                if ncmp > 8:
                    st.do("scalar", "copy", out=Sw[h][:, 0:ncmp - 8], in_=ps[si][:, 0:ncmp - 8], reads=[f"ps{si}"], writes=[(f"Sw{h}", 0), (f"Sw{h}", 1)])
                    st.do("vector", "tensor_tensor", out=Sw[h][:, ncmp - 8:ncmp], in0=ps[si][:, ncmp - 8:ncmp],
                          in1=cst["cmpbias"][:, 0:8], op=ALU.add, reads=[f"ps{si}", "c_cmpbias"], writes=[(f"Sw{h}", 0), (f"Sw{h}", 1)])
                else:
                    st.do("vector", "tensor_tensor", out=Sw[h][:, 0:7], in0=ps[si][:, 0:7],
                          in1=cst["cmpbias"][:, 1:8], op=ALU.add, reads=[f"ps{si}", "c_cmpbias"], writes=[(f"Sw{h}", 0), (f"Sw{h}", 1)])
            for h in range(4):
                st.do("vector", "reduce_max", out=mxw[h][:], in_=Sw[h][:, 0:ncmp], axis=AX.X, negate=True, reads=[(f"Sw{h}", 0), (f"Sw{h}", 1)], writes=[f"mxw{h}"])
            for h in range(4):
                sidx = h * 3
                st.do("scalar", "activation", out=Sw[h][:, 0:ncmp], in_=Sw[h][:, 0:ncmp], func=AF.Exp, bias=mxw[h][:, 0:1],
                      accum_out=sums[cb][:, sidx:sidx + 1], reads=[(f"Sw{h}", 0), (f"Sw{h}", 1), f"mxw{h}"], writes=[(f"Sw{h}", 0), (f"Sw{h}", 1), (f"sums{cb}", sidx)])
            for h in range(4):
                sidx = h * 3
                st.do("vector", "reciprocal", out=mxw[h][:], in_=sums[cb][:, sidx:sidx + 1], reads=[(f"sums{cb}", sidx)], writes=[f"mxw{h}"])
                if c == 0:
                    st.do("vector", "tensor_tensor", out=mxw[h][:], in0=mxw[h][:], in1=cst["rowvalid"][:], op=ALU.mult,
                          reads=[f"mxw{h}", "c_rowvalid"], writes=[f"mxw{h}"])
            for h in range(4):
                st.do("gpsimd", "memset", Pw[h][:, ncmp:ncp32], 0.0, reads=[], writes=[f"Pw{h}"])
                st.do("vector", "tensor_scalar", out=Pw[h][:, 0:ncmp], in0=Sw[h][:, 0:ncmp], scalar1=mxw[h][:, 0:1], scalar2=None,
                      op0=ALU.mult, reads=[(f"Sw{h}", 0), (f"Sw{h}", 1), f"mxw{h}"], writes=[f"Pw{h}"])
            items = []
            for h in range(4):
                def extra(kt, nj, nkt, h=h):
                    st.do("tensor", "matmul", pimp[:, 0:NSLC], PTw[h % 2][0:nj, kt, :], ovl[0:nj, kt, :],
                          start=(h == 0 and kt == 0), stop=(h == 3 and kt == nkt - 1), reads=[(f"PTw{h % 2}", 0, 0), (f"PTw{h % 2}", 0, 1), "ovl"], writes=["pimp"])
                items.append((Pw[h], f"Pw{h}", PTw[h % 2], f"PTw{h % 2}", ncp32, lambda kt, nj: VC[0:nj, kt, :], "VC",
                              po[:, h * 64:(h + 1) * 64], "po", extra))
            tail_group(items)
            use_sel = (2 * c + 2) > 16
            if use_sel:
                st.do("vector", "tensor_copy", out=impS[:], in_=pimp[:, 0:NSLC], reads=["pimp"], writes=["impS"])
                if 2 * c + 2 < NSLC:
                    st.do("vector", "memset", impS[:, 2 * c + 2:NSLC], -1e30, reads=[], writes=["impS"])
                lo = 2 * c - 1
                st.do("vector", "tensor_tensor", out=impS[:, lo:lo + 3], in0=impS[:, lo:lo + 3], in1=cst["selK"][:, 0:3], op=ALU.mult,
                      reads=["impS", "c_selK"], writes=["impS"])
                st.do("vector", "tensor_tensor", out=impS[:, lo:lo + 3], in0=impS[:, lo:lo + 3], in1=cst["selB"][:, 0:3], op=ALU.add,
                      reads=["impS", "c_selB"], writes=["impS"])
                st.do("vector", "memset", impS[:, 0:1], 1e4, reads=[], writes=["impS"])
                st.do("vector", "max", out=m8a[:], in_=impS[:], reads=["impS"], writes=["m8a"])
                st.do("vector", "match_replace", out=imp2[:], in_to_replace=m8a[:], in_values=impS[:], imm_value=-3e38,
                      reads=["impS", "m8a"], writes=["imp2"])
                st.do("vector", "max", out=m8b[:], in_=imp2[:], reads=["imp2"], writes=["m8b"])
                st.do("vector", "tensor_scalar", out=mb[:], in0=impS[:], scalar1=m8b[:, 7:8], scalar2=-NEG, op0=ALU.is_ge, op1=ALU.mult,
                      reads=["impS", "m8b"], writes=["mb"])
                st.do("vector", "tensor_scalar", out=mb[:], in0=mb[:], scalar1=NEG, scalar2=None, op0=ALU.add,
                      reads=["mb"], writes=["mb"])
            L = 128 * (c + 1)
            k0w = max(0, 128 * c - 512)
            Lw = L - k0w
            boff = k0w - (128 * c - 512)
            kt0 = k0w // 128

            def slc_p1():
                for h in range(4):
                    sk = f"S_sb{h}"
                    for k0 in range(0, L, 512):
                        w = min(512, L - k0)
                        si = next_ps()
                        st.do("tensor", "matmul", ps[si][:, 0:w], QTb[qb][:, h, :], KS[:, k0:k0 + w], start=True, stop=True,
                              reads=[qk, "KS"], writes=[f"ps{si}"])
                        if use_sel:
                            nb = w // 64
                            mbb = mb[:, k0 // 64:k0 // 64 + nb].unsqueeze(2).broadcast_to([128, nb, 64])
                            st.do("vector", "tensor_tensor", out=S_sb[h][:, k0:k0 + w].rearrange("p (b k) -> p b k", k=64),
                                  in0=ps[si][:, 0:w].rearrange("p (b k) -> p b k", k=64), in1=mbb, op=ALU.add,
                                  reads=[f"ps{si}", "mb"], writes=[(sk, k0 // 512)])
                        else:
                            st.do("scalar", "copy", out=S_sb[h][:, k0:k0 + w], in_=ps[si][:, 0:w], reads=[f"ps{si}"], writes=[(sk, k0 // 512)])
                    lk = (sk, (L - 128) // 512)
                    st.do("vector", "tensor_tensor", out=S_sb[h][:, L - 128:L], in0=S_sb[h][:, L - 128:L], in1=cst["tri"][:], op=ALU.add,
                          reads=[lk, "c_tri"], writes=[lk])

            def slc_p2():
                for h in range(4):
                    st.do("vector", "reduce_max", out=mx[h][:], in_=S_sb[h][:, 0:L], axis=AX.X, negate=True,
                          reads=[(f"S_sb{h}", k) for k in range((L + 511) // 512)], writes=[f"mx{h}"])

            def slc_p3():
                for h in range(4):
                    sidx = h * 3 + 1
                    st.do("scalar", "activation", out=P_sb[h][:, 0:L], in_=S_sb[h][:, 0:L], func=AF.Exp, bias=mx[h][:, 0:1],
                          accum_out=sums[cb][:, sidx:sidx + 1], reads=[(f"S_sb{h}", k) for k in range((L + 511) // 512)] + [f"mx{h}"],
                          writes=[f"P_sb{h}", (f"sums{cb}", sidx)])

            def slc_p4():
                tail_group([(P_sb[h], f"P_sb{h}", PT_sb[h % 2], f"PT_sb{h % 2}", L, lambda kt, nj: VS[:, kt, :], "VS",
                             po[:, 256 + h * 64:256 + (h + 1) * 64], "po", None) for h in range(4)], act_only=True)

            def win_p1():
                for h in range(4):
                    for k0 in range(0, Lw, 512):
                        w = min(512, Lw - k0)
                        si = next_ps()
                        st.do("tensor", "matmul", ps[si][:, 0:w], QTb[qb][:, h, :], KW[:, k0w + k0:k0w + k0 + w], start=True, stop=True,
                              reads=[qk, "KW"], writes=[f"ps{si}"])
                        st.do("vector", "tensor_tensor", out=Sw[h][:, k0:k0 + w], in0=ps[si][:, 0:w],
                              in1=cst["winbias"][:, boff + k0:boff + k0 + w], op=ALU.add, reads=[f"ps{si}", "c_winbias"], writes=[(f"Sw{h}", k0 // 512)])

            def win_p2():
                for h in range(4):
                    st.do("vector", "reduce_max", out=mxw[h][:], in_=Sw[h][:, 0:Lw], axis=AX.X, negate=True, reads=[(f"Sw{h}", 0), (f"Sw{h}", 1)], writes=[f"mxw{h}"])

            def win_p3():
                for h in range(4):
                    sidx = h * 3 + 2
                    st.do("scalar", "activation", out=Pw[h][:, 0:Lw], in_=Sw[h][:, 0:Lw], func=AF.Exp, bias=mxw[h][:, 0:1],
                          accum_out=sums[cb][:, sidx:sidx + 1], reads=[(f"Sw{h}", 0), (f"Sw{h}", 1), f"mxw{h}"], writes=[f"Pw{h}", (f"sums{cb}", sidx)])

            def win_p4():
                tail_group([(Pw[h], f"Pw{h}", PTw[h % 2], f"PTw{h % 2}", Lw, lambda kt, nj, kt0=kt0: VW[:, kt0 + kt, :], "VW",
                             po2[:, h * 64:(h + 1) * 64], "po_win", None) for h in range(4)])

            win_p1()
            slc_p1()
            win_p2()
            win_p3()
            slc_p2()
            slc_p3()
            win_p4()
            slc_p4()
            sumkeys = [(f"sums{cb}", i) for i in range(12)]
            st.do("vector", "reciprocal", out=rr[cb][:], in_=sums[cb][:], reads=sumkeys, writes=[f"rr{cb}"])
            st.do("vector", "memset", rr[cb][:].rearrange("p (h b) -> p h b", b=3)[:, :, 0], 1.0, reads=[], writes=[f"rr{cb}"])
            st.do("vector", "tensor_tensor", out=rr[cb][:], in0=rr[cb][:], in1=gates[:, c, 12 * g:12 * g + 12], op=ALU.mult,
                  reads=[f"rr{cb}", "gates"], writes=[f"rr{cb}"])
            ob = c % 2
            for h in range(4):
                hs = slice(h * 64, (h + 1) * 64)
                st.do("vector", "tensor_scalar", out=acc[:, hs], in0=po[:, hs], scalar1=rr[cb][:, 3 * h:3 * h + 1], scalar2=None,
                      op0=ALU.mult, reads=["po", f"rr{cb}"], writes=[("acc", h)])
            for h in range(4):
                hs = slice(h * 64, (h + 1) * 64)
                st.do("vector", "scalar_tensor_tensor", out=acc[:, hs], in0=po[:, 256 + h * 64:256 + (h + 1) * 64],
                      scalar=rr[cb][:, 3 * h + 1:3 * h + 2], in1=acc[:, hs], op0=ALU.mult, op1=ALU.add,
                      reads=["po", f"rr{cb}", ("acc", h)], writes=[("acc", h)])
            for h in range(4):
                hs = slice(h * 64, (h + 1) * 64)
                st.do("vector", "scalar_tensor_tensor", out=oout[ob][:, hs], in0=po2[:, hs],
                      scalar=rr[cb][:, 3 * h + 2:3 * h + 3], in1=acc[:, hs], op0=ALU.mult, op1=ALU.add,
                      reads=["po_win", f"rr{cb}", ("acc", h)], writes=[(f"oout{ob}", h)])
            st.dma("sync", lambda e, cblk=cblk, ob=ob, g=g: e.dma_start(out=scr["ONSA"][cblk, g * 256:(g + 1) * 256], in_=oout[ob][:]),
                   f"oout{ob}", reads=[(f"oout{ob}", h) for h in range(4)])
    st.emit()


def stage_D(nc, S, T, C, scr):
    st = Stage(nc, "D")
    NCK = S // 64
    NC4 = S // 512
    ident = st.sb("ident", [128, 128], BF16)
    st.dma("gpsimd", lambda e: e.dma_start(out=ident[:], in_=C["ident"]), "ident", writes=["ident"])
    gmask = st.sb("gmask", [64, 64], F32)
    st.load("sync", gmask[:], C["glamask"], "gmask")
    alT = st.sb("alT", [16, S], BF16)
    st.load("sync", alT[:], scr["ALT"], "alT")
    wa2 = st.sb("wa2", [16, 256], BF16)
    st.dma("gpsimd", lambda e: e.dma_start(out=wa2[:], in_=T["gla_w_a2"][0]), "wa2", writes=["wa2"])
    rmask = st.sb("rmask", [128, S], BF16)
    st.do("vector", "memset", rmask[:], 1.0, reads=[], writes=["rmask"])
    st.do("vector", "memset", rmask[:, 0:S:64], 0.0, reads=[], writes=["rmask"])

    bufA = st.sb("bufA", [128, S], F32)
    bufB = st.sb("bufB", [128, S], F32)
    qT = st.sb("qT", [128, S], BF16)
    kT = st.sb("kT", [128, S], BF16)
    kdec = st.sb("kdec", [128, S], BF16)
    V64 = st.sb("V64", [64, NCK, 256], BF16)
    Sf = [st.sb(f"Sf{i}", [128, 256], F32) for i in range(2)]
    Sb = st.sb("Sb", [128, NCK, 256], BF16)
    Qbd = st.sb("Qbd", [128, NCK, 128], BF16)
    nb = st.sb("nb", [128, 1], F32)
    gb = st.sb("gb", [128, 128], F32)
    tmp = [st.sb(f"tmp{i}", [128, 512], F32) for i in range(2)]
    pkv_ = [st.ps(f"pkv{i}", [128, 512], F32) for i in range(2)]
    pkv = [p[:, 0:256] for p in pkv_]
    pxa = pkv
    pTk_ = [st.ps(f"pTk{i}", [128, 1024], BF16) for i in range(2)]
    pTk = [pTk_[0][0:64, 0:128], pTk_[1][0:64, 0:128]]
    pA_ = [st.ps(f"pA{i}", [128, 512], F32) for i in range(2)]
    pA = [p[0:64, 0:128] for p in pA_]
    pO_ = [st.ps(f"pO{i}", [128, 512], F32) for i in range(2)]
    pO = [p[:, 0:256] for p in pO_]
    kdT = [st.sb(f"kdT{i}", [64, 128], BF16) for i in range(2)]
    ATs = [st.sb(f"ATs{i}", [64, 128], BF16) for i in range(2)]
    R64 = [st.sb(f"R64{i}", [128, 128], BF16) for i in range(2)]
    gg = [st.sb(f"gg{i}", [128, 128], F32) for i in range(2)]
    sqj = [st.sb(f"sqj{i}", [128, 128], F32) for i in range(2)]
    ssq = [st.sb(f"ssq{i}", [128, 1], F32) for i in range(2)]
    og = [st.sb(f"og{i}", [128, 128], BF16) for i in range(2)]

    for hp in range(2):
        rows = slice(hp * 128, (hp + 1) * 128)
        cols = slice(hp * 256, (hp + 1) * 256)
        st.load("sync", qT[:], scr["GQT"][rows, :], "qT")
        st.load("sync", kT[:], scr["GKT"][rows, :], "kT")
        st.load("gpsimd", V64[:], scr["GV"][:, cols].rearrange("(c s) e -> s c e", s=64), "V64")
        st.load("sync", nb[:], T["gla_b_a"][:, rows].rearrange("o p -> p o"), "nb")
        st.do("vector", "tensor_scalar", out=nb[:], in0=nb[:], scalar1=-1.0, scalar2=None, op0=ALU.mult, reads=["nb"], writes=["nb"])
        for tc in range(S // 256):
            i = tc % 2
            tsl = slice(tc * 256, (tc + 1) * 256)
            st.do("tensor", "matmul", pxa[i], wa2[:, rows], alT[:, tsl], start=True, stop=True,
                  reads=["wa2", "alT"], writes=[f"pkv{i}"])
            st.do("scalar", "activation", out=tmp[i][:, 0:256], in_=pxa[i], func=AF.Exp, bias=nb[:, 0:1], scale=-1.0,
                  reads=[f"pkv{i}", "nb"], writes=[f"tmp{i}"])
            st.do("vector", "tensor_scalar", out=tmp[i][:, 0:256], in0=tmp[i][:, 0:256], scalar1=1.0, scalar2=None, op0=ALU.add,
                  reads=[f"tmp{i}"], writes=[f"tmp{i}"])
            st.do("scalar", "activation", out=bufA[:, tsl], in_=tmp[i][:, 0:256], func=AF.Ln, reads=[f"tmp{i}"], writes=["bufA"])
        st.do("vector", "tensor_tensor_scan", out=bufB[:], data0=rmask[:], data1=bufA[:], initial=0.0, op0=ALU.mult, op1=ALU.add,
              reads=["rmask", "bufA"], writes=["bufB"])
        st.do("scalar", "activation", out=bufA[:], in_=bufB[:], func=AF.Exp, scale=-1.0 / 16.0, reads=["bufB"], writes=["bufA"])
        st.do("scalar", "activation", out=bufB[:], in_=bufB[:], func=AF.Exp, scale=1.0 / 16.0, reads=["bufB"], writes=["bufB"])
        st.do("vector", "tensor_tensor", out=qT[:], in0=qT[:], in1=bufA[:], op=ALU.mult, reads=["qT", "bufA"], writes=["qT"])
        st.do("vector", "tensor_tensor", out=kT[:], in0=kT[:], in1=bufB[:], op=ALU.mult, reads=["kT", "bufB"], writes=["kT"])
        dec = bufA[:, 63:63 + 64 * (NCK - 1) + 1:64]
        st.do("vector", "tensor_tensor", out=kdec[:].rearrange("p (c s) -> p c s", s=64),
              in0=kT[:].rearrange("p (c s) -> p c s", s=64), in1=dec.unsqueeze(2).broadcast_to([128, NCK, 64]), op=ALU.mult,
              reads=["kT", "bufA"], writes=["kdec"])
        st.do("gpsimd", "memset", Qbd[:], 0.0, reads=[], writes=["Qbd"])
        for h in range(2):
            hr = slice(h * 64, (h + 1) * 64)
            st.do("gpsimd", "tensor_copy", out=Qbd[hr, :, h * 64:(h + 1) * 64], in_=qT[hr, :].rearrange("p (c s) -> p c s", s=64),
                  reads=["qT"], writes=["Qbd"])
        st.do("vector", "memset", Sf[0][:], 0.0, reads=[], writes=["Sf0"])
        def rec_T(c):
            i = c % 2
            csl = slice(c * 64, (c + 1) * 64)
            st.do("tensor", "transpose", out=pTk[i], in_=kdec[:, csl], identity=ident[:], reads=["kdec", "ident"], writes=[f"pTk{i}"])
            st.do("scalar", "copy", out=kdT[i][:], in_=pTk[i], reads=[f"pTk{i}"], writes=[f"kdT{i}"])

        rec_T(0)
        for c in range(NCK):
            i = c % 2
            if c + 1 < NCK:
                rec_T(c + 1)
            st.do("tensor", "matmul", pkv[i], kdT[i][:], V64[:, c, :], start=True, stop=True,
                  reads=[f"kdT{i}", "V64"], writes=[f"pkv{i}"])
            st.do("gpsimd", "tensor_copy", out=Sb[:, c, :], in_=Sf[i][:], reads=[f"Sf{i}"], writes=[("Sb", c)])
            st.do("vector", "scalar_tensor_tensor", out=Sf[1 - i][:], in0=Sf[i][:], scalar=dec[:, c:c + 1],
                  in1=pkv[i], op0=ALU.mult, op1=ALU.add, reads=[f"Sf{i}", "bufA", f"pkv{i}"], writes=[f"Sf{1 - i}"])
        if DBG_D == 2:
            continue
        for h in range(2):
            st.load("sync", gb[h * 64:(h + 1) * 64, :], bcast_rows(T["gla_norm_g"][:, hp * 256 + h * 128:hp * 256 + (h + 1) * 128], 64), "gb")
        def out_A(c):
            i = c % 2
            csl = slice(c * 64, (c + 1) * 64)
            st.do("tensor", "matmul", pA[i], kT[:, csl], Qbd[:, c, :], start=True, stop=True, reads=["kT", "Qbd"], writes=[f"pA{i}"])
            st.do("vector", "tensor_tensor", out=ATs[i][:].rearrange("p (h t) -> p h t", h=2), in0=pA[i].rearrange("p (h t) -> p h t", h=2),
                  in1=gmask[:].unsqueeze(1).broadcast_to([64, 2, 64]), op=ALU.mult, reads=[f"pA{i}", "gmask"], writes=[f"ATs{i}"])

        for c in range(NCK):
            i = c % 2
            csl = slice(c * 64, (c + 1) * 64)
            for h in range(2):
                st.load("sync", R64[i][h * 64:(h + 1) * 64, :], scr["GR"][csl, hp * 256 + h * 128:hp * 256 + (h + 1) * 128], f"R64{i}")
            st.do("gpsimd", "tensor_tensor", out=gg[i][:], in0=R64[i][:], in1=gb[:], op=ALU.mult, reads=[f"R64{i}", "gb"], writes=[f"gg{i}"])
            if c == 0:
                out_A(0)
            if c + 1 < NCK:
                out_A(c + 1)
            st.do("tensor", "matmul", pO[i], Qbd[:, c, :], Sb[:, c, :], start=True, stop=False, reads=["Qbd", ("Sb", c)], writes=[f"pO{i}"])
            st.do("tensor", "matmul", pO[i], ATs[i][:], V64[:, c, :], start=False, stop=True, reads=[f"ATs{i}", "V64"], writes=[f"pO{i}"])
            for h in range(2):
                hr = slice(h * 64, (h + 1) * 64)
                st.do("scalar", "activation", out=sqj[i][hr, :], in_=pO[i][hr, h * 128:(h + 1) * 128], func=AF.Square,
                      accum_out=ssq[i][hr, 0:1], reads=[f"pO{i}"], writes=[(f"sqj{i}", h), (f"ssq{i}", h)])
            st.do("vector", "tensor_scalar", out=ssq[i][:], in0=ssq[i][:], scalar1=1.0 / 128.0, scalar2=1e-6, op0=ALU.mult, op1=ALU.add,
                  reads=[(f"ssq{i}", 0), (f"ssq{i}", 1)], writes=[f"rs{i}"])
            st.do("scalar", "activation", out=ssq[i][:], in_=ssq[i][:], func=AF.Sqrt, reads=[f"rs{i}"], writes=[f"rs{i}"])
            st.do("vector", "reciprocal", out=ssq[i][:], in_=ssq[i][:], reads=[f"rs{i}"], writes=[f"rs{i}", (f"ssq{i}", 0), (f"ssq{i}", 1)])
            for h in range(2):
                hr = slice(h * 64, (h + 1) * 64)
                st.do("vector", "scalar_tensor_tensor", out=og[i][hr, :], in0=pO[i][hr, h * 128:(h + 1) * 128],
                      scalar=ssq[i][hr, 0:1], in1=gg[i][hr, :], op0=ALU.mult, op1=ALU.mult,
                      reads=[f"pO{i}", f"rs{i}", (f"ssq{i}", 0), (f"ssq{i}", 1), f"gg{i}"], writes=[f"og{i}"])
            for h in range(2):
                st.store("sync", scr["OGLA"][csl, hp * 256 + h * 128:hp * 256 + (h + 1) * 128], og[i][h * 64:(h + 1) * 64, :], f"og{i}")
    st.emit()


def rms_rstd(st, src_ap, src_key, sq, ss, rs, i, dim):
    st.do("scalar", "activation", out=sq[i][:], in_=src_ap, func=AF.Square, accum_out=ss[i][:],
          reads=[src_key], writes=[f"sq{i}", f"ss{i}"])
    st.do("vector", "tensor_scalar", out=rs[i][:], in0=ss[i][:], scalar1=1.0 / dim, scalar2=1e-6, op0=ALU.mult, op1=ALU.add,
          reads=[f"ss{i}"], writes=[f"rs{i}"])
    st.do("scalar", "activation", out=rs[i][:], in_=rs[i][:], func=AF.Sqrt, reads=[f"rs{i}"], writes=[f"rs{i}"])
    st.do("vector", "reciprocal", out=rs[i][:], in_=rs[i][:], reads=[f"rs{i}"], writes=[f"rs{i}"])


def stage_E0(nc, S, T, C, scr):
    st = Stage(nc, "E0")
    TH = S // 2
    NTH = TH // 128
    ident = st.sb("ident", [128, 128], BF16)
    st.dma("gpsimd", lambda e: e.dma_start(out=ident[:], in_=C["ident"]), "ident", writes=["ident"])
    gm = st.sb("gm", [128, 8], F32)
    st.load("sync", gm[:], T["g_mix"].rearrange("o (kt p) -> p (o kt)", p=128), "gm")
    Wm = st.sb("Wm", [128, 8, 2048], BF16)
    wst = [st.sb(f"wst{i}", [128, 2048], F32) for i in range(2)]
    for kt in range(8):
        i = kt % 2
        st.load("sync" if i == 0 else "gpsimd", wst[i][:], T["w_in"][0, kt * 128:(kt + 1) * 128, 2856:4904], f"wst{i}")
        st.do("vector" if i == 0 else "gpsimd", "tensor_scalar", out=Wm[:, kt, :], in0=wst[i][:], scalar1=gm[:, kt:kt + 1], scalar2=None,
              op0=ALU.mult, reads=[f"wst{i}", "gm"], writes=[("Wm", kt)])
    xt = [st.sb(f"xt{i}", [128, D], F32) for i in range(2)]
    sq = [st.sb(f"sq{i}", [128, D], BF16) for i in range(2)]
    xn = [st.sb(f"xn{i}", [128, D], BF16) for i in range(2)]
    ss = [st.sb(f"ss{i}", [128, 1], F32) for i in range(2)]
    rs = [st.sb(f"rs{i}", [128, 1], F32) for i in range(2)]
    uTt = [st.sb(f"uTt{i}", [128, 8, 128], BF16) for i in range(2)]
    sg = [st.sb(f"sg{i}", [128, 2048], BF16) for i in range(2)]
    ptr = [st.ps(f"ptr{i}", [128, 8, 128], BF16) for i in range(2)]
    pm = [st.ps(f"pm{i}", [128, 512], F32) for i in range(4)]
    for t in range(NTH):
        i = t % 2
        st.load("sync", xt[i][:], T["xh"][t * 128:(t + 1) * 128, :], f"xt{i}")
        rms_rstd(st, xt[i][:], f"xt{i}", sq, ss, rs, i, D)
        st.do("vector", "tensor_scalar", out=xn[i][:], in0=xt[i][:], scalar1=rs[i][:, 0:1], scalar2=None, op0=ALU.mult,
              reads=[f"xt{i}", f"rs{i}"], writes=[f"xn{i}"])
        for kt in range(8):
            st.do("tensor", "transpose", out=ptr[i][:, kt, :], in_=xn[i][:, kt * 128:(kt + 1) * 128], identity=ident[:],
                  reads=[f"xn{i}", "ident"], writes=[f"ptr{i}"])
        st.do("scalar", "copy", out=uTt[i][:], in_=ptr[i][:], reads=[f"ptr{i}"], writes=[f"uTt{i}"])
        for cc in range(4):
            for kt in range(8):
                st.do("tensor", "matmul", pm[cc][:], uTt[i][:, kt, :], Wm[:, kt, cc * 512:(cc + 1) * 512], start=(kt == 0), stop=(kt == 7),
                      reads=[f"uTt{i}", ("Wm", kt)], writes=[f"pm{cc}"])
            st.do("scalar", "activation", out=sg[i][:, cc * 512:(cc + 1) * 512], in_=pm[cc][:], func=AF.Sigmoid,
                  reads=[f"pm{cc}"], writes=[(f"sg{i}", cc)])
        st.dma("sync", lambda e, t=t, i=i: e.dma_start(out=scr["SIGM"][t * 128:(t + 1) * 128, :], in_=sg[i][:]),
               f"sg{i}", reads=[(f"sg{i}", cc) for cc in range(4)])
    st.emit()


def stage_E1(nc, S, T, C, scr, P, ncores):
    st = Stage(nc, "E1")
    TH = S // 2
    NTH = TH // 128
    h1, vT, combT = P["h1"], P["vT"], P["combT"]
    identb = st.sb("identb", [128, 128], BF16)
    st.dma("gpsimd", lambda e: e.dma_start(out=identb[:], in_=C["ident"]), "identb", writes=["identb"])
    Wpn = st.sb("Wpn", [128, 4, D], BF16)
    Wpg = st.sb("Wpg", [128, 4, D], BF16)
    Wo = st.sb("Wo", [128, 8, D], BF16)
    st.dma("gpsimd", lambda e: e.dma_start(out=Wpn[:], in_=T["w_proj_nsa"][0].rearrange("(j p) d -> p j d", p=128)), "Wpn", writes=["Wpn"])
    st.dma("gpsimd", lambda e: e.dma_start(out=Wpg[:], in_=T["w_proj_gla"][0].rearrange("(j p) d -> p j d", p=128)), "Wpg", writes=["Wpg"])
    st.dma("gpsimd", lambda e: e.dma_start(out=Wo[:], in_=T["w_out"][0].rearrange("(j p) d -> p j d", p=128)), "Wo", writes=["Wo"])
    Wr = st.sb("Wr", [128, 8, 36], BF16)
    st.dma("gpsimd", lambda e: e.dma_start(out=Wr[:, :, 0:4], in_=T["w_grp"][0].rearrange("(j p) g -> p j g", p=128)), "Wr", writes=["Wr"])
    st.dma("gpsimd", lambda e: e.dma_start(out=Wr[:, :, 4:36], in_=T["w_exp"][0].rearrange("(j p) g -> p j g", p=128)), "Wr", writes=["Wr"])
    br = st.sb("br", [128, 36], F32)
    st.load("sync", br[:, 0:4], bcast_rows(T["b_grp"], 128), "br")
    st.load("sync", br[:, 4:36], bcast_rows(T["b_exp"], 128), "br")
    gfb = st.sb("gfb", [128, D], F32)
    st.load("sync", gfb[:], bcast_rows(T["g_ffn"], 128), "gfb")

    xt = [st.sb(f"xt{i}", [128, D], F32) for i in range(2)]
    on = [st.sb(f"on{i}", [128, 512], BF16) for i in range(2)]
    og = [st.sb(f"og{i}", [128, 512], BF16) for i in range(2)]
    sgm = [st.sb(f"sgm{i}", [128, 2048], BF16) for i in range(2)]
    onT = [st.sb(f"onT{i}", [128, 8, 128], BF16) for i in range(2)]
    m1 = st.sb("m1", [128, D], F32)
    m2 = st.sb("m2", [128, D], F32)
    mx_ = st.sb("mixed", [128, D], BF16)
    mT = st.sb("mT", [128, 8, 128], BF16)
    sq = [st.sb(f"sq{i}", [128, D], BF16) for i in range(2)]
    ss = [st.sb(f"ss{i}", [128, 1], F32) for i in range(2)]
    rs = [st.sb(f"rs{i}", [128, 1], F32) for i in range(2)]
    vnb = st.sb("vnb", [128, D], BF16)
    combb = st.sb("combb", [128, 32], BF16)
    lg = st.sb("lg", [128, 36], F32)
    sm = st.sb("sm", [128, 8], F32)
    bg = st.sb("bg", [128, 4], F32)
    lem = st.sb("lem", [128, 32], F32)
    top8 = st.sb("top8", [128, 8], F32)
    msk = st.sb("msk", [128, 32], F32)
    ex = st.sb("ex", [128, 32], F32)
    comb = st.sb("comb", [128, 32], F32)
    eg = st.sb("eg", [128, 4], F32)

    pT = [st.ps(f"pT{i}", [128, 8, 128], BF16) for i in range(2)]
    py = [st.ps(f"py{i}", [128, 512], F32) for i in range(4)]

    for t in range(NTH):
        i = t % 2
        rows = slice(t * 128, (t + 1) * 128)
        st.load("sync", xt[i][:], T["xh"][rows, :], f"xt{i}")
        st.load("sync", sgm[i][:], scr["SIGM"][rows, :], f"sgm{i}")
        st.dma_percore(ncores, lambda e, k, i=i, t=t: e.dma_start(out=on[i][:], in_=scr["ONSA"][(k % 2) * TH + t * 128:(k % 2) * TH + (t + 1) * 128, :]),
                       f"on{i}", writes=[f"on{i}"])
        st.dma_percore(ncores, lambda e, k, i=i, t=t: e.dma_start(out=og[i][:], in_=scr["OGLA"][(k % 2) * TH + t * 128:(k % 2) * TH + (t + 1) * 128, :]),
                       f"og{i}", writes=[f"og{i}"])
        for j in range(4):
            st.do("tensor", "transpose", out=pT[0][:, j, :], in_=on[i][:, j * 128:(j + 1) * 128], identity=identb[:],
                  reads=[f"on{i}", "identb"], writes=["pT0"])
        for j in range(4):
            st.do("tensor", "transpose", out=pT[0][:, 4 + j, :], in_=og[i][:, j * 128:(j + 1) * 128], identity=identb[:],
                  reads=[f"og{i}", "identb"], writes=["pT0"])
        st.do("scalar", "copy", out=onT[i][:], in_=pT[0][:], reads=["pT0"], writes=[f"onT{i}"])
        for hf in range(2):
            cs_ = slice(hf * 512, (hf + 1) * 512)
            for j in range(4):
                st.do("tensor", "matmul", py[hf][:], onT[i][:, j, :], Wpn[:, j, cs_], start=(j == 0), stop=(j == 3),
                      reads=[f"onT{i}", "Wpn"], writes=[f"py{hf}"])
            for j in range(4):
                st.do("tensor", "matmul", py[2 + hf][:], onT[i][:, 4 + j, :], Wpg[:, j, cs_], start=(j == 0), stop=(j == 3),
                      reads=[f"onT{i}", "Wpg"], writes=[f"py{2 + hf}"])
            st.do("vector", "tensor_tensor", out=m1[:, cs_], in0=py[hf][:], in1=sgm[i][:, hf * 512:(hf + 1) * 512], op=ALU.mult,
                  reads=[f"py{hf}", f"sgm{i}"], writes=[("m1", hf)])
            st.do("vector", "tensor_tensor", out=m2[:, cs_], in0=py[2 + hf][:], in1=sgm[i][:, 1024 + hf * 512:1024 + (hf + 1) * 512], op=ALU.mult,
                  reads=[f"py{2 + hf}", f"sgm{i}"], writes=[("m2", hf)])
            st.do("gpsimd", "tensor_tensor", out=mx_[:, cs_], in0=m1[:, cs_], in1=m2[:, cs_], op=ALU.add,
                  reads=[("m1", hf), ("m2", hf)], writes=[("mixed", hf)])
        for kt in range(8):
            st.do("tensor", "transpose", out=pT[1][:, kt, :], in_=mx_[:, kt * 128:(kt + 1) * 128], identity=identb[:],
                  reads=[("mixed", kt // 4), "identb"], writes=["pT1"])
        st.do("scalar", "copy", out=mT[:], in_=pT[1][:], reads=["pT1"], writes=["mT"])
        for hf in range(2):
            cs_ = slice(hf * 512, (hf + 1) * 512)
            for kt in range(8):
                st.do("tensor", "matmul", py[hf][:], mT[:, kt, :], Wo[:, kt, cs_], start=(kt == 0), stop=(kt == 7),
                      reads=["mT", "Wo"], writes=[f"py{hf}"])
            st.do("vector", "tensor_tensor", out=h1[:, t, cs_], in0=py[hf][:], in1=xt[i][:, cs_], op=ALU.add,
                  reads=[f"py{hf}", f"xt{i}"], writes=[("h1", t, hf)])
        st.do("scalar", "activation", out=sq[i][:], in_=h1[:, t, :], func=AF.Square, accum_out=ss[i][:],
              reads=[("h1", t, 0), ("h1", t, 1)], writes=[f"sq{i}", f"ss{i}"])
        st.do("vector", "tensor_scalar", out=rs[i][:], in0=ss[i][:], scalar1=1.0 / D, scalar2=1e-6, op0=ALU.mult, op1=ALU.add,
              reads=[f"ss{i}"], writes=[f"rs{i}"])
        st.do("scalar", "activation", out=rs[i][:], in_=rs[i][:], func=AF.Sqrt, reads=[f"rs{i}"], writes=[f"rs{i}"])
        st.do("vector", "reciprocal", out=rs[i][:], in_=rs[i][:], reads=[f"rs{i}"], writes=[f"rs{i}"])
        st.do("vector", "scalar_tensor_tensor", out=vnb[:], in0=h1[:, t, :], scalar=rs[i][:, 0:1], in1=gfb[:], op0=ALU.mult, op1=ALU.mult,
              reads=[("h1", t, 0), ("h1", t, 1), f"rs{i}", "gfb"], writes=["vn"])
        for kt in range(8):
            st.do("tensor", "transpose", out=pT[0][:, kt, :], in_=vnb[:, kt * 128:(kt + 1) * 128], identity=identb[:],
                  reads=["vn", "identb"], writes=["pT0"])
        st.do("scalar", "copy", out=vT[:, :, rows], in_=pT[0][:], reads=["pT0"], writes=[("vT", t, 0), ("vT", t, 1)])
        for kt in range(8):
            st.do("tensor", "matmul", py[2][:, 0:36], vT[:, kt, rows], Wr[:, kt, :], start=(kt == 0), stop=(kt == 7),
                  reads=[("vT", t, 0), ("vT", t, 1), "Wr"], writes=["py2"])
        st.do("vector", "tensor_tensor", out=lg[:], in0=py[2][:, 0:36], in1=br[:], op=ALU.add, reads=["py2", "br"], writes=["lg"])
        st.do("vector", "reduce_max", out=sm[:, 0:1], in_=lg[:, 0:4], axis=AX.X, reads=["lg"], writes=["sm0"])
        st.do("vector", "tensor_scalar", out=bg[:], in0=lg[:, 0:4], scalar1=sm[:, 0:1], scalar2=-NEG, op0=ALU.is_ge, op1=ALU.mult,
              reads=["lg", "sm0"], writes=["bg"])
        st.do("vector", "tensor_scalar", out=bg[:], in0=bg[:], scalar1=NEG, scalar2=None, op0=ALU.add, reads=["bg"], writes=["bg"])
        st.do("vector", "tensor_scalar", out=sm[:, 1:2], in0=sm[:, 0:1], scalar1=-1.0, scalar2=None, op0=ALU.mult, reads=["sm0"], writes=["sm1"])
        st.do("scalar", "activation", out=eg[:], in_=lg[:, 0:4], func=AF.Exp, bias=sm[:, 1:2], accum_out=sm[:, 2:3],
              reads=["lg", "sm1"], writes=["eg", "sm2"])
        st.do("vector", "tensor_tensor", out=lem[:].rearrange("p (g e) -> p g e", e=8), in0=lg[:, 4:36].rearrange("p (g e) -> p g e", e=8),
              in1=bg[:].unsqueeze(2).broadcast_to([128, 4, 8]), op=ALU.add, reads=["lg", "bg"], writes=["lem"])
        st.do("vector", "max", out=top8[:], in_=lem[:], reads=["lem"], writes=["top8"])
        st.do("vector", "tensor_scalar", out=msk[:], in0=lem[:], scalar1=top8[:, 1:2], scalar2=None, op0=ALU.is_ge, reads=["lem", "top8"], writes=["msk"])
        st.do("vector", "tensor_scalar", out=sm[:, 3:4], in0=top8[:, 0:1], scalar1=-1.0, scalar2=None, op0=ALU.mult, reads=["top8"], writes=["sm3"])
        st.do("scalar", "activation", out=ex[:], in_=lem[:], func=AF.Exp, bias=sm[:, 3:4], reads=["lem", "sm3"], writes=["ex"])
        st.do("vector", "tensor_tensor", out=ex[:], in0=ex[:], in1=msk[:], op=ALU.mult, reads=["ex", "msk"], writes=["ex"])
        st.do("vector", "reduce_sum", out=sm[:, 4:5], in_=ex[:], axis=AX.X, reads=["ex"], writes=["sm4"])
        st.do("vector", "tensor_tensor", out=sm[:, 5:6], in0=sm[:, 4:5], in1=sm[:, 2:3], op=ALU.mult, reads=["sm4", "sm2"], writes=["sm5"])
        st.do("vector", "reciprocal", out=sm[:, 5:6], in_=sm[:, 5:6], reads=["sm5"], writes=["sm5"])
        st.do("vector", "tensor_scalar", out=comb[:], in0=ex[:], scalar1=sm[:, 5:6], scalar2=None, op0=ALU.mult, reads=["ex", "sm5"], writes=["comb"])
        st.do("vector", "tensor_copy", out=combb[:], in_=comb[:], reads=["comb"], writes=["combb"])
        st.do("tensor", "transpose", out=pT[1][0:32, 0, :], in_=combb[:], identity=identb[:], reads=["combb", "identb"], writes=["pT1"])
        st.do("scalar", "copy", out=combT[:, rows], in_=pT[1][0:32, 0, :], reads=["pT1"], writes=[("combT", t)])
    st.emit()


def stage_E2(nc, S, T, C, scr, P, out):
    st = Stage(nc, "E2")
    TH = S // 2
    NTH = TH // 128
    CH = min(512, TH)
    NCH = TH // CH
    TPC = CH // 128
    h1, vT, combT = P["h1"], P["vT"], P["combT"]
    identb = st.sb("identb", [128, 128], BF16)
    st.dma("gpsimd", lambda e: e.dma_start(out=identb[:], in_=C["ident"]), "identb", writes=["identb"])
    selall = st.sb("selall", [32, 32, 128], BF16)
    st.do("vector", "tensor_copy", out=selall[:], in_=identb[0:32, 0:32].unsqueeze(2).broadcast_to([32, 32, 128]),
          reads=["identb"], writes=["selall"])
    Wg = [st.sb(f"Wg{i}", [128, 8, 512], BF16) for i in range(2)]
    Wu = [st.sb(f"Wu{i}", [128, 8, 512], BF16) for i in range(2)]
    Wd = [st.sb(f"Wd{i}", [128, 4, D], BF16) for i in range(2)]
    cB = [st.sb(f"cB{i}", [128, CH], BF16) for i in range(2)]
    sgl = [st.sb(f"sgl{i}", [128, CH], BF16) for i in range(2)]
    tu = [st.sb(f"tu{i}", [128, CH], BF16) for i in range(2)]
    hT = [st.sb(f"hT{i}", [128, 4, CH], BF16) for i in range(2)]
    pcb = st.ps("pcb", [128, 512], F32)
    pG = [st.ps(f"pG{i}", [128, 512], F32) for i in range(2)]
    pU = [st.ps(f"pU{i}", [128, 512], F32) for i in range(2)]
    py = [st.ps(f"py{i}", [128, 512], F32) for i in range(2)]
    gu = 0
    hb = 0
    def load_expert(ex_):
        w = ex_ % 2
        st.dma("gpsimd", lambda e, w=w, ex_=ex_: e.dma_start(out=Wg[w][:], in_=T["w_gate"][0, ex_].rearrange("(kt p) f -> p kt f", p=128)),
               f"Wg{w}", writes=[f"Wg{w}"])
        st.dma("gpsimd", lambda e, w=w, ex_=ex_: e.dma_start(out=Wu[w][:], in_=T["w_up"][0, ex_].rearrange("(kt p) f -> p kt f", p=128)),
               f"Wu{w}", writes=[f"Wu{w}"])
        st.dma("gpsimd", lambda e, w=w, ex_=ex_: e.dma_start(out=Wd[w][:], in_=T["w_down"][0, ex_].rearrange("(fc p) d -> p fc d", p=128)),
               f"Wd{w}", writes=[f"Wd{w}"])

    load_expert(0)
    for ex_ in range(32):
        w = ex_ % 2
        if ex_ + 1 < 32:
            load_expert(ex_ + 1)
        for tc in range(NCH):
            tsl = slice(tc * CH, (tc + 1) * CH)
            cbi = hb % 2
            hb += 1
            ckeys = [("combT", tc * TPC + j) for j in range(TPC)]
            vkeys = [("vT", tc * TPC + j, q) for j in range(TPC) for q in range(2)]
            st.do("tensor", "matmul", pcb[:, 0:CH], selall[:, ex_, :], combT[:, tsl], start=True, stop=True,
                  reads=["selall"] + ckeys, writes=["pcb"])
            st.do("scalar", "copy", out=cB[cbi][:], in_=pcb[:, 0:CH], reads=["pcb"], writes=[f"cB{cbi}"])
            for fc in range(4):
                g = gu % 2
                gu += 1
                fsl = slice(fc * 128, (fc + 1) * 128)
                for kt in range(8):
                    st.do("tensor", "matmul", pG[g][:, 0:CH], Wg[w][:, kt, fsl], vT[:, kt, tsl], start=(kt == 0), stop=(kt == 7),
                          reads=[f"Wg{w}"] + vkeys, writes=[f"pG{g}"])
                for kt in range(8):
                    st.do("tensor", "matmul", pU[g][:, 0:CH], Wu[w][:, kt, fsl], vT[:, kt, tsl], start=(kt == 0), stop=(kt == 7),
                          reads=[f"Wu{w}"] + vkeys, writes=[f"pU{g}"])
                st.do("scalar", "activation", out=sgl[g][:], in_=pG[g][:, 0:CH], func=AF.Silu, reads=[f"pG{g}"], writes=[f"sgl{g}"])
                st.do("vector", "tensor_tensor", out=tu[g][:], in0=pU[g][:, 0:CH], in1=cB[cbi][:], op=ALU.mult,
                      reads=[f"pU{g}", f"cB{cbi}"], writes=[f"tu{g}"])
                st.do("gpsimd", "tensor_tensor", out=hT[cbi][:, fc, :], in0=sgl[g][:], in1=tu[g][:], op=ALU.mult,
                      reads=[f"sgl{g}", f"tu{g}"], writes=[(f"hT{cbi}", fc)])
            for tt in range(TPC):
                t = tc * TPC + tt
                for hf in range(2):
                    cs_ = slice(hf * 512, (hf + 1) * 512)
                    for fc in range(4):
                        st.do("tensor", "matmul", py[hf][:], hT[cbi][:, fc, tt * 128:(tt + 1) * 128], Wd[w][:, fc, cs_],
                              start=(fc == 0), stop=(fc == 3), reads=[(f"hT{cbi}", f) for f in range(4)] + [f"Wd{w}"], writes=[f"py{hf}"])
                    st.do("vector", "tensor_tensor", out=h1[:, t, cs_], in0=py[hf][:], in1=h1[:, t, cs_], op=ALU.add,
                          reads=[f"py{hf}", ("h1", t, hf)], writes=[("h1", t, hf)])
    gfin = st.sb("gfin", [128, D], F32)
    st.load("sync", gfin[:], bcast_rows(T["g_final"], 128), "gfin")
    sq = [st.sb(f"sq{i}", [128, D], BF16) for i in range(2)]
    ss = [st.sb(f"ss{i}", [128, 1], F32) for i in range(2)]
    rs = [st.sb(f"rs{i}", [128, 1], F32) for i in range(2)]
    ot = [st.sb(f"ot{i}", [128, D], F32) for i in range(2)]
    for t in range(NTH):
        i = t % 2
        st.do("scalar", "activation", out=sq[i][:], in_=h1[:, t, :], func=AF.Square, accum_out=ss[i][:],
              reads=[("h1", t, 0), ("h1", t, 1)], writes=[f"sq{i}", f"ss{i}"])
        st.do("vector", "tensor_scalar", out=rs[i][:], in0=ss[i][:], scalar1=1.0 / D, scalar2=1e-6, op0=ALU.mult, op1=ALU.add,
              reads=[f"ss{i}"], writes=[f"rs{i}"])
        st.do("scalar", "activation", out=rs[i][:], in_=rs[i][:], func=AF.Sqrt, reads=[f"rs{i}"], writes=[f"rs{i}"])
        st.do("vector", "reciprocal", out=rs[i][:], in_=rs[i][:], reads=[f"rs{i}"], writes=[f"rs{i}"])
        st.do("vector", "scalar_tensor_tensor", out=ot[i][:], in0=h1[:, t, :], scalar=rs[i][:, 0:1], in1=gfin[:], op0=ALU.mult, op1=ALU.mult,
              reads=[("h1", t, 0), ("h1", t, 1), f"rs{i}", "gfin"], writes=[f"ot{i}"])
        st.store("sync", out[t * 128:(t + 1) * 128, :], ot[i][:], f"ot{i}")
    st.emit()


def build_full(S, ncores, stages="RABCDE", debug=(), scr_in=()):
    nc = bass.Bass("TRN2", target_bir_lowering=False)
    T = {k: nc.dram_tensor(k, shp, dt, kind="ExternalInput").ap() for k, (shp, dt) in IN_SHAPES(S).items()}
    C = {k: nc.dram_tensor("c_" + k, shp, F32, kind="ExternalInput").ap() for k, shp in CONST_SHAPES.items()}
    scr = {}
    spec = dict(SCR_SPEC(S))
    spec["SIGM"] = ([S // 2, 2048], BF16)
    for k, (shp, dt) in spec.items():
        kind = "ExternalOutput" if k in debug else ("ExternalInput" if k in scr_in else "Internal")
        scr[k] = nc.dram_tensor("scr_" + k, shp, dt, kind=kind).ap()
    out = nc.dram_tensor("out", [S // 2, D], F32, kind="ExternalOutput").ap()
    TH = S // 2
    with nc.allow_non_contiguous_dma(reason="small strided parameter loads"), ExitStack() as es:
        if "R" in stages:
            stage_R(nc, S, T, C, scr)
        if "A" in stages:
            stage_A(nc, S, T, C, scr)
        if "B" in stages:
            stage_B(nc, S, T, C, scr)
        if "C" in stages:
            stage_C(nc, S, T, C, scr)
        if "D" in stages:
            stage_D(nc, S, T, C, scr)
        if "E" in stages:
            if "0" in DBG_E:
                stage_E0(nc, S, T, C, scr)
            P = {"h1": es.enter_context(nc.sbuf_tensor("P_h1", [128, TH // 128, D], F32)),
                 "vT": es.enter_context(nc.sbuf_tensor("P_vT", [128, 8, TH], BF16)),
                 "combT": es.enter_context(nc.sbuf_tensor("P_combT", [32, TH], BF16))}
            if "1" in DBG_E:
                stage_E1(nc, S, T, C, scr, P, ncores)
            if "2" in DBG_E:
                stage_E2(nc, S, T, C, scr, P, out)
    return nc


SEQ = 4096
BATCH = 4
_CACHE = {}


def kernel(**inputs):
    S = SEQ
    ncores = 8
    if "nc" not in _CACHE:
        _CACHE["nc"] = build_full(S, ncores)
    nc = _CACHE["nc"]
    consts = host_consts(S)
    shapes = IN_SHAPES(S)
    x = np.ascontiguousarray(np.asarray(inputs["x"], dtype=np.float32))
    pos = np.ascontiguousarray(np.asarray(inputs["positions"]).astype(np.int32))
    in_maps = []
    for core in range(ncores):
        b, half = core // 2, core % 2
        m = {}
        for k, (shp, dt) in shapes.items():
            if k == "x":
                m[k] = x[b]
            elif k == "xh":
                m[k] = np.ascontiguousarray(x[b, half * S // 2:(half + 1) * S // 2])
            elif k == "positions":
                m[k] = pos[b:b + 1]
            elif k == "halfidx":
                m[k] = np.array([[half]], np.int32)
            elif k == "g_final":
                m[k] = np.asarray(inputs[k], dtype=np.float32).reshape(1, -1)
            else:
                m[k] = np.ascontiguousarray(np.asarray(inputs[k], dtype=np.float32))
        for k, v in consts.items():
            m["c_" + k] = v
        in_maps.append(m)
    res = run_bass_kernel_spmd(nc, in_maps, core_ids=list(range(ncores)))
    out = np.empty((BATCH, S, D), np.float32)
    for core in range(ncores):
        b, half = core // 2, core % 2
        out[b, half * S // 2:(half + 1) * S // 2] = res.results[core]["out"]
    return out
```

```python
import math
import numpy as np
from contextlib import ExitStack
import concourse.bass as bass
import concourse.mybir as mybir
from concourse.bass_utils import run_bass_kernel_spmd

F32 = mybir.dt.float32
BF16 = mybir.dt.bfloat16
I32 = mybir.dt.int32
AF = mybir.ActivationFunctionType
ALU = mybir.AluOpType
AX = mybir.AxisListType

SAME_ENGINE_SYNC = True
EPOCH = 12000
NEG = -30000.0
DBG_BR = None
DBG_D = 3
DBG_NOIF = False
DBG_E = '012'
D = 1024


class Stage:
    ENGS = ("tensor", "vector", "scalar", "gpsimd", "sync")

    def __init__(self, nc, name):
        self.nc = nc
        self.name = name
        self.es = ExitStack()
        self.ops = {e: [] for e in self.ENGS}
        self.last_write = {}
        self.readers = {}
        self.seen = {e: {} for e in self.ENGS}
        self.sems = {}
        self.cnt = {}
        self.epoch = {e: 0 for e in self.ENGS}
        self.dma_sems_by_eng = {e: set() for e in self.ENGS}
        self.nalloc = 0

    def sb(self, name, shape, dtype):
        return self.es.enter_context(self.nc.sbuf_tensor(f"{self.name}_{name}", list(shape), dtype))

    def ps(self, name, shape, dtype=F32):
        return self.es.enter_context(self.nc.psum_tensor(f"{self.name}_{name}", list(shape), dtype))

    def _sem(self, semkey):
        if semkey not in self.sems:
            self.nalloc += 1
            self.sems[semkey] = self.nc.alloc_semaphore(name=f"{self.name}_s{self.nalloc}")
            self.cnt[semkey] = 0
        return self.sems[semkey]

    def _deps(self, eng, reads, writes):
        need = {}

        def want(c):
            if c is None:
                return
            sk, v = c
            if need.get(sk, 0) < v:
                need[sk] = v

        for r in reads:
            want(self.last_write.get(r))
        for w in writes:
            want(self.last_write.get(w))
            for c in self.readers.get(w, ()):
                want(c)
        waits = []
        for sk, v in need.items():
            if (not SAME_ENGINE_SYNC or eng == "tensor") and sk[0] == "E" and sk[1] == eng:
                continue
            if self.seen[eng].get(sk, 0) >= v:
                continue
            self.seen[eng][sk] = v
            waits.append((sk, v))
        return waits

    def _commit(self, comp, reads, writes):
        for w in writes:
            self.last_write[w] = comp
            self.readers[w] = []
        for r in reads:
            if r in writes:
                continue
            self.readers.setdefault(r, []).append(comp)

    def op(self, eng, fn, reads=(), writes=()):
        reads = list(reads)
        writes = list(writes)
        waits = self._deps(eng, reads, writes)
        sk = ("E", eng, self.epoch[eng])
        self._sem(sk)
        self.cnt[sk] += 1
        comp = (sk, self.cnt[sk])
        if self.cnt[sk] >= EPOCH:
            self.epoch[eng] += 1
        self.ops[eng].append((fn, waits, sk, 1))
        self._commit(comp, reads, writes)
        return comp

    def dma(self, eng, fn, semkey, reads=(), writes=()):
        reads = list(reads)
        writes = list(writes)
        waits = self._deps(eng, reads, writes)
        sk = ("D", semkey)
        self._sem(sk)
        self.cnt[sk] += 16
        comp = (sk, self.cnt[sk])
        self.ops[eng].append((fn, waits, sk, 16))
        self.dma_sems_by_eng[eng].add(sk)
        self._commit(comp, reads, writes)
        return comp

    def do(self, eng, method, *args, reads=(), writes=(), **kw):
        return self.op(eng, lambda e: getattr(e, method)(*args, **kw), reads=reads, writes=writes)

    def dma_percore(self, ncores, mk, semkey, reads=(), writes=()):
        if DBG_NOIF:
            return self.dma("gpsimd", lambda e: mk(e, 0), semkey, reads=reads, writes=writes)
        sk = ("D", semkey)
        self._sem(sk)

        class _Done:
            def then_inc(self_, *a):
                return self_

        def fn(e):
            if getattr(self, "_pid", None) is None:
                self._pid = e.partition_id()
            pid = self._pid
            for k in range(ncores):
                with e.If(pid == k):
                    mk(e, k).then_inc(self.sems[sk], 16)
            return _Done()

        return self.dma("gpsimd", fn, semkey, reads=reads, writes=writes)

    def load(self, eng, out_ap, in_ap, key):
        return self.dma(eng, lambda e: e.dma_start(out=out_ap, in_=in_ap), key, writes=[key])

    def store(self, eng, out_ap, in_ap, key):
        return self.dma(eng, lambda e: e.dma_start(out=out_ap, in_=in_ap), key, reads=[key])

    def emit(self):
        nc = self.nc
        with nc.Block() as block:
            for ename in self.ENGS:
                ops = self.ops[ename]
                if not ops:
                    continue
                final = []
                for sk in self.dma_sems_by_eng[ename]:
                    final.append((sk, self.cnt[sk]))
                for ep in range(self.epoch[ename] + 1):
                    sk = ("E", ename, ep)
                    if sk in self.cnt and self.cnt[sk] > 0:
                        final.append((sk, self.cnt[sk]))

                def body(e, ops=ops, final=final):
                    for fn, waits, sk, inc in ops:
                        for wk, wv in waits:
                            e.wait_ge(self.sems[wk], wv)
                        fn(e).then_inc(self.sems[sk], inc)
                    for wk, wv in final:
                        e.wait_ge(self.sems[wk], wv)

                getattr(block, ename)(body)
        self.nc.clear_and_free_semaphores(list(self.sems.values()))
        self.nc.all_engine_barrier()
        self.es.close()


def host_consts(S):
    c = {}
    c["ident"] = np.eye(128, dtype=np.float32)
    half = 32
    inv = 1.0 / (10000.0 ** (np.arange(half, dtype=np.float32) / half))
    inv64 = np.concatenate([inv, inv]).astype(np.float32)
    c["invs"] = (np.concatenate([inv64, inv64]) / np.float32(2 * math.pi)).astype(np.float32).reshape(128, 1)
    tl = np.arange(128)[:, None]
    npr = np.arange(-1, 7)[None, :]
    c["cmpbias"] = np.where(16 * npr + 31 <= tl, 0.0, NEG).astype(np.float32)
    kk = np.arange(128)[None, :]
    c["tri"] = np.where(kk <= tl, 0.0, NEG).astype(np.float32)
    kw = np.arange(640)[None, :]
    c["winbias"] = np.where((kw > tl) & (kw <= tl + 512), 0.0, NEG).astype(np.float32)
    n = np.arange(256)[:, None]
    j = np.arange(64)[None, :]
    ov = np.clip(np.minimum(16 * n + 32, 64 * j + 64) - np.maximum(16 * n, 64 * j), 0, None) / 32.0
    ov[255] = 0.0
    c["overlap"] = ov.astype(np.float32)
    K = np.zeros((128, 3), np.float32)
    B = np.zeros((128, 3), np.float32)
    lo = np.arange(128) < 64
    K[lo] = [0, 0, 0]
    B[lo] = [1e4, 1e4, -1e30]
    K[~lo] = [1, 0, 0]
    B[~lo] = [0, 1e4, 1e4]
    c["selK"] = K
    c["selB"] = B
    c["rowvalid"] = (np.arange(128) >= 31).astype(np.float32).reshape(128, 1)
    s_ = np.arange(64)[:, None]
    t_ = np.arange(64)[None, :]
    c["glamask"] = (s_ <= t_).astype(np.float32)
    return c


CONST_SHAPES = {"ident": [128, 128], "invs": [128, 1], "cmpbias": [128, 8], "tri": [128, 128],
                "winbias": [128, 640], "overlap": [256, 64], "selK": [128, 3], "selB": [128, 3],
                "rowvalid": [128, 1], "glamask": [64, 64]}

WA_COLS = 3624
SRC = dict(q=0, kc=512, vc=640, ks=768, vs=896, kw=1024, vw=1152, gates=1280, gq=1304, gk=1560,
           gv=1816, al=2328, gr=2344, ma=2856, mb=3880)
DST = dict(q=0, qR=512, ks=1024, ksR=1152, kw=1280, kwR=1408, kc=1536, vc=1664, gq=1792, gk=2048,
           al=2304, vs=2320, vw=2448, gates=2576, gv=2600, gr=3112)


def bcast_rows(ap_row, nparts):
    return ap_row.broadcast_to([nparts, ap_row.shape[-1]])


def stage_R(nc, S, T, C, scr):
    st = Stage(nc, "R")
    pos = T["positions"]
    invs = st.sb("invs", [64, 1], F32)
    cosT = st.sb("cosT", [64, S], F32)
    sinT = st.sb("sinT", [64, S], F32)
    posi = st.sb("posi", [64, S], I32)
    tq = st.sb("tq", [64, S], F32)
    t2 = st.sb("t2", [64, S], F32)
    t3 = st.sb("t3", [64, S], F32)
    ni = st.sb("ni", [64, S], I32)
    st.load("sync", invs[:], C["invs"][0:64, :], "invs")
    st.load("sync", posi[:], bcast_rows(pos, 64), "posi")
    st.op("vector", lambda e: e.tensor_copy(out=tq[:], in_=posi[:]), reads=["posi"], writes=["tq"])
    st.op("vector", lambda e: e.tensor_scalar(out=tq[:], in0=tq[:], scalar1=invs[:, 0:1], scalar2=None, op0=ALU.mult),
          reads=["tq", "invs"], writes=["tq"])

    def table(dst, shift):
        st.op("vector", lambda e: e.tensor_scalar(out=t2[:], in0=tq[:], scalar1=float(shift), scalar2=None, op0=ALU.add),
              reads=["tq"], writes=["t2"])
        st.op("vector", lambda e: e.tensor_copy(out=ni[:], in_=t2[:]), reads=["t2"], writes=["ni"])
        st.op("vector", lambda e: e.tensor_copy(out=t3[:], in_=ni[:]), reads=["ni"], writes=["t3"])
        st.op("vector", lambda e: e.tensor_tensor(out=t2[:], in0=t2[:], in1=t3[:], op=ALU.subtract),
              reads=["t2", "t3"], writes=["t2"])
        st.op("vector", lambda e: e.tensor_scalar(out=t3[:], in0=t2[:], scalar1=0.5, scalar2=None, op0=ALU.is_gt),
              reads=["t2"], writes=["t3"])
        st.op("vector", lambda e: e.tensor_tensor(out=t2[:], in0=t2[:], in1=t3[:], op=ALU.subtract),
              reads=["t2", "t3"], writes=["t2"])
        st.op("vector", lambda e: e.tensor_scalar(out=t3[:], in0=t2[:], scalar1=-0.5, scalar2=None, op0=ALU.is_lt),
              reads=["t2"], writes=["t3"])
        st.op("vector", lambda e: e.tensor_tensor(out=t2[:], in0=t2[:], in1=t3[:], op=ALU.add),
              reads=["t2", "t3"], writes=["t2"])
        st.op("scalar", lambda e: e.activation(out=dst[:], in_=t2[:], func=AF.Sin, scale=6.283185),
              reads=["t2"], writes=[dst.name])

    table(sinT, 0.0)
    table(cosT, 0.25)
    st.store("sync", scr["SIN"], sinT[:], sinT.name)
    st.store("sync", scr["COS"], cosT[:], cosT.name)
    st.emit()


def stage_A(nc, S, T, C, scr):
    st = Stage(nc, "A")
    SH = S // 2
    NT = SH // 128
    NC4 = SH // 512
    x, w_in = T["x"], T["w_in"]
    uT = st.sb("uT", [128, 8, SH], BF16)
    WA = st.sb("WA", [128, 8, WA_COLS], BF16)
    ident = st.sb("ident", [128, 128], BF16)
    gm = st.sb("gm", [128, 8], F32)
    cosT = st.sb("cosT", [128, SH], F32)
    sinT = st.sb("sinT", [128, SH], F32)
    st.dma("gpsimd", lambda e: e.dma_start(out=ident[:], in_=C["ident"]), "ident", writes=["ident"])
    st.dma("sync", lambda e: e.dma_start(out=gm[:], in_=T["g_mix"].rearrange("o (kt p) -> p (o kt)", p=128)),
           "gm", writes=["gm"])

    wst = [st.sb(f"wst{i}", [128, 2856], F32) for i in range(2)]
    WASUB = {}
    for kt in range(8):
        i = kt % 2
        wk = f"wst{i}"
        st.load("sync" if kt % 2 == 0 else "gpsimd", wst[i][:], w_in[0, kt * 128:(kt + 1) * 128, 0:2856], wk)
        g = gm[:, kt:kt + 1]
        wkey = ("WA", kt)

        def cp(eng, dst0, src0, n, mul=1.0, i=i, kt=kt, g=g, wk=wk, wkey=wkey):
            wkey = ("WA", kt, dst0)
            WASUB.setdefault(kt, []).append(wkey)
            st.op(eng, lambda e: e.tensor_scalar(out=WA[:, kt, dst0:dst0 + n], in0=wst[i][:, src0:src0 + n],
                                                 scalar1=g, scalar2=float(mul), op0=ALU.mult, op1=ALU.mult),
                  reads=[wk, "gm"], writes=[wkey])

        def rot(eng, dst0, src0, nh, mul=1.0, i=i, kt=kt, g=g, wk=wk, wkey=wkey):
            wkey = ("WA", kt, dst0)
            WASUB.setdefault(kt, []).append(wkey)
            s4 = wst[i][:, src0:src0 + nh * 64].rearrange("p (h two j) -> p h two j", two=2, j=32)
            d4 = WA[:, kt, dst0:dst0 + nh * 64].rearrange("p (h two j) -> p h two j", two=2, j=32)
            st.op(eng, lambda e: e.tensor_scalar(out=d4[:, :, 0, :], in0=s4[:, :, 1, :], scalar1=g, scalar2=float(-mul),
                                                 op0=ALU.mult, op1=ALU.mult), reads=[wk, "gm"], writes=[wkey])
            st.op(eng, lambda e: e.tensor_scalar(out=d4[:, :, 1, :], in0=s4[:, :, 0, :], scalar1=g, scalar2=float(mul),
                                                 op0=ALU.mult, op1=ALU.mult), reads=[wk, "gm"], writes=[wkey])

        cp("vector", DST["q"], SRC["q"], 512, 0.125)
        rot("gpsimd", DST["qR"], SRC["q"], 8, 0.125)
        cp("vector", DST["ks"], SRC["ks"], 128)
        rot("gpsimd", DST["ksR"], SRC["ks"], 2)
        cp("vector", DST["kw"], SRC["kw"], 128)
        rot("gpsimd", DST["kwR"], SRC["kw"], 2)
        cp("vector", DST["kc"], SRC["kc"], 256)
        cp("gpsimd", DST["gq"], SRC["gq"], 256, 0.125)
        cp("gpsimd", DST["gk"], SRC["gk"], 256)
        cp("vector", DST["al"], SRC["al"], 16)
        cp("vector", DST["vs"], SRC["vs"], 128)
        cp("vector", DST["vw"], SRC["vw"], 128)
        cp("vector", DST["gates"], SRC["gates"], 24)
        cp("gpsimd", DST["gv"], SRC["gv"], 512)
        cp("vector", DST["gr"], SRC["gr"], 512)
    WAK = [("WA", kt) for kt in range(8)]

    xt = [st.sb(f"xt{i}", [128, D], F32) for i in range(2)]
    sq = [st.sb(f"sq{i}", [128, D], BF16) for i in range(2)]
    xn = [st.sb(f"xn{i}", [128, D], BF16) for i in range(2)]
    ss = [st.sb(f"ss{i}", [128, 1], F32) for i in range(2)]
    rs = [st.sb(f"rs{i}", [128, 1], F32) for i in range(2)]
    ptr = [st.ps(f"ptr{i}", [128, 8, 128], BF16) for i in range(2)]
    pa = [st.ps(f"pa{i}", [128, 512], F32) for i in range(2)]
    pb = [st.ps(f"pb{i}", [128, 512], F32) for i in range(2)]
    r1 = [st.sb(f"r1{i}", [128, 512], F32) for i in range(2)]
    r2 = [st.sb(f"r2{i}", [128, 512], F32) for i in range(2)]
    fo = [st.sb(f"fo{i}", [128, 512], BF16) for i in range(3)]
    pt = [st.ps(f"pt{i}", [128, 512], F32) for i in range(2)]
    tA = [st.sb(f"tA{i}", [128, 256], BF16) for i in range(2)]
    tG = [st.sb(f"tG{i}", [128, 24], F32) for i in range(2)]
    tV = [st.sb(f"tV{i}", [128, 512], BF16) for i in range(2)]
    tR = [st.sb(f"tR{i}", [128, 512], BF16) for i in range(2)]
    for hs in range(2):
        H0 = hs * SH
        for hh in range(2):
            st.load("sync", cosT[hh * 64:(hh + 1) * 64, :], scr["COS"][:, H0:H0 + SH], cosT.name)
            st.load("sync", sinT[hh * 64:(hh + 1) * 64, :], scr["SIN"][:, H0:H0 + SH], sinT.name)
        for t in range(NT):
            i = t % 2
            st.load("sync", xt[i][:], x[H0 + t * 128:H0 + (t + 1) * 128, :], f"xt{i}")
            st.op("scalar", lambda e, i=i: e.activation(out=sq[i][:], in_=xt[i][:], func=AF.Square, accum_out=ss[i][:]),
                  reads=[f"xt{i}"], writes=[f"sq{i}", f"ss{i}"])
            st.op("vector", lambda e, i=i: e.tensor_scalar(out=rs[i][:], in0=ss[i][:], scalar1=1.0 / D, scalar2=1e-6,
                                                           op0=ALU.mult, op1=ALU.add), reads=[f"ss{i}"], writes=[f"rs{i}"])
            st.op("scalar", lambda e, i=i: e.activation(out=rs[i][:], in_=rs[i][:], func=AF.Sqrt),
                  reads=[f"rs{i}"], writes=[f"rs{i}"])
            st.op("vector", lambda e, i=i: e.reciprocal(out=rs[i][:], in_=rs[i][:]), reads=[f"rs{i}"], writes=[f"rs{i}"])
            st.op("vector", lambda e, i=i: e.tensor_scalar(out=xn[i][:], in0=xt[i][:], scalar1=rs[i][:, 0:1], scalar2=None,
                                                           op0=ALU.mult), reads=[f"xt{i}", f"rs{i}"], writes=[f"xn{i}"])
            for kt in range(8):
                st.op("tensor", lambda e, i=i, kt=kt: e.transpose(out=ptr[i][:, kt, :], in_=xn[i][:, kt * 128:(kt + 1) * 128],
                                                                  identity=ident[:]),
                      reads=[f"xn{i}", "ident"], writes=[f"ptr{i}"])
            st.op("scalar", lambda e, i=i, t=t: e.copy(out=uT[:, :, t * 128:(t + 1) * 128], in_=ptr[i][:]),
                  reads=[f"ptr{i}"], writes=[("uT", t)])

        rope_groups = [(DST["q"] + 128 * p, DST["qR"] + 128 * p, scr["QT"], 128 * p) for p in range(4)]
        rope_groups += [(DST["ks"], DST["ksR"], scr["KST"], 0), (DST["kw"], DST["kwR"], scr["KWT"], 0)]
        plain_groups = [(DST["kc"], 128, scr["KCT"], 0), (DST["vc"], 128, scr["VCT"], 0),
                        (DST["gq"], 128, scr["GQT"], 0), (DST["gq"] + 128, 128, scr["GQT"], 128),
                        (DST["gk"], 128, scr["GKT"], 0), (DST["gk"] + 128, 128, scr["GKT"], 128),
                        (DST["al"], 16, scr["ALT"], 0)]
        it = 0
        fi = 0
        for tc in range(NC4):
            tsl = slice(tc * 512, (tc + 1) * 512)
            ukeys = [("uT", tc * 4 + j) for j in range(4)]
            for (ca, cb, dst, row0) in rope_groups:
                i = it % 2
                it += 1
                f = fi % 3
                fi += 1
                for kt in range(8):
                    st.op("tensor", lambda e, i=i, kt=kt, ca=ca, tsl=tsl: e.matmul(pa[i][:], WA[:, kt, ca:ca + 128], uT[:, kt, tsl],
                                                                          start=(kt == 0), stop=(kt == 7)),
                          reads=ukeys + WASUB[kt], writes=[f"pa{i}"])
                for kt in range(8):
                    st.op("tensor", lambda e, i=i, kt=kt, cb=cb, tsl=tsl: e.matmul(pb[i][:], WA[:, kt, cb:cb + 128], uT[:, kt, tsl],
                                                                          start=(kt == 0), stop=(kt == 7)),
                          reads=ukeys + WASUB[kt], writes=[f"pb{i}"])
                st.op("vector", lambda e, i=i, tsl=tsl: e.tensor_tensor(out=r1[i][:], in0=pa[i][:], in1=cosT[:, tsl], op=ALU.mult),
                      reads=[f"pa{i}", cosT.name], writes=[f"r1{i}"])
                st.op("vector", lambda e, i=i, tsl=tsl: e.tensor_tensor(out=r2[i][:], in0=pb[i][:], in1=sinT[:, tsl], op=ALU.mult),
                      reads=[f"pb{i}", sinT.name], writes=[f"r2{i}"])
                st.op("gpsimd", lambda e, i=i, f=f: e.tensor_tensor(out=fo[f][:], in0=r1[i][:], in1=r2[i][:], op=ALU.add),
                      reads=[f"r1{i}", f"r2{i}"], writes=[f"fo{f}"])
                st.store("sync", dst[row0:row0 + 128, H0 + tc * 512:H0 + (tc + 1) * 512], fo[f][:], f"fo{f}")
            for (ca, n, dst, row0) in plain_groups:
                i = it % 2
                it += 1
                f = fi % 3
                fi += 1
                for kt in range(8):
                    st.op("tensor", lambda e, i=i, kt=kt, ca=ca, n=n, tsl=tsl: e.matmul(pa[i][0:n, :], WA[:, kt, ca:ca + n], uT[:, kt, tsl],
                                                                               start=(kt == 0), stop=(kt == 7)),
                          reads=ukeys + WASUB[kt], writes=[f"pa{i}"])
                st.op("scalar", lambda e, i=i, f=f, n=n: e.copy(out=fo[f][0:n, :], in_=pa[i][0:n, :]),
                      reads=[f"pa{i}"], writes=[f"fo{f}"])
                st.store("sync", dst[row0:row0 + n, H0 + tc * 512:H0 + (tc + 1) * 512], fo[f][0:n, :], f"fo{f}")

        it = 0
        for t in range(NT):
            rows = slice(t * 128, (t + 1) * 128)
            drows = slice(H0 + t * 128, H0 + (t + 1) * 128)
            b = t % 2
            for (c0, n, kind) in [(DST["vs"], 280, 0), (DST["gv"], 512, 1), (DST["gr"], 512, 2)]:
                i = it % 2
                it += 1
                for kt in range(8):
                    st.op("tensor", lambda e, i=i, kt=kt, c0=c0, n=n, rows=rows: e.matmul(pt[i][:, 0:n], uT[:, kt, rows], WA[:, kt, c0:c0 + n],
                                                                               start=(kt == 0), stop=(kt == 7)),
                          reads=[("uT", t)] + WASUB[kt], writes=[f"pt{i}"])
                if kind == 0:
                    st.op("vector", lambda e, i=i, b=b: e.tensor_copy(out=tA[b][:], in_=pt[i][:, 0:256]),
                          reads=[f"pt{i}"], writes=[f"tA{b}"])
                    st.op("scalar", lambda e, i=i, b=b: e.activation(out=tG[b][:], in_=pt[i][:, 256:280], func=AF.Sigmoid),
                          reads=[f"pt{i}"], writes=[f"tG{b}"])
                    st.store("gpsimd", scr["VS"][drows, :], tA[b][:, 0:128], f"tA{b}")
                    st.store("gpsimd", scr["VW"][drows, :], tA[b][:, 128:256], f"tA{b}")
                    st.store("gpsimd", scr["GATE"][drows, :], tG[b][:], f"tG{b}")
                elif kind == 1:
                    st.op("vector", lambda e, i=i, b=b: e.tensor_copy(out=tV[b][:], in_=pt[i][:]),
                          reads=[f"pt{i}"], writes=[f"tV{b}"])
                    st.store("gpsimd", scr["GV"][drows, :], tV[b][:], f"tV{b}")
                else:
                    st.op("scalar", lambda e, i=i, b=b: e.activation(out=tR[b][:], in_=pt[i][:], func=AF.Silu),
                          reads=[f"pt{i}"], writes=[f"tR{b}"])
                    st.store("gpsimd", scr["GR"][drows, :], tR[b][:], f"tR{b}")

    st.emit()


SCR_SPEC = lambda S: {
    "SIN": ([64, S], F32), "COS": ([64, S], F32),
    "QT": ([512, S], BF16), "KST": ([128, S], BF16), "KWT": ([128, S], BF16),
    "KCT": ([128, S], BF16), "VCT": ([128, S], BF16), "GQT": ([256, S], BF16), "GKT": ([256, S], BF16),
    "ALT": ([16, S], BF16), "VS": ([S, 128], BF16), "VW": ([S, 128], BF16), "GATE": ([S, 24], F32),
    "GV": ([S, 512], BF16), "GR": ([S, 512], BF16),
    "KCMPT": ([128, S // 16], BF16), "VCMP": ([2, S // 16, 64], BF16),
    "ONSA": ([S, 512], BF16), "OGLA": ([S, 512], BF16),
}

IN_SHAPES = lambda S: {
    "x": ([S, D], F32), "xh": ([S // 2, D], F32), "positions": ([1, S], I32), "g_mix": ([1, D], F32),
    "w_in": ([1, D, 4904], F32),
    "cmp_pos_k": ([1, 32, 64], F32), "cmp_w1_k": ([1, 2048, 256], F32), "cmp_b1_k": ([1, 256], F32),
    "cmp_w2_k": ([1, 256, 64], F32), "cmp_b2_k": ([1, 64], F32),
    "cmp_pos_v": ([1, 32, 64], F32), "cmp_w1_v": ([1, 2048, 256], F32), "cmp_b1_v": ([1, 256], F32),
    "cmp_w2_v": ([1, 256, 64], F32), "cmp_b2_v": ([1, 64], F32),
    "gla_w_a2": ([1, 16, 256], F32), "gla_b_a": ([1, 256], F32), "gla_norm_g": ([1, 512], F32),
    "w_proj_nsa": ([1, 512, D], F32), "w_proj_gla": ([1, 512, D], F32), "w_out": ([1, D, D], F32),
    "g_ffn": ([1, D], F32), "w_grp": ([1, D, 4], F32), "b_grp": ([1, 4], F32), "w_exp": ([1, D, 32], F32),
    "b_exp": ([1, 32], F32), "w_gate": ([1, 32, D, 512], F32), "w_up": ([1, 32, D, 512], F32),
    "w_down": ([1, 32, 512, D], F32), "g_final": ([1, D], F32), "halfidx": ([1, 1], I32),
}


def build(S, stages="ABCDE", debug=(), scr_in=(), ncores=2):
    return build_full(S, ncores, stages=("R" + stages) if "A" in stages else stages, debug=debug, scr_in=scr_in)


def stage_B(nc, S, T, C, scr):
    st = Stage(nc, "B")
    NCMP = S // 16 - 1
    srcT = {"k": st.sb("kcT", [128, S], BF16), "v": st.sb("vcT", [128, S], BF16)}
    st.load("sync", srcT["k"][:], scr["KCT"], "kcT")
    st.load("sync", srcT["v"][:], scr["VCT"], "vcT")
    cosf = st.sb("cosf", [64, S], F32)
    sinf = st.sb("sinf", [64, S], F32)
    st.load("sync", cosf[:], scr["COS"], "cosf")
    st.load("sync", sinf[:], scr["SIN"], "sinf")
    cos_e = cosf[:, 31:31 + 16 * (NCMP - 1) + 1:16]
    sin_e = sinf[:, 31:31 + 16 * (NCMP - 1) + 1:16]
    ph = [st.ps(f"ph{i}", [128, 512], F32) for i in range(2)]
    pbias = st.ps("pbias", [128, 2], F32)
    pk = [st.ps(f"pk{i}", [64, 512], F32) for i in range(2)]
    pv = st.ps("pv", [128, 64], F32)
    for kv in ("k", "v"):
        W1 = st.sb(f"W1{kv}", [128, 32, 256], BF16)
        w1src = T[f"cmp_w1_{kv}"][0].rearrange("(i d) h -> d i h", d=64)
        st.dma("gpsimd", lambda e, W1=W1, w1src=w1src: e.dma_start(out=W1[0:64], in_=w1src), f"W1{kv}", writes=[f"W1{kv}"])
        st.dma("gpsimd", lambda e, W1=W1, w1src=w1src: e.dma_start(out=W1[64:128], in_=w1src), f"W1{kv}", writes=[f"W1{kv}"])
        posf = st.sb(f"posf{kv}", [64, 32], F32)
        posb = st.sb(f"posb{kv}", [64, 32], BF16)
        st.load("sync", posf[:], T[f"cmp_pos_{kv}"][0].rearrange("i d -> d i"), f"posf{kv}")
        st.do("vector", "tensor_copy", out=posb[:], in_=posf[:], reads=[f"posf{kv}"], writes=[f"posb{kv}"])
        b1 = st.sb(f"b1{kv}", [128, 2], F32)
        st.load("sync", b1[:], T[f"cmp_b1_{kv}"].rearrange("o (hh p) -> p (o hh)", p=128), f"b1{kv}")
        w2f = st.sb(f"w2f{kv}", [128, 2, 64], F32)
        st.load("sync", w2f[:], T[f"cmp_w2_{kv}"][0].rearrange("(hh p) d -> p hh d", p=128), f"w2f{kv}")
        W2 = st.sb(f"W2{kv}", [128, 2, 64], BF16)
        st.do("vector", "tensor_copy", out=W2[:], in_=w2f[:], reads=[f"w2f{kv}"], writes=[f"W2{kv}"])
        bias1 = st.sb(f"bias1{kv}", [128, 2], F32)
        for hh in range(2):
            for i in range(32):
                st.do("tensor", "matmul", pbias[:, hh:hh + 1], W1[0:64, i, hh * 128:(hh + 1) * 128], posb[:, i:i + 1],
                      start=(i == 0), stop=(i == 31), reads=[f"W1{kv}", f"posb{kv}"], writes=["pbias"])
        st.do("vector", "tensor_tensor", out=bias1[:], in0=pbias[:], in1=b1[:], op=ALU.add,
              reads=["pbias", f"b1{kv}"], writes=[f"bias1{kv}"])
        if kv == "k":
            W2R = st.sb("W2R", [128, 2, 64], BF16)
            for hh in range(2):
                st.do("vector", "tensor_scalar", out=W2R[:, hh, 0:32], in0=w2f[:, hh, 32:64], scalar1=-1.0, scalar2=None,
                      op0=ALU.mult, reads=["w2fk"], writes=["W2R"])
                st.do("vector", "tensor_copy", out=W2R[:, hh, 32:64], in_=w2f[:, hh, 0:32], reads=["w2fk"], writes=["W2R"])
            b2 = st.sb("b2k", [64, 1], F32)
            b2R = st.sb("b2R", [64, 1], F32)
            b2src = T["cmp_b2_k"].rearrange("o d -> d o")
            st.load("sync", b2[:], b2src, "b2k")
            st.load("sync", b2R[0:32], b2src[32:64], "b2R")
            st.load("sync", b2R[32:64], b2src[0:32], "b2R")
            st.do("vector", "tensor_scalar", out=b2R[0:32], in0=b2R[0:32], scalar1=-1.0, scalar2=None, op0=ALU.mult,
                  reads=["b2R"], writes=["b2R"])
        else:
            b2v = st.sb("b2v", [128, 64], F32)
            st.load("sync", b2v[:], bcast_rows(T["cmp_b2_v"], 128), "b2v")
        for g in range(2):
            gp = slice(g * 64, (g + 1) * 64)
            h1T = [st.sb(f"h1T{kv}{g}{hh}", [128, 256 if NCMP <= 256 else NCMP], BF16) for hh in range(2)]
            for hh in range(2):
                hk = f"h1T{kv}{g}{hh}"
                for i in range(32):
                    st.do("tensor", "matmul", ph[hh][:, 0:NCMP], W1[gp, i, hh * 128:(hh + 1) * 128],
                          srcT[kv][gp, i:i + 16 * (NCMP - 1) + 1:16], start=(i == 0), stop=(i == 31),
                          reads=[f"W1{kv}", f"{kv}cT"], writes=[f"ph{hh}"])
                xh = st.sb(f"xh{kv}{g}{hh}", [128, NCMP], F32)
                t1 = st.sb(f"t1{kv}{g}{hh}", [128, NCMP], F32)
                xk, tk = f"xh{kv}{g}{hh}", f"t1{kv}{g}{hh}"
                st.do("scalar", "activation", out=xh[:], in_=ph[hh][:, 0:NCMP], func=AF.Identity, bias=bias1[:, hh:hh + 1],
                      reads=[f"ph{hh}", f"bias1{kv}"], writes=[xk])
                st.do("vector", "tensor_tensor", out=t1[:], in0=xh[:], in1=xh[:], op=ALU.mult, reads=[xk], writes=[tk])
                st.do("vector", "tensor_scalar", out=t1[:], in0=t1[:], scalar1=0.044715, scalar2=1.0, op0=ALU.mult, op1=ALU.add,
                      reads=[tk], writes=[tk])
                st.do("vector", "tensor_tensor", out=t1[:], in0=t1[:], in1=xh[:], op=ALU.mult, reads=[tk, xk], writes=[tk])
                st.do("scalar", "activation", out=t1[:], in_=t1[:], func=AF.Tanh, scale=0.7978845608028654,
                      reads=[tk], writes=[tk])
                st.do("vector", "tensor_scalar", out=t1[:], in0=t1[:], scalar1=1.0, scalar2=0.5, op0=ALU.add, op1=ALU.mult,
                      reads=[tk], writes=[tk])
                st.do("vector", "tensor_tensor", out=h1T[hh][:, 0:NCMP], in0=t1[:], in1=xh[:], op=ALU.mult,
                      reads=[tk, xk], writes=[hk])
            hks = [f"h1T{kv}{g}{hh}" for hh in range(2)]
            if kv == "k":
                for hh in range(2):
                    st.do("tensor", "matmul", pk[0][:, 0:NCMP], W2[:, hh, :], h1T[hh][:, 0:NCMP], start=(hh == 0), stop=(hh == 1),
                          reads=hks + ["W2k"], writes=["pk0"])
                for hh in range(2):
                    st.do("tensor", "matmul", pk[1][:, 0:NCMP], W2R[:, hh, :], h1T[hh][:, 0:NCMP], start=(hh == 0), stop=(hh == 1),
                          reads=hks + ["W2R"], writes=["pk1"])
                ka = st.sb(f"ka{g}", [64, NCMP], F32)
                kb = st.sb(f"kb{g}", [64, NCMP], F32)
                ko = st.sb(f"ko{g}", [64, NCMP], BF16)
                st.do("scalar", "activation", out=ka[:], in_=pk[0][:, 0:NCMP], func=AF.Identity, bias=b2[:, 0:1],
                      reads=["pk0", "b2k"], writes=[f"ka{g}"])
                st.do("scalar", "activation", out=kb[:], in_=pk[1][:, 0:NCMP], func=AF.Identity, bias=b2R[:, 0:1],
                      reads=["pk1", "b2R"], writes=[f"kb{g}"])
                st.do("vector", "tensor_tensor", out=ka[:], in0=ka[:], in1=cos_e, op=ALU.mult, reads=[f"ka{g}", "cosf"], writes=[f"ka{g}"])
                st.do("vector", "tensor_tensor", out=kb[:], in0=kb[:], in1=sin_e, op=ALU.mult, reads=[f"kb{g}", "sinf"], writes=[f"kb{g}"])
                st.do("vector", "tensor_tensor", out=ko[:], in0=ka[:], in1=kb[:], op=ALU.add, reads=[f"ka{g}", f"kb{g}"], writes=[f"ko{g}"])
                st.store("sync", scr["KCMPT"][gp, 0:NCMP], ko[:], f"ko{g}")
            else:
                for ci, n0 in enumerate(range(0, NCMP, 128)):
                    n = min(128, NCMP - n0)
                    for hh in range(2):
                        st.do("tensor", "matmul", pv[0:n, :], h1T[hh][:, n0:n0 + n], W2[:, hh, :], start=(hh == 0), stop=(hh == 1),
                              reads=hks + ["W2v"], writes=["pv"])
                    vo = st.sb(f"vo{g}{ci}", [128, 64], BF16)
                    st.do("vector", "tensor_tensor", out=vo[0:n, :], in0=pv[0:n, :], in1=b2v[0:n, :], op=ALU.add,
                          reads=["pv", "b2v"], writes=[f"vo{g}{ci}"])
                    st.store("sync", scr["VCMP"][g, n0:n0 + n, :], vo[0:n, :], f"vo{g}{ci}")
    st.emit()


def stage_C(nc, S, T, C, scr):
    st = Stage(nc, "C")
    NT = S // 128
    NSLC = S // 64
    NCP = S // 16
    NCH = (NCP + 127) // 128
    SW = 640
    ident = st.sb("ident", [128, 128], BF16)
    st.dma("gpsimd", lambda e: e.dma_start(out=ident[:], in_=C["ident"]), "ident", writes=["ident"])
    ovl = st.sb("ovl", [128, NCH, NSLC], BF16)
    for j in range(NCH):
        n = min(128, NCP - j * 128)
        st.dma("gpsimd", lambda e, j=j, n=n: e.dma_start(out=ovl[0:n, j, :], in_=C["overlap"][j * 128:j * 128 + n, 0:NSLC]),
               "ovl", writes=["ovl"])
    cst = {}
    for k in ("cmpbias", "tri", "winbias", "selK", "selB", "rowvalid"):
        cst[k] = st.sb("c_" + k, CONST_SHAPES[k], F32)
        st.load("sync", cst[k][:], C[k], "c_" + k)
    gates = st.sb("gates", [128, NT, 24], F32)
    st.load("sync", gates[:], scr["GATE"].rearrange("(c p) k -> p c k", p=128), "gates")

    ps = [st.ps(f"ps{i}", [128, 512], F32) for i in range(3)]
    pT = [st.ps(f"pT{i}", [128, 8, 128], BF16) for i in range(2)]
    po = st.ps("po", [128, 512], F32)
    po2 = st.ps("po2", [128, 512], F32)
    pimp = st.ps("pimp", [128, 512], F32)

    S_sb = [st.sb(f"S_sb{i}", [128, S], F32) for i in range(4)]
    P_sb = [st.sb(f"P_sb{i}", [128, S], BF16) for i in range(4)]
    PT_sb = [st.sb(f"PT_sb{i}", [128, NT, 128], BF16) for i in range(2)]
    Sw = [st.sb(f"Sw{i}", [128, SW], F32) for i in range(4)]
    Pw = [st.sb(f"Pw{i}", [128, SW], BF16) for i in range(4)]
    PTw = [st.sb(f"PTw{i}", [128, 5, 128], BF16) for i in range(2)]
    mx = [st.sb(f"mx{i}", [128, 1], F32) for i in range(4)]
    mxw = [st.sb(f"mxw{i}", [128, 1], F32) for i in range(4)]
    sums = [st.sb(f"sums{i}", [128, 12], F32) for i in range(2)]
    rr = [st.sb(f"rr{i}", [128, 12], F32) for i in range(2)]
    impS = st.sb("impS", [128, NSLC], F32)
    imp2 = st.sb("imp2", [128, NSLC], F32)
    m8a = st.sb("m8a", [128, 8], F32)
    m8b = st.sb("m8b", [128, 8], F32)
    mb = st.sb("mb", [128, NSLC], F32)
    acc = st.sb("acc", [128, 256], F32)
    oout = [st.sb(f"oout{i}", [128, 256], BF16) for i in range(2)]
    QTb = [st.sb(f"QTb{i}", [64, 4, 128], BF16) for i in range(2)]
    KS = st.sb("KS", [64, S], BF16)
    KW = st.sb("KW", [64, S], BF16)
    VS = st.sb("VS", [128, NT, 64], BF16)
    VW = st.sb("VW", [128, NT, 64], BF16)
    KC = st.sb("KC", [64, NCP], BF16)
    VC = st.sb("VC", [128, NCH, 64], BF16)
    cnt = {"tb": 0, "sb": 0, "cp": 0}

    def next_ps():
        i = cnt["sb"] % 3
        cnt["sb"] += 1
        return i

    def tail_group(items, act_only=False):
        rounds = []
        for it in items:
            L = it[4]
            nkt = (L + 127) // 128
            for k0 in range(0, nkt, 8):
                rounds.append((it, k0, min(8, nkt - k0), nkt))

        def emit_T(r):
            (Pt, pkey, PTt, ptkey, L, Vt, vkey, po_ap, pokey, extra), k0, nb, nkt = r
            b = cnt["tb"] % 2
            cnt["tb"] += 1
            for kk in range(nb):
                kt = k0 + kk
                nj = min(128, L - kt * 128)
                st.do("tensor", "transpose", out=pT[b][0:nj, kk, :], in_=Pt[:, kt * 128:kt * 128 + nj], identity=ident[:],
                      reads=[pkey, "ident"], writes=[f"pT{b}"])
            njl = min(128, L - (k0 + nb - 1) * 128)
            cnt["cp"] += 1
            full = nb if njl == 128 else nb - 1
            if act_only or cnt["cp"] % 2 == 0:
                eng, meth = "scalar", "copy"
            else:
                eng, meth = "vector", "tensor_copy"
            if full > 0:
                st.do(eng, meth, out=PTt[:, k0:k0 + full, :], in_=pT[b][:, 0:full, :], reads=[f"pT{b}"], writes=[(ptkey, k0, 0)])
            if full < nb:
                st.do(eng, meth, out=PTt[0:njl, k0 + nb - 1, :], in_=pT[b][0:njl, nb - 1, :], reads=[f"pT{b}"], writes=[(ptkey, k0, 1)])

        def emit_PV(r):
            (Pt, pkey, PTt, ptkey, L, Vt, vkey, po_ap, pokey, extra), k0, nb, nkt = r
            for kk in range(nb):
                kt = k0 + kk
                nj = min(128, L - kt * 128)
                st.do("tensor", "matmul", po_ap, PTt[0:nj, kt, :], Vt(kt, nj), start=(kt == 0), stop=(kt == nkt - 1),
                      reads=[(ptkey, k0, 0), (ptkey, k0, 1), vkey], writes=[pokey])
                if extra is not None:
                    extra(kt, nj, nkt)

        for i, r in enumerate(rounds):
            emit_T(r)
            if i >= 1:
                emit_PV(rounds[i - 1])
        emit_PV(rounds[-1])

    for g in range(2):
        gp = slice(g * 64, (g + 1) * 64)
        st.load("sync", KS[:], scr["KST"][gp, :], "KS")
        st.load("sync", KW[:], scr["KWT"][gp, :], "KW")
        st.load("gpsimd", VS[:], scr["VS"][:, gp].rearrange("(kt p) d -> p kt d", p=128), "VS")
        st.load("gpsimd", VW[:], scr["VW"][:, gp].rearrange("(kt p) d -> p kt d", p=128), "VW")
        st.load("sync", KC[:, 0:NCP - 1], scr["KCMPT"][gp, 0:NCP - 1], "KC")
        st.do("vector", "memset", VC[:], 0.0, reads=[], writes=["VC"])
        for j in range(NCH):
            n = min(128, NCP - 1 - j * 128)
            st.load("sync", VC[0:n, j, :], scr["VCMP"][g, j * 128:j * 128 + n, :], "VC")

        for c in range(NT):
            cblk = slice(c * 128, (c + 1) * 128)
            cb = c % 2
            qb = c % 2
            qk = f"QTb{qb}"
            if c == 0:
                st.load("sync", QTb[0][:], scr["QT"][g * 256:(g + 1) * 256, 0:128].rearrange("(h d) t -> d h t", d=64), "QTb0")
            if c + 1 < NT:
                st.load("sync", QTb[(c + 1) % 2][:],
                        scr["QT"][g * 256:(g + 1) * 256, (c + 1) * 128:(c + 2) * 128].rearrange("(h d) t -> d h t", d=64), f"QTb{(c + 1) % 2}")
            ncmp = 8 * c + 7
            ncp32 = ((ncmp + 31) // 32) * 32
            for h in range(4):
                si = next_ps()
                st.do("tensor", "matmul", ps[si][:, 0:ncmp], QTb[qb][:, h, :], KC[:, 0:ncmp], start=True, stop=True,
                      reads=[qk, "KC"], writes=[f"ps{si}"])
                if ncmp > 8:
                    st.do("scalar", "copy", out=Sw[h][:, 0:ncmp - 8], in_=ps[si][:, 0:ncmp - 8], reads=[f"ps{si}"], writes=[(f"Sw{h}", 0), (f"Sw{h}", 1)])
                    st.do("vector", "tensor_tensor", out=Sw[h][:, ncmp - 8:ncmp], in0=ps[si][:, ncmp - 8:ncmp],
                          in1=cst["cmpbias"][:, 0:8], op=ALU.add, reads=[f"ps{si}", "c_cmpbias"], writes=[(f"Sw{h}", 0), (f"Sw{h}", 1)])
                else:
                    st.do("vector", "tensor_tensor", out=Sw[h][:, 0:7], in0=ps[si][:, 0:7],
                          in1=cst["cmpbias"][:, 1:8], op=ALU.add, reads=[f"ps{si}", "c_cmpbias"], writes=[(f"Sw{h}", 0), (f"Sw{h}", 1)])
            for h in range(4):
                st.do("vector", "reduce_max", out=mxw[h][:], in_=Sw[h][:, 0:ncmp], axis=AX.X, negate=True, reads=[(f"Sw{h}", 0), (f"Sw{h}", 1)], writes=[f"mxw{h}"])
            for h in range(4):
                sidx = h * 3
                st.do("scalar", "activation", out=Sw[h][:, 0:ncmp], in_=Sw[h][:, 0:ncmp], func=AF.Exp, bias=mxw[h][:, 0:1],
                      accum_out=sums[cb][:, sidx:sidx + 1], reads=[(f"Sw{h}", 0), (f"Sw{h}", 1), f"mxw{h}"], writes=[(f"Sw{h}", 0), (f"Sw{h}", 1), (f"sums{cb}", sidx)])
            for h in range(4):
                sidx = h * 3
                st.do("vector", "reciprocal", out=mxw[h][:], in_=sums[cb][:, sidx:sidx + 1], reads=[(f"sums{cb}", sidx)], writes=[f"mxw{h}"])
                if c == 0:
                    st.do("vector", "tensor_tensor", out=mxw[h][:], in0=mxw[h][:], in1=cst["rowvalid"][:], op=ALU.mult,
                          reads=[f"mxw{h}", "c_rowvalid"], writes=[f"mxw{h}"])
            for h in range(4):
                st.do("gpsimd", "memset", Pw[h][:, ncmp:ncp32], 0.0, reads=[], writes=[f"Pw{h}"])
                st.do("vector", "tensor_scalar", out=Pw[h][:, 0:ncmp], in0=Sw[h][:, 0:ncmp], scalar1=mxw[h][:, 0:1], scalar2=None,
                      op0=ALU.mult, reads=[(f"Sw{h}", 0), (f"Sw{h}", 1), f"mxw{h}"], writes=[f"Pw{h}"])
            items = []
            for h in range(4):
                def extra(kt, nj, nkt, h=h):
                    st.do("tensor", "matmul", pimp[:, 0:NSLC], PTw[h % 2][0:nj, kt, :], ovl[0:nj, kt, :],
                          start=(h == 0 and kt == 0), stop=(h == 3 and kt == nkt - 1), reads=[(f"PTw{h % 2}", 0, 0), (f"PTw{h % 2}", 0, 1), "ovl"], writes=["pimp"])
                items.append((Pw[h], f"Pw{h}", PTw[h % 2], f"PTw{h % 2}", ncp32, lambda kt, nj: VC[0:nj, kt, :], "VC",
                              po[:, h * 64:(h + 1) * 64], "po", extra))
            tail_group(items)
            use_sel = (2 * c + 2) > 16
            if use_sel:
                st.do("vector", "tensor_copy", out=impS[:], in_=pimp[:, 0:NSLC], reads=["pimp"], writes=["impS"])
                if 2 * c + 2 < NSLC:
                    st.do("vector", "memset", impS[:, 2 * c + 2:NSLC], -1e30, reads=[], writes=["impS"])
                lo = 2 * c - 1
                st.do("vector", "tensor_tensor", out=impS[:, lo:lo + 3], in0=impS[:, lo:lo + 3], in1=cst["selK"][:, 0:3], op=ALU.mult,
                      reads=["impS", "c_selK"], writes=["impS"])
                st.do("vector", "tensor_tensor", out=impS[:, lo:lo + 3], in0=impS[:, lo:lo + 3], in1=cst["selB"][:, 0:3], op=ALU.add,
                      reads=["impS", "c_selB"], writes=["impS"])
                st.do("vector", "memset", impS[:, 0:1], 1e4, reads=[], writes=["impS"])
                st.do("vector", "max", out=m8a[:], in_=impS[:], reads=["impS"], writes=["m8a"])
                st.do("vector", "match_replace", out=imp2[:], in_to_replace=m8a[:], in_values=impS[:], imm_value=-3e38,
                      reads=["impS", "m8a"], writes=["imp2"])
                st.do("vector", "max", out=m8b[:], in_=imp2[:], reads=["imp2"], writes=["m8b"])
                st.do("vector", "tensor_scalar", out=mb[:], in0=impS[:], scalar1=m8b[:, 7:8], scalar2=-NEG, op0=ALU.is_ge, op1=ALU.mult,
                      reads=["impS", "m8b"], writes=["mb"])
                st.do("vector", "tensor_scalar", out=mb[:], in0=mb[:], scalar1=NEG, scalar2=None, op0=ALU.add,
                      reads=["mb"], writes=["mb"])
            L = 128 * (c + 1)
            k0w = max(0, 128 * c - 512)
            Lw = L - k0w
            boff = k0w - (128 * c - 512)
            kt0 = k0w // 128

            def slc_p1():
                for h in range(4):
                    sk = f"S_sb{h}"
                    for k0 in range(0, L, 512):
                        w = min(512, L - k0)
                        si = next_ps()
                        st.do("tensor", "matmul", ps[si][:, 0:w], QTb[qb][:, h, :], KS[:, k0:k0 + w], start=True, stop=True,
                              reads=[qk, "KS"], writes=[f"ps{si}"])
                        if use_sel:
                            nb = w // 64
                            mbb = mb[:, k0 // 64:k0 // 64 + nb].unsqueeze(2).broadcast_to([128, nb, 64])
                            st.do("vector", "tensor_tensor", out=S_sb[h][:, k0:k0 + w].rearrange("p (b k) -> p b k", k=64),
                                  in0=ps[si][:, 0:w].rearrange("p (b k) -> p b k", k=64), in1=mbb, op=ALU.add,
                                  reads=[f"ps{si}", "mb"], writes=[(sk, k0 // 512)])
                        else:
                            st.do("scalar", "copy", out=S_sb[h][:, k0:k0 + w], in_=ps[si][:, 0:w], reads=[f"ps{si}"], writes=[(sk, k0 // 512)])
                    lk = (sk, (L - 128) // 512)
                    st.do("vector", "tensor_tensor", out=S_sb[h][:, L - 128:L], in0=S_sb[h][:, L - 128:L], in1=cst["tri"][:], op=ALU.add,
                          reads=[lk, "c_tri"], writes=[lk])

            def slc_p2():
                for h in range(4):
                    st.do("vector", "reduce_max", out=mx[h][:], in_=S_sb[h][:, 0:L], axis=AX.X, negate=True,
                          reads=[(f"S_sb{h}", k) for k in range((L + 511) // 512)], writes=[f"mx{h}"])

            def slc_p3():
                for h in range(4):
                    sidx = h * 3 + 1
                    st.do("scalar", "activation", out=P_sb[h][:, 0:L], in_=S_sb[h][:, 0:L], func=AF.Exp, bias=mx[h][:, 0:1],
                          accum_out=sums[cb][:, sidx:sidx + 1], reads=[(f"S_sb{h}", k) for k in range((L + 511) // 512)] + [f"mx{h}"],
                          writes=[f"P_sb{h}", (f"sums{cb}", sidx)])

            def slc_p4():
                tail_group([(P_sb[h], f"P_sb{h}", PT_sb[h % 2], f"PT_sb{h % 2}", L, lambda kt, nj: VS[:, kt, :], "VS",
                             po[:, 256 + h * 64:256 + (h + 1) * 64], "po", None) for h in range(4)], act_only=True)

            def win_p1():
                for h in range(4):
                    for k0 in range(0, Lw, 512):
                        w = min(512, Lw - k0)
                        si = next_ps()
                        st.do("tensor", "matmul", ps[si][:, 0:w], QTb[qb][:, h, :], KW[:, k0w + k0:k0w + k0 + w], start=True, stop=True,
                              reads=[qk, "KW"], writes=[f"ps{si}"])
                        st.do("vector", "tensor_tensor", out=Sw[h][:, k0:k0 + w], in0=ps[si][:, 0:w],
                              in1=cst["winbias"][:, boff + k0:boff + k0 + w], op=ALU.add, reads=[f"ps{si}", "c_winbias"], writes=[(f"Sw{h}", k0 // 512)])

            def win_p2():
                for h in range(4):
                    st.do("vector", "reduce_max", out=mxw[h][:], in_=Sw[h][:, 0:Lw], axis=AX.X, negate=True, reads=[(f"Sw{h}", 0), (f"Sw{h}", 1)], writes=[f"mxw{h}"])

            def win_p3():
                for h in range(4):
                    sidx = h * 3 + 2
                    st.do("scalar", "activation", out=Pw[h][:, 0:Lw], in_=Sw[h][:, 0:Lw], func=AF.Exp, bias=mxw[h][:, 0:1],
                          accum_out=sums[cb][:, sidx:sidx + 1], reads=[(f"Sw{h}", 0), (f"Sw{h}", 1), f"mxw{h}"], writes=[f"Pw{h}", (f"sums{cb}", sidx)])

            def win_p4():
                tail_group([(Pw[h], f"Pw{h}", PTw[h % 2], f"PTw{h % 2}", Lw, lambda kt, nj, kt0=kt0: VW[:, kt0 + kt, :], "VW",
                             po2[:, h * 64:(h + 1) * 64], "po_win", None) for h in range(4)])

            win_p1()
            slc_p1()
            win_p2()
            win_p3()
            slc_p2()
            slc_p3()
            win_p4()
            slc_p4()
            sumkeys = [(f"sums{cb}", i) for i in range(12)]
            st.do("vector", "reciprocal", out=rr[cb][:], in_=sums[cb][:], reads=sumkeys, writes=[f"rr{cb}"])
            st.do("vector", "memset", rr[cb][:].rearrange("p (h b) -> p h b", b=3)[:, :, 0], 1.0, reads=[], writes=[f"rr{cb}"])
            st.do("vector", "tensor_tensor", out=rr[cb][:], in0=rr[cb][:], in1=gates[:, c, 12 * g:12 * g + 12], op=ALU.mult,
                  reads=[f"rr{cb}", "gates"], writes=[f"rr{cb}"])
            ob = c % 2
            for h in range(4):
                hs = slice(h * 64, (h + 1) * 64)
                st.do("vector", "tensor_scalar", out=acc[:, hs], in0=po[:, hs], scalar1=rr[cb][:, 3 * h:3 * h + 1], scalar2=None,
                      op0=ALU.mult, reads=["po", f"rr{cb}"], writes=[("acc", h)])
            for h in range(4):
                hs = slice(h * 64, (h + 1) * 64)
                st.do("vector", "scalar_tensor_tensor", out=acc[:, hs], in0=po[:, 256 + h * 64:256 + (h + 1) * 64],
                      scalar=rr[cb][:, 3 * h + 1:3 * h + 2], in1=acc[:, hs], op0=ALU.mult, op1=ALU.add,
                      reads=["po", f"rr{cb}", ("acc", h)], writes=[("acc", h)])
            for h in range(4):
                hs = slice(h * 64, (h + 1) * 64)
                st.do("vector", "scalar_tensor_tensor", out=oout[ob][:, hs], in0=po2[:, hs],
                      scalar=rr[cb][:, 3 * h + 2:3 * h + 3], in1=acc[:, hs], op0=ALU.mult, op1=ALU.add,
                      reads=["po_win", f"rr{cb}", ("acc", h)], writes=[(f"oout{ob}", h)])
            st.dma("sync", lambda e, cblk=cblk, ob=ob, g=g: e.dma_start(out=scr["ONSA"][cblk, g * 256:(g + 1) * 256], in_=oout[ob][:]),
                   f"oout{ob}", reads=[(f"oout{ob}", h) for h in range(4)])
    st.emit()


def stage_D(nc, S, T, C, scr):
    st = Stage(nc, "D")
    NCK = S // 64
    NC4 = S // 512
    ident = st.sb("ident", [128, 128], BF16)
    st.dma("gpsimd", lambda e: e.dma_start(out=ident[:], in_=C["ident"]), "ident", writes=["ident"])
    gmask = st.sb("gmask", [64, 64], F32)
    st.load("sync", gmask[:], C["glamask"], "gmask")
    alT = st.sb("alT", [16, S], BF16)
    st.load("sync", alT[:], scr["ALT"], "alT")
    wa2 = st.sb("wa2", [16, 256], BF16)
    st.dma("gpsimd", lambda e: e.dma_start(out=wa2[:], in_=T["gla_w_a2"][0]), "wa2", writes=["wa2"])
    rmask = st.sb("rmask", [128, S], BF16)
    st.do("vector", "memset", rmask[:], 1.0, reads=[], writes=["rmask"])
    st.do("vector", "memset", rmask[:, 0:S:64], 0.0, reads=[], writes=["rmask"])

    bufA = st.sb("bufA", [128, S], F32)
    bufB = st.sb("bufB", [128, S], F32)
    qT = st.sb("qT", [128, S], BF16)
    kT = st.sb("kT", [128, S], BF16)
    kdec = st.sb("kdec", [128, S], BF16)
    V64 = st.sb("V64", [64, NCK, 256], BF16)
    Sf = [st.sb(f"Sf{i}", [128, 256], F32) for i in range(2)]
    Sb = st.sb("Sb", [128, NCK, 256], BF16)
    Qbd = st.sb("Qbd", [128, NCK, 128], BF16)
    nb = st.sb("nb", [128, 1], F32)
    gb = st.sb("gb", [128, 128], F32)
    tmp = [st.sb(f"tmp{i}", [128, 512], F32) for i in range(2)]
    pkv_ = [st.ps(f"pkv{i}", [128, 512], F32) for i in range(2)]
    pkv = [p[:, 0:256] for p in pkv_]
    pxa = pkv
    pTk_ = [st.ps(f"pTk{i}", [128, 1024], BF16) for i in range(2)]
    pTk = [pTk_[0][0:64, 0:128], pTk_[1][0:64, 0:128]]
    pA_ = [st.ps(f"pA{i}", [128, 512], F32) for i in range(2)]
    pA = [p[0:64, 0:128] for p in pA_]
    pO_ = [st.ps(f"pO{i}", [128, 512], F32) for i in range(2)]
    pO = [p[:, 0:256] for p in pO_]
    kdT = [st.sb(f"kdT{i}", [64, 128], BF16) for i in range(2)]
    ATs = [st.sb(f"ATs{i}", [64, 128], BF16) for i in range(2)]
    R64 = [st.sb(f"R64{i}", [128, 128], BF16) for i in range(2)]
    gg = [st.sb(f"gg{i}", [128, 128], F32) for i in range(2)]
    sqj = [st.sb(f"sqj{i}", [128, 128], F32) for i in range(2)]
    ssq = [st.sb(f"ssq{i}", [128, 1], F32) for i in range(2)]
    og = [st.sb(f"og{i}", [128, 128], BF16) for i in range(2)]

    for hp in range(2):
        rows = slice(hp * 128, (hp + 1) * 128)
        cols = slice(hp * 256, (hp + 1) * 256)
        st.load("sync", qT[:], scr["GQT"][rows, :], "qT")
        st.load("sync", kT[:], scr["GKT"][rows, :], "kT")
        st.load("gpsimd", V64[:], scr["GV"][:, cols].rearrange("(c s) e -> s c e", s=64), "V64")
        st.load("sync", nb[:], T["gla_b_a"][:, rows].rearrange("o p -> p o"), "nb")
        st.do("vector", "tensor_scalar", out=nb[:], in0=nb[:], scalar1=-1.0, scalar2=None, op0=ALU.mult, reads=["nb"], writes=["nb"])
        for tc in range(S // 256):
            i = tc % 2
            tsl = slice(tc * 256, (tc + 1) * 256)
            st.do("tensor", "matmul", pxa[i], wa2[:, rows], alT[:, tsl], start=True, stop=True,
                  reads=["wa2", "alT"], writes=[f"pkv{i}"])
            st.do("scalar", "activation", out=tmp[i][:, 0:256], in_=pxa[i], func=AF.Exp, bias=nb[:, 0:1], scale=-1.0,
                  reads=[f"pkv{i}", "nb"], writes=[f"tmp{i}"])
            st.do("vector", "tensor_scalar", out=tmp[i][:, 0:256], in0=tmp[i][:, 0:256], scalar1=1.0, scalar2=None, op0=ALU.add,
                  reads=[f"tmp{i}"], writes=[f"tmp{i}"])
            st.do("scalar", "activation", out=bufA[:, tsl], in_=tmp[i][:, 0:256], func=AF.Ln, reads=[f"tmp{i}"], writes=["bufA"])
        st.do("vector", "tensor_tensor_scan", out=bufB[:], data0=rmask[:], data1=bufA[:], initial=0.0, op0=ALU.mult, op1=ALU.add,
              reads=["rmask", "bufA"], writes=["bufB"])
        st.do("scalar", "activation", out=bufA[:], in_=bufB[:], func=AF.Exp, scale=-1.0 / 16.0, reads=["bufB"], writes=["bufA"])
        st.do("scalar", "activation", out=bufB[:], in_=bufB[:], func=AF.Exp, scale=1.0 / 16.0, reads=["bufB"], writes=["bufB"])
        st.do("vector", "tensor_tensor", out=qT[:], in0=qT[:], in1=bufA[:], op=ALU.mult, reads=["qT", "bufA"], writes=["qT"])
        st.do("vector", "tensor_tensor", out=kT[:], in0=kT[:], in1=bufB[:], op=ALU.mult, reads=["kT", "bufB"], writes=["kT"])
        dec = bufA[:, 63:63 + 64 * (NCK - 1) + 1:64]
        st.do("vector", "tensor_tensor", out=kdec[:].rearrange("p (c s) -> p c s", s=64),
              in0=kT[:].rearrange("p (c s) -> p c s", s=64), in1=dec.unsqueeze(2).broadcast_to([128, NCK, 64]), op=ALU.mult,
              reads=["kT", "bufA"], writes=["kdec"])
        st.do("gpsimd", "memset", Qbd[:], 0.0, reads=[], writes=["Qbd"])
        for h in range(2):
            hr = slice(h * 64, (h + 1) * 64)
            st.do("gpsimd", "tensor_copy", out=Qbd[hr, :, h * 64:(h + 1) * 64], in_=qT[hr, :].rearrange("p (c s) -> p c s", s=64),
                  reads=["qT"], writes=["Qbd"])
        st.do("vector", "memset", Sf[0][:], 0.0, reads=[], writes=["Sf0"])
        def rec_T(c):
            i = c % 2
            csl = slice(c * 64, (c + 1) * 64)
            st.do("tensor", "transpose", out=pTk[i], in_=kdec[:, csl], identity=ident[:], reads=["kdec", "ident"], writes=[f"pTk{i}"])
            st.do("scalar", "copy", out=kdT[i][:], in_=pTk[i], reads=[f"pTk{i}"], writes=[f"kdT{i}"])

        rec_T(0)
        for c in range(NCK):
            i = c % 2
            if c + 1 < NCK:
                rec_T(c + 1)
            st.do("tensor", "matmul", pkv[i], kdT[i][:], V64[:, c, :], start=True, stop=True,
                  reads=[f"kdT{i}", "V64"], writes=[f"pkv{i}"])
            st.do("gpsimd", "tensor_copy", out=Sb[:, c, :], in_=Sf[i][:], reads=[f"Sf{i}"], writes=[("Sb", c)])
            st.do("vector", "scalar_tensor_tensor", out=Sf[1 - i][:], in0=Sf[i][:], scalar=dec[:, c:c + 1],
                  in1=pkv[i], op0=ALU.mult, op1=ALU.add, reads=[f"Sf{i}", "bufA", f"pkv{i}"], writes=[f"Sf{1 - i}"])
        if DBG_D == 2:
            continue
        for h in range(2):
            st.load("sync", gb[h * 64:(h + 1) * 64, :], bcast_rows(T["gla_norm_g"][:, hp * 256 + h * 128:hp * 256 + (h + 1) * 128], 64), "gb")
        def out_A(c):
            i = c % 2
            csl = slice(c * 64, (c + 1) * 64)
            st.do("tensor", "matmul", pA[i], kT[:, csl], Qbd[:, c, :], start=True, stop=True, reads=["kT", "Qbd"], writes=[f"pA{i}"])
            st.do("vector", "tensor_tensor", out=ATs[i][:].rearrange("p (h t) -> p h t", h=2), in0=pA[i].rearrange("p (h t) -> p h t", h=2),
                  in1=gmask[:].unsqueeze(1).broadcast_to([64, 2, 64]), op=ALU.mult, reads=[f"pA{i}", "gmask"], writes=[f"ATs{i}"])

        for c in range(NCK):
            i = c % 2
            csl = slice(c * 64, (c + 1) * 64)
            for h in range(2):
                st.load("gpsimd", R64[i][h * 64:(h + 1) * 64, :], scr["GR"][csl, hp * 256 + h * 128:hp * 256 + (h + 1) * 128], (f"R64{i}", h))
            st.do("gpsimd", "tensor_tensor", out=gg[i][:], in0=R64[i][:], in1=gb[:], op=ALU.mult, reads=[(f"R64{i}", 0), (f"R64{i}", 1), "gb"], writes=[f"gg{i}"])
            if c == 0:
                out_A(0)
            if c + 1 < NCK:
                out_A(c + 1)
            st.do("tensor", "matmul", pO[i], Qbd[:, c, :], Sb[:, c, :], start=True, stop=False, reads=["Qbd", ("Sb", c)], writes=[f"pO{i}"])
            st.do("tensor", "matmul", pO[i], ATs[i][:], V64[:, c, :], start=False, stop=True, reads=[f"ATs{i}", "V64"], writes=[f"pO{i}"])
            for h in range(2):
                hr = slice(h * 64, (h + 1) * 64)
                st.do("scalar", "activation", out=sqj[i][hr, :], in_=pO[i][hr, h * 128:(h + 1) * 128], func=AF.Square,
                      accum_out=ssq[i][hr, 0:1], reads=[f"pO{i}"], writes=[(f"sqj{i}", h), (f"ssq{i}", h)])
            st.do("vector", "tensor_scalar", out=ssq[i][:], in0=ssq[i][:], scalar1=1.0 / 128.0, scalar2=1e-6, op0=ALU.mult, op1=ALU.add,
                  reads=[(f"ssq{i}", 0), (f"ssq{i}", 1)], writes=[f"rs{i}"])
            st.do("scalar", "activation", out=ssq[i][:], in_=ssq[i][:], func=AF.Sqrt, reads=[f"rs{i}"], writes=[f"rs{i}"])
            st.do("vector", "reciprocal", out=ssq[i][:], in_=ssq[i][:], reads=[f"rs{i}"], writes=[f"rs{i}", (f"ssq{i}", 0), (f"ssq{i}", 1)])
            for h in range(2):
                hr = slice(h * 64, (h + 1) * 64)
                st.do("vector", "scalar_tensor_tensor", out=og[i][hr, :], in0=pO[i][hr, h * 128:(h + 1) * 128],
                      scalar=ssq[i][hr, 0:1], in1=gg[i][hr, :], op0=ALU.mult, op1=ALU.mult,
                      reads=[f"pO{i}", f"rs{i}", (f"ssq{i}", 0), (f"ssq{i}", 1), f"gg{i}"], writes=[(f"og{i}", h)])
            for h in range(2):
                st.store("sync", scr["OGLA"][csl, hp * 256 + h * 128:hp * 256 + (h + 1) * 128], og[i][h * 64:(h + 1) * 64, :], (f"og{i}", h))
    st.emit()


def rms_rstd(st, src_ap, src_key, sq, ss, rs, i, dim):
    st.do("scalar", "activation", out=sq[i][:], in_=src_ap, func=AF.Square, accum_out=ss[i][:],
          reads=[src_key], writes=[f"sq{i}", f"ss{i}"])
    st.do("vector", "tensor_scalar", out=rs[i][:], in0=ss[i][:], scalar1=1.0 / dim, scalar2=1e-6, op0=ALU.mult, op1=ALU.add,
          reads=[f"ss{i}"], writes=[f"rs{i}"])
    st.do("scalar", "activation", out=rs[i][:], in_=rs[i][:], func=AF.Sqrt, reads=[f"rs{i}"], writes=[f"rs{i}"])
    st.do("vector", "reciprocal", out=rs[i][:], in_=rs[i][:], reads=[f"rs{i}"], writes=[f"rs{i}"])


def stage_E0(nc, S, T, C, scr):
    st = Stage(nc, "E0")
    TH = S // 2
    NTH = TH // 128
    ident = st.sb("ident", [128, 128], BF16)
    st.dma("gpsimd", lambda e: e.dma_start(out=ident[:], in_=C["ident"]), "ident", writes=["ident"])
    gm = st.sb("gm", [128, 8], F32)
    st.load("sync", gm[:], T["g_mix"].rearrange("o (kt p) -> p (o kt)", p=128), "gm")
    Wm = st.sb("Wm", [128, 8, 2048], BF16)
    wst = [st.sb(f"wst{i}", [128, 2048], F32) for i in range(2)]
    for kt in range(8):
        i = kt % 2
        st.load("sync" if i == 0 else "gpsimd", wst[i][:], T["w_in"][0, kt * 128:(kt + 1) * 128, 2856:4904], f"wst{i}")
        st.do("vector" if i == 0 else "gpsimd", "tensor_scalar", out=Wm[:, kt, :], in0=wst[i][:], scalar1=gm[:, kt:kt + 1], scalar2=None,
              op0=ALU.mult, reads=[f"wst{i}", "gm"], writes=[("Wm", kt)])
    xt = [st.sb(f"xt{i}", [128, D], F32) for i in range(2)]
    sq = [st.sb(f"sq{i}", [128, D], BF16) for i in range(2)]
    xn = [st.sb(f"xn{i}", [128, D], BF16) for i in range(2)]
    ss = [st.sb(f"ss{i}", [128, 1], F32) for i in range(2)]
    rs = [st.sb(f"rs{i}", [128, 1], F32) for i in range(2)]
    uTt = [st.sb(f"uTt{i}", [128, 8, 128], BF16) for i in range(2)]
    sg = [st.sb(f"sg{i}", [128, 2048], BF16) for i in range(2)]
    ptr = [st.ps(f"ptr{i}", [128, 8, 128], BF16) for i in range(2)]
    pm = [st.ps(f"pm{i}", [128, 512], F32) for i in range(4)]
    for t in range(NTH):
        i = t % 2
        st.load("sync", xt[i][:], T["xh"][t * 128:(t + 1) * 128, :], f"xt{i}")
        rms_rstd(st, xt[i][:], f"xt{i}", sq, ss, rs, i, D)
        st.do("vector", "tensor_scalar", out=xn[i][:], in0=xt[i][:], scalar1=rs[i][:, 0:1], scalar2=None, op0=ALU.mult,
              reads=[f"xt{i}", f"rs{i}"], writes=[f"xn{i}"])
        for kt in range(8):
            st.do("tensor", "transpose", out=ptr[i][:, kt, :], in_=xn[i][:, kt * 128:(kt + 1) * 128], identity=ident[:],
                  reads=[f"xn{i}", "ident"], writes=[f"ptr{i}"])
        st.do("scalar", "copy", out=uTt[i][:], in_=ptr[i][:], reads=[f"ptr{i}"], writes=[f"uTt{i}"])
        for cc in range(4):
            for kt in range(8):
                st.do("tensor", "matmul", pm[cc][:], uTt[i][:, kt, :], Wm[:, kt, cc * 512:(cc + 1) * 512], start=(kt == 0), stop=(kt == 7),
                      reads=[f"uTt{i}", ("Wm", kt)], writes=[f"pm{cc}"])
            st.do("scalar", "activation", out=sg[i][:, cc * 512:(cc + 1) * 512], in_=pm[cc][:], func=AF.Sigmoid,
                  reads=[f"pm{cc}"], writes=[(f"sg{i}", cc)])
        st.dma("gpsimd", lambda e, t=t, i=i: e.dma_start(out=scr["SIGM"][t * 128:(t + 1) * 128, :], in_=sg[i][:]),
               f"sg{i}", reads=[(f"sg{i}", cc) for cc in range(4)])
    st.emit()


def stage_E1(nc, S, T, C, scr, P, ncores):
    st = Stage(nc, "E1")
    TH = S // 2
    NTH = TH // 128
    h1, vT, combT = P["h1"], P["vT"], P["combT"]
    identb = st.sb("identb", [128, 128], BF16)
    st.dma("gpsimd", lambda e: e.dma_start(out=identb[:], in_=C["ident"]), "identb", writes=["identb"])
    Wpn = st.sb("Wpn", [128, 4, D], BF16)
    Wpg = st.sb("Wpg", [128, 4, D], BF16)
    Wo = st.sb("Wo", [128, 8, D], BF16)
    st.dma("gpsimd", lambda e: e.dma_start(out=Wpn[:], in_=T["w_proj_nsa"][0].rearrange("(j p) d -> p j d", p=128)), "Wpn", writes=["Wpn"])
    st.dma("gpsimd", lambda e: e.dma_start(out=Wpg[:], in_=T["w_proj_gla"][0].rearrange("(j p) d -> p j d", p=128)), "Wpg", writes=["Wpg"])
    st.dma("gpsimd", lambda e: e.dma_start(out=Wo[:], in_=T["w_out"][0].rearrange("(j p) d -> p j d", p=128)), "Wo", writes=["Wo"])
    Wr = st.sb("Wr", [128, 8, 36], BF16)
    st.dma("gpsimd", lambda e: e.dma_start(out=Wr[:, :, 0:4], in_=T["w_grp"][0].rearrange("(j p) g -> p j g", p=128)), "Wr", writes=["Wr"])
    st.dma("gpsimd", lambda e: e.dma_start(out=Wr[:, :, 4:36], in_=T["w_exp"][0].rearrange("(j p) g -> p j g", p=128)), "Wr", writes=["Wr"])
    br = st.sb("br", [128, 36], F32)
    st.load("sync", br[:, 0:4], bcast_rows(T["b_grp"], 128), "br")
    st.load("sync", br[:, 4:36], bcast_rows(T["b_exp"], 128), "br")
    gfb = st.sb("gfb", [128, D], F32)
    st.load("sync", gfb[:], bcast_rows(T["g_ffn"], 128), "gfb")

    xt = [st.sb(f"xt{i}", [128, D], F32) for i in range(2)]
    on = [st.sb(f"on{i}", [128, 512], BF16) for i in range(2)]
    og = [st.sb(f"og{i}", [128, 512], BF16) for i in range(2)]
    sgm = [st.sb(f"sgm{i}", [128, 2048], BF16) for i in range(2)]
    onT = [st.sb(f"onT{i}", [128, 8, 128], BF16) for i in range(2)]
    m1 = st.sb("m1", [128, D], F32)
    m2 = st.sb("m2", [128, D], F32)
    mx_ = st.sb("mixed", [128, D], BF16)
    mT = st.sb("mT", [128, 8, 128], BF16)
    sq = [st.sb(f"sq{i}", [128, D], BF16) for i in range(2)]
    ss = [st.sb(f"ss{i}", [128, 1], F32) for i in range(2)]
    rs = [st.sb(f"rs{i}", [128, 1], F32) for i in range(2)]
    vnb = st.sb("vnb", [128, D], BF16)
    combb = st.sb("combb", [128, 32], BF16)
    lg = st.sb("lg", [128, 36], F32)
    sm = st.sb("sm", [128, 8], F32)
    bg = st.sb("bg", [128, 4], F32)
    lem = st.sb("lem", [128, 32], F32)
    top8 = st.sb("top8", [128, 8], F32)
    msk = st.sb("msk", [128, 32], F32)
    ex = st.sb("ex", [128, 32], F32)
    comb = st.sb("comb", [128, 32], F32)
    eg = st.sb("eg", [128, 4], F32)

    pT = [st.ps(f"pT{i}", [128, 8, 128], BF16) for i in range(2)]
    py = [st.ps(f"py{i}", [128, 512], F32) for i in range(4)]

    for t in range(NTH):
        i = t % 2
        rows = slice(t * 128, (t + 1) * 128)
        st.load("sync", xt[i][:], T["xh"][rows, :], f"xt{i}")
        st.load("sync", sgm[i][:], scr["SIGM"][rows, :], f"sgm{i}")
        st.dma_percore(ncores, lambda e, k, i=i, t=t: e.dma_start(out=on[i][:], in_=scr["ONSA"][(k % 2) * TH + t * 128:(k % 2) * TH + (t + 1) * 128, :]),
                       f"on{i}", writes=[f"on{i}"])
        st.dma_percore(ncores, lambda e, k, i=i, t=t: e.dma_start(out=og[i][:], in_=scr["OGLA"][(k % 2) * TH + t * 128:(k % 2) * TH + (t + 1) * 128, :]),
                       f"og{i}", writes=[f"og{i}"])
        for j in range(4):
            st.do("tensor", "transpose", out=pT[0][:, j, :], in_=on[i][:, j * 128:(j + 1) * 128], identity=identb[:],
                  reads=[f"on{i}", "identb"], writes=["pT0"])
        for j in range(4):
            st.do("tensor", "transpose", out=pT[0][:, 4 + j, :], in_=og[i][:, j * 128:(j + 1) * 128], identity=identb[:],
                  reads=[f"og{i}", "identb"], writes=["pT0"])
        st.do("scalar", "copy", out=onT[i][:], in_=pT[0][:], reads=["pT0"], writes=[f"onT{i}"])
        for hf in range(2):
            cs_ = slice(hf * 512, (hf + 1) * 512)
            for j in range(4):
                st.do("tensor", "matmul", py[hf][:], onT[i][:, j, :], Wpn[:, j, cs_], start=(j == 0), stop=(j == 3),
                      reads=[f"onT{i}", "Wpn"], writes=[f"py{hf}"])
            for j in range(4):
                st.do("tensor", "matmul", py[2 + hf][:], onT[i][:, 4 + j, :], Wpg[:, j, cs_], start=(j == 0), stop=(j == 3),
                      reads=[f"onT{i}", "Wpg"], writes=[f"py{2 + hf}"])
            st.do("vector", "tensor_tensor", out=m1[:, cs_], in0=py[hf][:], in1=sgm[i][:, hf * 512:(hf + 1) * 512], op=ALU.mult,
                  reads=[f"py{hf}", f"sgm{i}"], writes=[("m1", hf)])
            st.do("vector", "tensor_tensor", out=m2[:, cs_], in0=py[2 + hf][:], in1=sgm[i][:, 1024 + hf * 512:1024 + (hf + 1) * 512], op=ALU.mult,
                  reads=[f"py{2 + hf}", f"sgm{i}"], writes=[("m2", hf)])
            st.do("vector", "tensor_tensor", out=mx_[:, cs_], in0=m1[:, cs_], in1=m2[:, cs_], op=ALU.add,
                  reads=[("m1", hf), ("m2", hf)], writes=[("mixed", hf)])
        for kt in range(8):
            st.do("tensor", "transpose", out=pT[1][:, kt, :], in_=mx_[:, kt * 128:(kt + 1) * 128], identity=identb[:],
                  reads=[("mixed", kt // 4), "identb"], writes=["pT1"])
        st.do("scalar", "copy", out=mT[:], in_=pT[1][:], reads=["pT1"], writes=["mT"])
        for hf in range(2):
            cs_ = slice(hf * 512, (hf + 1) * 512)
            for kt in range(8):
                st.do("tensor", "matmul", py[hf][:], mT[:, kt, :], Wo[:, kt, cs_], start=(kt == 0), stop=(kt == 7),
                      reads=["mT", "Wo"], writes=[f"py{hf}"])
            st.do("vector", "tensor_tensor", out=h1[:, t, cs_], in0=py[hf][:], in1=xt[i][:, cs_], op=ALU.add,
                  reads=[f"py{hf}", f"xt{i}"], writes=[("h1", t, hf)])
        st.do("scalar", "activation", out=sq[i][:], in_=h1[:, t, :], func=AF.Square, accum_out=ss[i][:],
              reads=[("h1", t, 0), ("h1", t, 1)], writes=[f"sq{i}", f"ss{i}"])
        st.do("vector", "tensor_scalar", out=rs[i][:], in0=ss[i][:], scalar1=1.0 / D, scalar2=1e-6, op0=ALU.mult, op1=ALU.add,
              reads=[f"ss{i}"], writes=[f"rs{i}"])
        st.do("scalar", "activation", out=rs[i][:], in_=rs[i][:], func=AF.Sqrt, reads=[f"rs{i}"], writes=[f"rs{i}"])
        st.do("vector", "reciprocal", out=rs[i][:], in_=rs[i][:], reads=[f"rs{i}"], writes=[f"rs{i}"])
        st.do("vector", "scalar_tensor_tensor", out=vnb[:], in0=h1[:, t, :], scalar=rs[i][:, 0:1], in1=gfb[:], op0=ALU.mult, op1=ALU.mult,
              reads=[("h1", t, 0), ("h1", t, 1), f"rs{i}", "gfb"], writes=["vn"])
        for kt in range(8):
            st.do("tensor", "transpose", out=pT[0][:, kt, :], in_=vnb[:, kt * 128:(kt + 1) * 128], identity=identb[:],
                  reads=["vn", "identb"], writes=["pT0"])
        st.do("scalar", "copy", out=vT[:, :, rows], in_=pT[0][:], reads=["pT0"], writes=[("vT", t, 0), ("vT", t, 1)])
        for kt in range(8):
            st.do("tensor", "matmul", py[2][:, 0:36], vT[:, kt, rows], Wr[:, kt, :], start=(kt == 0), stop=(kt == 7),
                  reads=[("vT", t, 0), ("vT", t, 1), "Wr"], writes=["py2"])
        st.do("vector", "tensor_tensor", out=lg[:], in0=py[2][:, 0:36], in1=br[:], op=ALU.add, reads=["py2", "br"], writes=["lg"])
        st.do("vector", "reduce_max", out=sm[:, 0:1], in_=lg[:, 0:4], axis=AX.X, reads=["lg"], writes=["sm0"])
        st.do("vector", "tensor_scalar", out=bg[:], in0=lg[:, 0:4], scalar1=sm[:, 0:1], scalar2=-NEG, op0=ALU.is_ge, op1=ALU.mult,
              reads=["lg", "sm0"], writes=["bg"])
        st.do("vector", "tensor_scalar", out=bg[:], in0=bg[:], scalar1=NEG, scalar2=None, op0=ALU.add, reads=["bg"], writes=["bg"])
        st.do("vector", "tensor_scalar", out=sm[:, 1:2], in0=sm[:, 0:1], scalar1=-1.0, scalar2=None, op0=ALU.mult, reads=["sm0"], writes=["sm1"])
        st.do("scalar", "activation", out=eg[:], in_=lg[:, 0:4], func=AF.Exp, bias=sm[:, 1:2], accum_out=sm[:, 2:3],
              reads=["lg", "sm1"], writes=["eg", "sm2"])
        st.do("vector", "tensor_tensor", out=lem[:].rearrange("p (g e) -> p g e", e=8), in0=lg[:, 4:36].rearrange("p (g e) -> p g e", e=8),
              in1=bg[:].unsqueeze(2).broadcast_to([128, 4, 8]), op=ALU.add, reads=["lg", "bg"], writes=["lem"])
        st.do("vector", "max", out=top8[:], in_=lem[:], reads=["lem"], writes=["top8"])
        st.do("vector", "tensor_scalar", out=msk[:], in0=lem[:], scalar1=top8[:, 1:2], scalar2=None, op0=ALU.is_ge, reads=["lem", "top8"], writes=["msk"])
        st.do("vector", "tensor_scalar", out=sm[:, 3:4], in0=top8[:, 0:1], scalar1=-1.0, scalar2=None, op0=ALU.mult, reads=["top8"], writes=["sm3"])
        st.do("scalar", "activation", out=ex[:], in_=lem[:], func=AF.Exp, bias=sm[:, 3:4], reads=["lem", "sm3"], writes=["ex"])
        st.do("vector", "tensor_tensor", out=ex[:], in0=ex[:], in1=msk[:], op=ALU.mult, reads=["ex", "msk"], writes=["ex"])
        st.do("vector", "reduce_sum", out=sm[:, 4:5], in_=ex[:], axis=AX.X, reads=["ex"], writes=["sm4"])
        st.do("vector", "tensor_tensor", out=sm[:, 5:6], in0=sm[:, 4:5], in1=sm[:, 2:3], op=ALU.mult, reads=["sm4", "sm2"], writes=["sm5"])
        st.do("vector", "reciprocal", out=sm[:, 5:6], in_=sm[:, 5:6], reads=["sm5"], writes=["sm5"])
        st.do("vector", "tensor_scalar", out=comb[:], in0=ex[:], scalar1=sm[:, 5:6], scalar2=None, op0=ALU.mult, reads=["ex", "sm5"], writes=["comb"])
        st.do("vector", "tensor_copy", out=combb[:], in_=comb[:], reads=["comb"], writes=["combb"])
        st.do("tensor", "transpose", out=pT[1][0:32, 0, :], in_=combb[:], identity=identb[:], reads=["combb", "identb"], writes=["pT1"])
        st.do("scalar", "copy", out=combT[:, rows], in_=pT[1][0:32, 0, :], reads=["pT1"], writes=[("combT", t)])
    st.emit()


def stage_E2(nc, S, T, C, scr, P, out):
    st = Stage(nc, "E2")
    TH = S // 2
    NTH = TH // 128
    CH = min(512, TH)
    NCH = TH // CH
    TPC = CH // 128
    h1, vT, combT = P["h1"], P["vT"], P["combT"]
    identb = st.sb("identb", [128, 128], BF16)
    st.dma("gpsimd", lambda e: e.dma_start(out=identb[:], in_=C["ident"]), "identb", writes=["identb"])
    selall = st.sb("selall", [32, 32, 128], BF16)
    st.do("vector", "tensor_copy", out=selall[:], in_=identb[0:32, 0:32].unsqueeze(2).broadcast_to([32, 32, 128]),
          reads=["identb"], writes=["selall"])
    Wg = [st.sb(f"Wg{i}", [128, 8, 512], BF16) for i in range(2)]
    Wu = [st.sb(f"Wu{i}", [128, 8, 512], BF16) for i in range(2)]
    Wd = [st.sb(f"Wd{i}", [128, 4, D], BF16) for i in range(2)]
    cB = [st.sb(f"cB{i}", [128, CH], BF16) for i in range(2)]
    sgl = [st.sb(f"sgl{i}", [128, CH], BF16) for i in range(2)]
    tu = [st.sb(f"tu{i}", [128, CH], BF16) for i in range(2)]
    hT = [st.sb(f"hT{i}", [128, 4, CH], BF16) for i in range(2)]
    pcb = st.ps("pcb", [128, 512], F32)
    pG = [st.ps(f"pG{i}", [128, 512], F32) for i in range(2)]
    pU = [st.ps(f"pU{i}", [128, 512], F32) for i in range(2)]
    py = [st.ps(f"py{i}", [128, 512], F32) for i in range(2)]
    gu = 0
    hb = 0
    def load_expert(ex_):
        w = ex_ % 2
        st.dma("gpsimd", lambda e, w=w, ex_=ex_: e.dma_start(out=Wg[w][:], in_=T["w_gate"][0, ex_].rearrange("(kt p) f -> p kt f", p=128)),
               f"Wg{w}", writes=[f"Wg{w}"])
        st.dma("gpsimd", lambda e, w=w, ex_=ex_: e.dma_start(out=Wu[w][:], in_=T["w_up"][0, ex_].rearrange("(kt p) f -> p kt f", p=128)),
               f"Wu{w}", writes=[f"Wu{w}"])
        st.dma("gpsimd", lambda e, w=w, ex_=ex_: e.dma_start(out=Wd[w][:], in_=T["w_down"][0, ex_].rearrange("(fc p) d -> p fc d", p=128)),
               f"Wd{w}", writes=[f"Wd{w}"])

    load_expert(0)
    for ex_ in range(32):
        w = ex_ % 2
        if ex_ + 1 < 32:
            load_expert(ex_ + 1)
        for tc in range(NCH):
            tsl = slice(tc * CH, (tc + 1) * CH)
            cbi = hb % 2
            hb += 1
            ckeys = [("combT", tc * TPC + j) for j in range(TPC)]
            vkeys = [("vT", tc * TPC + j, q) for j in range(TPC) for q in range(2)]
            st.do("tensor", "matmul", pcb[:, 0:CH], selall[:, ex_, :], combT[:, tsl], start=True, stop=True,
                  reads=["selall"] + ckeys, writes=["pcb"])
            st.do("scalar", "copy", out=cB[cbi][:], in_=pcb[:, 0:CH], reads=["pcb"], writes=[f"cB{cbi}"])
            for fc in range(4):
                g = gu % 2
                gu += 1
                fsl = slice(fc * 128, (fc + 1) * 128)
                for kt in range(8):
                    st.do("tensor", "matmul", pG[g][:, 0:CH], Wg[w][:, kt, fsl], vT[:, kt, tsl], start=(kt == 0), stop=(kt == 7),
                          reads=[f"Wg{w}"] + vkeys, writes=[f"pG{g}"])
                for kt in range(8):
                    st.do("tensor", "matmul", pU[g][:, 0:CH], Wu[w][:, kt, fsl], vT[:, kt, tsl], start=(kt == 0), stop=(kt == 7),
                          reads=[f"Wu{w}"] + vkeys, writes=[f"pU{g}"])
                st.do("scalar", "activation", out=sgl[g][:], in_=pG[g][:, 0:CH], func=AF.Silu, reads=[f"pG{g}"], writes=[f"sgl{g}"])
                st.do("vector", "tensor_tensor", out=tu[g][:], in0=pU[g][:, 0:CH], in1=cB[cbi][:], op=ALU.mult,
                      reads=[f"pU{g}", f"cB{cbi}"], writes=[f"tu{g}"])
                st.do("gpsimd", "tensor_tensor", out=hT[cbi][:, fc, :], in0=sgl[g][:], in1=tu[g][:], op=ALU.mult,
                      reads=[f"sgl{g}", f"tu{g}"], writes=[(f"hT{cbi}", fc)])
            for tt in range(TPC):
                t = tc * TPC + tt
                for hf in range(2):
                    cs_ = slice(hf * 512, (hf + 1) * 512)
                    for fc in range(4):
                        st.do("tensor", "matmul", py[hf][:], hT[cbi][:, fc, tt * 128:(tt + 1) * 128], Wd[w][:, fc, cs_],
                              start=(fc == 0), stop=(fc == 3), reads=[(f"hT{cbi}", f) for f in range(4)] + [f"Wd{w}"], writes=[f"py{hf}"])
                    st.do("vector", "tensor_tensor", out=h1[:, t, cs_], in0=py[hf][:], in1=h1[:, t, cs_], op=ALU.add,
                          reads=[f"py{hf}", ("h1", t, hf)], writes=[("h1", t, hf)])
    gfin = st.sb("gfin", [128, D], F32)
    st.load("sync", gfin[:], bcast_rows(T["g_final"], 128), "gfin")
    sq = [st.sb(f"sq{i}", [128, D], BF16) for i in range(2)]
    ss = [st.sb(f"ss{i}", [128, 1], F32) for i in range(2)]
    rs = [st.sb(f"rs{i}", [128, 1], F32) for i in range(2)]
    ot = [st.sb(f"ot{i}", [128, D], F32) for i in range(2)]
    for t in range(NTH):
        i = t % 2
        st.do("scalar", "activation", out=sq[i][:], in_=h1[:, t, :], func=AF.Square, accum_out=ss[i][:],
              reads=[("h1", t, 0), ("h1", t, 1)], writes=[f"sq{i}", f"ss{i}"])
        st.do("vector", "tensor_scalar", out=rs[i][:], in0=ss[i][:], scalar1=1.0 / D, scalar2=1e-6, op0=ALU.mult, op1=ALU.add,
              reads=[f"ss{i}"], writes=[f"rs{i}"])
        st.do("scalar", "activation", out=rs[i][:], in_=rs[i][:], func=AF.Sqrt, reads=[f"rs{i}"], writes=[f"rs{i}"])
        st.do("vector", "reciprocal", out=rs[i][:], in_=rs[i][:], reads=[f"rs{i}"], writes=[f"rs{i}"])
        st.do("vector", "scalar_tensor_tensor", out=ot[i][:], in0=h1[:, t, :], scalar=rs[i][:, 0:1], in1=gfin[:], op0=ALU.mult, op1=ALU.mult,
              reads=[("h1", t, 0), ("h1", t, 1), f"rs{i}", "gfin"], writes=[f"ot{i}"])
        st.store("sync", out[t * 128:(t + 1) * 128, :], ot[i][:], f"ot{i}")
    st.emit()


def build_full(S, ncores, stages="RABCDE", debug=(), scr_in=()):
    nc = bass.Bass("TRN2", target_bir_lowering=False)
    T = {k: nc.dram_tensor(k, shp, dt, kind="ExternalInput").ap() for k, (shp, dt) in IN_SHAPES(S).items()}
    C = {k: nc.dram_tensor("c_" + k, shp, F32, kind="ExternalInput").ap() for k, shp in CONST_SHAPES.items()}
    scr = {}
    spec = dict(SCR_SPEC(S))
    spec["SIGM"] = ([S // 2, 2048], BF16)
    for k, (shp, dt) in spec.items():
        kind = "ExternalOutput" if k in debug else ("ExternalInput" if k in scr_in else "Internal")
        scr[k] = nc.dram_tensor("scr_" + k, shp, dt, kind=kind).ap()
    out = nc.dram_tensor("out", [S // 2, D], F32, kind="ExternalOutput").ap()
    TH = S // 2
    with nc.allow_non_contiguous_dma(reason="small strided parameter loads"), ExitStack() as es:
        if "R" in stages:
            stage_R(nc, S, T, C, scr)
        if "A" in stages:
            stage_A(nc, S, T, C, scr)
        if "B" in stages:
            stage_B(nc, S, T, C, scr)
        if "C" in stages:
            stage_C(nc, S, T, C, scr)
        if "D" in stages:
            stage_D(nc, S, T, C, scr)
        if "E" in stages:
            if "0" in DBG_E:
                stage_E0(nc, S, T, C, scr)
            P = {"h1": es.enter_context(nc.sbuf_tensor("P_h1", [128, TH // 128, D], F32)),
                 "vT": es.enter_context(nc.sbuf_tensor("P_vT", [128, 8, TH], BF16)),
                 "combT": es.enter_context(nc.sbuf_tensor("P_combT", [32, TH], BF16))}
            if "1" in DBG_E:
                stage_E1(nc, S, T, C, scr, P, ncores)
            if "2" in DBG_E:
                stage_E2(nc, S, T, C, scr, P, out)
    return nc


SEQ = 4096
BATCH = 4
_CACHE = {}


def kernel(**inputs):
    S = SEQ
    ncores = 8
    if "nc" not in _CACHE:
        _CACHE["nc"] = build_full(S, ncores)
    nc = _CACHE["nc"]
    consts = host_consts(S)
    shapes = IN_SHAPES(S)
    x = np.ascontiguousarray(np.asarray(inputs["x"], dtype=np.float32))
    pos = np.ascontiguousarray(np.asarray(inputs["positions"]).astype(np.int32))
    in_maps = []
    for core in range(ncores):
        b, half = core // 2, core % 2
        m = {}
        for k, (shp, dt) in shapes.items():
            if k == "x":
                m[k] = x[b]
            elif k == "xh":
                m[k] = np.ascontiguousarray(x[b, half * S // 2:(half + 1) * S // 2])
            elif k == "positions":
                m[k] = pos[b:b + 1]
            elif k == "halfidx":
                m[k] = np.array([[half]], np.int32)
            elif k == "g_final":
                m[k] = np.asarray(inputs[k], dtype=np.float32).reshape(1, -1)
            else:
                m[k] = np.ascontiguousarray(np.asarray(inputs[k], dtype=np.float32))
        for k, v in consts.items():
            m["c_" + k] = v
        in_maps.append(m)
    res = run_bass_kernel_spmd(nc, in_maps, core_ids=list(range(ncores)))
    out = np.empty((BATCH, S, D), np.float32)
    for core in range(ncores):
        b, half = core // 2, core % 2
        out[b, half * S // 2:(half + 1) * S // 2] = res.results[core]["out"]
    return out
```
